# Optimizing a Trainium2 kernel written in Bass

```python
import math
import jax, jax.numpy as jnp
from jax import lax
import numpy as np

D_MODEL = 1024
BATCH = 32
SEQ = 2048
DEPTH = 1

CHUNK = 64
Q_BLOCK = 128

MIX_WIDTH = D_MODEL
HG_WIDTH = MIX_WIDTH // 2
HG_HEADS = 4
HG_DK = HG_WIDTH // HG_HEADS
FOX_WIDTH = MIX_WIDTH - HG_WIDTH
FOX_HEADS = 8
FOX_DH = FOX_WIDTH // FOX_HEADS
IN_COLS = 4 * HG_WIDTH + 3 * FOX_WIDTH + FOX_HEADS
N_EXPERT_GROUPS = 4
EXPERTS_PER_GROUP = 4
N_EXPERTS = N_EXPERT_GROUPS * EXPERTS_PER_GROUP
TOP_K = 2
EXPERT_HIDDEN = D_MODEL // 2
NORM_EPS = 1e-6
NEG_INF = -1e30

kernel_name = "hybrid_hgrn2_fox_hiermoe_block"


def rms_norm(x, gain):
    xf = x.astype(jnp.float32)
    y = xf * lax.rsqrt(jnp.mean(xf * xf, axis=-1, keepdims=True) + NORM_EPS)
    return (y * gain.astype(jnp.float32)).astype(x.dtype)


def hgrn2_mixer(q_raw, f_raw, i_raw, g_raw, lower_bound, norm_gain):
    B, S, _ = q_raw.shape
    nc = S // CHUNK
    f32 = jnp.float32
    q = jax.nn.silu(q_raw.astype(f32))
    lb = lower_bound.astype(f32)
    f = lb + (1.0 - lb) * jax.nn.sigmoid(f_raw.astype(f32))
    log_f = jnp.log(f)
    k = 1.0 - f
    v = i_raw.astype(f32)

    def to_chunks(t):
        return t.reshape(B, nc, CHUNK, HG_HEADS, HG_DK).transpose(1, 0, 3, 2, 4)

    causal = jnp.tril(jnp.ones((CHUNK, CHUNK), dtype=bool))

    def step(state, inp):
        qc, kc, vc, lfc = inp
        b = jnp.cumsum(lfc, axis=2)
        o_inter = jnp.einsum('bhtk,bhkv->bhtv', qc * jnp.exp(b), state)
        diff = b[:, :, :, None, :] - b[:, :, None, :, :]
        decay = jnp.where(causal[:, :, None], jnp.exp(jnp.minimum(diff, 0.0)), 0.0)
        scores = jnp.einsum('bhtk,bhtsk,bhsk->bhts', qc, decay, kc)
        o_intra = jnp.einsum('bhts,bhsv->bhtv', scores, vc)
        b_last = b[:, :, -1:, :]
        new_state = (jnp.exp(b_last[:, :, 0, :])[..., None] * state
                     + jnp.einsum('bhsk,bhsv->bhkv', kc * jnp.exp(b_last - b), vc))
        return new_state, o_inter + o_intra

    s0 = jnp.zeros((B, HG_HEADS, HG_DK, HG_DK), f32)
    _, o = lax.scan(step, s0, (to_chunks(q), to_chunks(k), to_chunks(v), to_chunks(log_f)))
    o = o.transpose(1, 0, 3, 2, 4).reshape(B, S, HG_HEADS, HG_DK)
    o = rms_norm(o, norm_gain)
    o = o.reshape(B, S, HG_WIDTH) * jax.nn.silu(g_raw.astype(f32))
    return o.astype(q_raw.dtype)


def fox_mixer(q_raw, k_raw, v_raw, f_raw, f_bias, norm_gain):
    B, S, _ = q_raw.shape
    f32 = jnp.float32

    def heads(t):
        return t.astype(f32).reshape(B, S, FOX_HEADS, FOX_DH).transpose(0, 2, 1, 3)

    q = heads(q_raw) * (FOX_DH ** -0.5)
    k = heads(k_raw)
    v = heads(v_raw)
    log_f = jax.nn.log_sigmoid(f_raw.astype(f32) + f_bias.astype(f32))
    cum = jnp.cumsum(log_f, axis=1).transpose(0, 2, 1)
    key_pos = jnp.arange(S)

    def block(i):
        start = i * Q_BLOCK
        qb = lax.dynamic_slice_in_dim(q, start, Q_BLOCK, axis=2)
        cb = lax.dynamic_slice_in_dim(cum, start, Q_BLOCK, axis=2)
        logits = jnp.einsum('bhtd,bhsd->bhts', qb, k) + (cb[..., :, None] - cum[..., None, :])
        mask = (start + jnp.arange(Q_BLOCK))[:, None] >= key_pos[None, :]
        logits = jnp.where(mask, logits, NEG_INF)
        p = jax.nn.softmax(logits, axis=-1)
        return jnp.einsum('bhts,bhsd->bhtd', p, v)

    o = lax.map(block, jnp.arange(S // Q_BLOCK))
    o = o.transpose(1, 0, 3, 2, 4).reshape(B, S, FOX_HEADS, FOX_DH)
    o = rms_norm(o, norm_gain)
    return o.reshape(B, S, FOX_WIDTH).astype(q_raw.dtype)


def hier_moe(x, w_group, b_group, w_expert, b_expert, w_gate, w_up, w_down):
    B, S, D = x.shape
    t = x.reshape(-1, D)
    pg = jax.nn.softmax((t @ w_group + b_group).astype(jnp.float32), axis=-1)
    gsel = jnp.argmax(pg, axis=-1)
    pg_sel = jnp.max(pg, axis=-1)
    e_logits = (t @ w_expert + b_expert).astype(jnp.float32).reshape(-1, N_EXPERT_GROUPS, EXPERTS_PER_GROUP)
    e_logits = jnp.take_along_axis(e_logits, gsel[:, None, None], axis=1)[:, 0]
    pe = jax.nn.softmax(e_logits, axis=-1)
    top_p, top_i = lax.top_k(pe, TOP_K)
    top_p = top_p / jnp.sum(top_p, axis=-1, keepdims=True)
    expert_id = gsel[:, None] * EXPERTS_PER_GROUP + top_i
    weights = pg_sel[:, None] * top_p
    gates = jnp.sum(jax.nn.one_hot(expert_id, N_EXPERTS, dtype=jnp.float32) * weights[..., None], axis=1)
    gates = gates.astype(t.dtype)
    y = jnp.zeros_like(t)
    for e in range(N_EXPERTS):
        h = jax.nn.silu(t @ w_gate[e]) * (t @ w_up[e])
        y = y + gates[:, e:e + 1] * (h @ w_down[e])
    return y.reshape(B, S, D)


def setup_inputs(seed: int = 0) -> dict:
    key = jax.random.key(seed)
    ks = jax.random.split(key, 20)
    f32 = jnp.float32
    L, D = DEPTH, D_MODEL
    nrm = lambda k, shape, scale: jax.random.normal(k, shape, f32) * scale
    return {
        "x": jax.random.normal(ks[0], (BATCH, SEQ, D), f32),
        "attn_norm": 1.0 + nrm(ks[1], (L, D), 0.02),
        "w_in": nrm(ks[2], (L, D, IN_COLS), D ** -0.5),
        "hg_lb_logits": nrm(ks[3], (L + 1, HG_WIDTH), 0.5),
        "hg_norm": 1.0 + nrm(ks[4], (L, HG_DK), 0.02),
        "fox_f_bias": nrm(ks[5], (L, FOX_HEADS), 0.1),
        "fox_norm": 1.0 + nrm(ks[6], (L, FOX_DH), 0.02),
        "w_out": nrm(ks[7], (L, MIX_WIDTH, D), MIX_WIDTH ** -0.5),
        "ffn_norm": 1.0 + nrm(ks[8], (L, D), 0.02),
        "w_group": nrm(ks[9], (L, D, N_EXPERT_GROUPS), D ** -0.5),
        "b_group": nrm(ks[10], (L, N_EXPERT_GROUPS), 0.01),
        "w_expert": nrm(ks[11], (L, D, N_EXPERTS), D ** -0.5),
        "b_expert": nrm(ks[12], (L, N_EXPERTS), 0.01),
        "w_gate": nrm(ks[13], (L, N_EXPERTS, D, EXPERT_HIDDEN), D ** -0.5),
        "w_up": nrm(ks[14], (L, N_EXPERTS, D, EXPERT_HIDDEN), D ** -0.5),
        "w_down": nrm(ks[15], (L, N_EXPERTS, EXPERT_HIDDEN, D), EXPERT_HIDDEN ** -0.5),
        "final_norm": 1.0 + nrm(ks[16], (D,), 0.02),
    }


def reference(x, attn_norm, w_in, hg_lb_logits, hg_norm, fox_f_bias, fox_norm, w_out, ffn_norm,
              w_group, b_group, w_expert, b_expert, w_gate, w_up, w_down, final_norm):
    lb_all = jnp.cumsum(jax.nn.softmax(hg_lb_logits.astype(jnp.float32), axis=0), axis=0)
    o0 = 0
    o1 = o0 + HG_WIDTH
    o2 = o1 + HG_WIDTH
    o3 = o2 + HG_WIDTH
    o4 = o3 + HG_WIDTH
    o5 = o4 + FOX_WIDTH
    o6 = o5 + FOX_WIDTH
    o7 = o6 + FOX_WIDTH
    h = x
    for l in range(DEPTH):
        xn = rms_norm(h, attn_norm[l])
        u = xn @ w_in[l]
        o_a = hgrn2_mixer(u[..., o0:o1], u[..., o1:o2], u[..., o2:o3], u[..., o3:o4],
                          lb_all[l], hg_norm[l])
        o_b = fox_mixer(u[..., o4:o5], u[..., o5:o6], u[..., o6:o7], u[..., o7:IN_COLS],
                        fox_f_bias[l], fox_norm[l])
        h = h + jnp.concatenate([o_a, o_b], axis=-1) @ w_out[l]
        h = h + hier_moe(rms_norm(h, ffn_norm[l]), w_group[l], b_group[l], w_expert[l], b_expert[l],
                         w_gate[l], w_up[l], w_down[l])
    return rms_norm(h, final_norm)
```

```python
import contextlib
import numpy as np
import ml_dtypes
import concourse.bass as bass
import concourse.mybir as mybir

F32 = mybir.dt.float32
BF16 = mybir.dt.bfloat16
U8 = mybir.dt.uint8
_ES = {F32: 4, BF16: 2, U8: 1, mybir.dt.int32: 4, mybir.dt.uint32: 4, mybir.dt.uint16: 2}


class Op:
    __slots__ = ("eng", "fn", "deps", "cdeps", "seq", "ms", "needed", "dma", "dsem", "dval", "dprev", "tag")

    def __init__(self, eng, fn, dma, tag):
        self.eng = eng
        self.fn = fn
        self.deps = set()
        self.cdeps = {}
        self.seq = 0
        self.ms = None
        self.needed = False
        self.dma = dma
        self.dsem = None
        self.dval = None
        self.dprev = 0
        self.tag = tag


class Sched:
    def __init__(self, n_dma_sems=8, same_engine_sync=True):
        self.ops = []
        self.recs = {}
        self.n_dma_sems = n_dma_sems
        self.same_engine_sync = same_engine_sync

    def _regions(self, ap):
        t = ap.tensor
        cls = type(t).__name__
        if cls.startswith("DRam"):
            return None
        psum = cls.startswith("PSum") or cls.startswith("Psum")
        name = ap.name
        if psum:
            return name, True, 0, 128, [(0, 1 << 20)]
        es = _ES[ap.dtype]
        pat = ap.ap
        off = int(ap.offset)
        pstride, pcnt = pat[0]
        p0 = off // pstride
        c0 = off % pstride
        free = list(pat[1:])
        if not free:
            return name, False, p0, p0 + pcnt, [(c0 * es, (c0 + 1) * es)]
        ls, ln = free[-1]
        run = (ln - 1) * abs(ls) + 1
        outer = free[:-1]
        nout = 1
        for s, n in outer:
            nout *= n
        ivs = []
        if nout <= 64:
            idx = [0] * len(outer)
            while True:
                st = c0 + sum(i * s for i, (s, n) in zip(idx, outer))
                ivs.append((st * es, (st + run) * es))
                k = len(outer) - 1
                while k >= 0:
                    idx[k] += 1
                    if idx[k] < outer[k][1]:
                        break
                    idx[k] = 0
                    k -= 1
                if k < 0:
                    break
        else:
            hi = c0 + sum((n - 1) * abs(s) for s, n in outer) + run
            ivs.append((c0 * es, hi * es))
        ivs.sort()
        out = [ivs[0]]
        for a, b in ivs[1:]:
            if a <= out[-1][1]:
                out[-1] = (out[-1][0], max(out[-1][1], b))
            else:
                out.append((a, b))
        return name, False, p0, p0 + pcnt, out

    def _access(self, op, ap, is_write):
        r = self._regions(ap)
        if r is None:
            return
        name, psum, p0, p1, ivs = r
        tab = self.recs.setdefault(name, {})
        w = is_write or psum
        SH = 11
        for (b0, b1) in ivs:
            newrec = (p0, p1, b0, b1, op, w)
            for bk in range(b0 >> SH, ((b1 - 1) >> SH) + 1):
                lst = tab.get(bk)
                if lst is None:
                    tab[bk] = [newrec]
                    continue
                keep = []
                for rec in lst:
                    rp0, rp1, rb0, rb1, rop, rw = rec
                    ov = rp0 < p1 and p0 < rp1 and rb0 < b1 and b0 < rb1
                    if ov and (rw or w) and rop is not op:
                        if rop.dma:
                            op.deps.add(rop)
                        else:
                            c = op.cdeps.get(rop.eng)
                            if c is None or c.seq < rop.seq:
                                op.cdeps[rop.eng] = rop
                    if ov and w and rp0 >= p0 and rp1 <= p1 and rb0 >= b0 and rb1 <= b1:
                        continue
                    if (not w) and (not rw) and rop.eng == op.eng and not rop.dma and not op.dma \
                            and rp0 == p0 and rp1 == p1 and rb0 == b0 and rb1 == b1:
                        continue
                    keep.append(rec)
                keep.append(newrec)
                tab[bk] = keep

    def add(self, eng, fn, reads=(), writes=(), dma=False, tag=None):
        op = Op(eng, fn, dma, tag)
        op.seq = len(self.ops)
        for ap in reads:
            self._access(op, ap, False)
        for ap in writes:
            self._access(op, ap, True)
        op.deps.update(op.cdeps.values())
        op.cdeps = None
        self.ops.append(op)
        return op

    def prepare(self):
        for op in self.ops:
            for d in op.deps:
                if d.dma:
                    continue
                if d.eng == op.eng and (d.eng == "pe" or not self.same_engine_sync) and not op.dma:
                    continue
                d.needed = True
        cnt = {}
        dcnt = {}
        dslot = {}
        for op in self.ops:
            if op.dma:
                k = dslot.get(op.eng, 0)
                dslot[op.eng] = k + 1
                slot = (op.eng, k % self.n_dma_sems)
                prev = dcnt.get(slot, 0)
                op.dsem = slot
                op.dprev = prev
                op.dval = prev + 16
                dcnt[slot] = op.dval
            elif op.needed:
                cnt[op.eng] = cnt.get(op.eng, 0) + 1
                op.ms = cnt[op.eng]
        self.final_dma = dcnt
        self.ms_total = cnt

    def emit_engine(self, eng_name, eng, sems, dsems):
        waited = {}

        def need(sem_key, sem, val):
            if val <= 0:
                return
            if waited.get(sem_key, 0) >= val:
                return
            waited[sem_key] = val
            eng.wait_ge(sem, val)

        n = 0
        for op in self.ops:
            if op.eng != eng_name:
                continue
            for d in op.deps:
                if d.dma:
                    need(d.dsem, dsems[d.dsem], d.dval)
                else:
                    if d.eng == eng_name and not op.dma and (eng_name == "pe" or not self.same_engine_sync):
                        continue
                    need(d.eng, sems[d.eng], d.ms)
            if op.dma:
                need(op.dsem, dsems[op.dsem], op.dprev)
            ins = op.fn(eng)
            if op.dma:
                ins.then_inc(dsems[op.dsem], 16)
            elif op.needed:
                ins.then_inc(sems[eng_name], 1)
            n += 1
        for slot, val in self.final_dma.items():
            if slot[0] == eng_name:
                need(slot, dsems[slot], val)
        return n


from concourse.bass_utils import run_bass_kernel_spmd

AF = mybir.ActivationFunctionType
ALU = mybir.AluOpType
AX = mybir.AxisListType
I32 = mybir.dt.int32

D = 1024
S = 2048
NT = S // 128
HGW = 512
FOXW = 512
INC = 3592
NE = 16
EH = 512
EPS = 1e-6

ARENA = 208000
C_IDENT = 0
C_MASKFOX = 256
C_MASKHG = 512
C_CMATHG = 768
C_CVECA = 1024
C_CVECB = 1152
C_SEL = 1408
C_ONES = 3648
C_RESET = 7744
C_GATTN = 11840
C_GFFN = 15936
C_GFIN = 20032
C_SMALL = 24128
C_WOUT = 24640
C_WR = 41024
C_STAT = 41344
C_GATES = 42368
C_RT = 43392
XNT = 44544
OCAT = XNT + 32768
W = OCAT + 32768
WSIZE = ARENA - W
TT = W + 65536


def make_consts():
    bf = ml_dtypes.bfloat16
    ident = np.eye(128, dtype=np.float32).astype(bf)
    s_ = np.arange(128)[:, None]
    t_ = np.arange(128)[None, :]
    maskfox = np.where(t_ >= s_, 0.0, -30000.0).astype(np.float32).astype(bf)
    maskhg = ((t_ >= s_) & ((t_ // 64) == (s_ // 64))).astype(np.float32).astype(bf)
    cvecA = np.zeros((65, 64), np.float32)
    cvecA[0:64, :] = 1.0 / 64
    cvecA[64, :] = EPS
    cvecB = np.zeros((128, 128), np.float32)
    cvecB[0, 64:128] = EPS
    cvecB[64:128, 64:128] = 1.0 / 64
    sel = np.zeros((97, 16, 70), np.float32)
    for h in range(8):
        sel[h, h, 64] = 8.0
        sel[32 + h, h, 65] = 8.0
        sel[64 + h, h, 66] = 8.0
        sel[96, h, 67:70] = 8.0
        sel[96, 8 + h, 64:67] = 1.0
        sel[h, 8 + h, 67] = -1.0
        sel[32 + h, 8 + h, 68] = -1.0
        sel[64 + h, 8 + h, 69] = -1.0
    return {
        "c_ident": ident,
        "c_maskfox": maskfox,
        "c_maskhg": maskhg,
        "c_cveca": cvecA.astype(bf),
        "c_cvecb": cvecB.astype(bf),
        "c_sel": sel.reshape(97, 16 * 70).astype(bf),
    }


def build(nseq, Sched, F32, BF16, U8, stop_after=None, dbg=False):
    nc = bass.Bass("TRN2", target_bir_lowering=False)
    dt_ = nc.dram_tensor
    x_d = dt_("x", [nseq, S, D], F32, kind="ExternalInput").ap()
    win_d = dt_("w_in", [D, INC], F32, kind="ExternalInput").ap()
    wout_d = dt_("w_out", [D, D], F32, kind="ExternalInput").ap()
    wg_d = dt_("w_gate", [NE, D, EH], F32, kind="ExternalInput").ap()
    wu_d = dt_("w_up", [NE, D, EH], F32, kind="ExternalInput").ap()
    wd_d = dt_("w_down", [NE, EH, D], F32, kind="ExternalInput").ap()
    wr_d = dt_("w_router", [D, 20], F32, kind="ExternalInput").ap()
    br_d = dt_("b_router", [1, 20], F32, kind="ExternalInput").ap()
    gattn_d = dt_("attn_norm", [1, D], F32, kind="ExternalInput").ap()
    gffn_d = dt_("ffn_norm", [1, D], F32, kind="ExternalInput").ap()
    gfin_d = dt_("final_norm", [1, D], F32, kind="ExternalInput").ap()
    lbl_d = dt_("hg_lb_logits", [2, HGW], F32, kind="ExternalInput").ap()
    hgn_d = dt_("hg_norm", [1, 128], F32, kind="ExternalInput").ap()
    fb_d = dt_("fox_f_bias", [1, 8], F32, kind="ExternalInput").ap()
    fxn_d = dt_("fox_norm", [1, 64], F32, kind="ExternalInput").ap()
    ci_d = dt_("c_ident", [128, 128], BF16, kind="ExternalInput").ap()
    cmf_d = dt_("c_maskfox", [128, 128], BF16, kind="ExternalInput").ap()
    cmh_d = dt_("c_maskhg", [128, 128], BF16, kind="ExternalInput").ap()
    cva_d = dt_("c_cveca", [65, 64], BF16, kind="ExternalInput").ap()
    cvb_d = dt_("c_cvecb", [128, 128], BF16, kind="ExternalInput").ap()
    csel_d = dt_("c_sel", [97, 16 * 70], BF16, kind="ExternalInput").ap()
    out_d = dt_("out", [nseq, S, D], F32, kind="ExternalOutput").ap()
    dbg_d = None
    if dbg:
        dbg_d = dt_("dbg", [S, D], F32, kind="ExternalOutput").ap()

    sch = Sched()
    es = contextlib.ExitStack()
    arena = es.enter_context(nc.sbuf_tensor("arena", [128, ARENA], U8))
    banks = [es.enter_context(nc.psum_tensor(f"bank{i}", [128, 512], F32)) for i in range(8)]

    def sb(off, nbytes, dt):
        return arena[:, off:off + nbytes].bitcast(dt)

    def bankbf(i):
        return banks[i][:, :].bitcast(BF16)

    ident = sb(C_IDENT, 256, BF16)
    maskfox = sb(C_MASKFOX, 256, BF16)
    maskhg = sb(C_MASKHG, 256, BF16)
    cmathg = sb(C_CMATHG, 256, BF16)
    cvecA = sb(C_CVECA, 128, BF16)
    cvecB = sb(C_CVECB, 256, BF16)
    sel = sb(C_SEL, 2240, BF16).rearrange("p (a b) -> p a b", a=16)
    ones = sb(C_ONES, 4096, BF16)
    reset = sb(C_RESET, 4096, BF16)
    gattn = sb(C_GATTN, 4096, F32)
    gffn = sb(C_GFFN, 4096, F32)
    gfin = sb(C_GFIN, 4096, F32)
    small = sb(C_SMALL, 512, F32)
    lbc = small[:, 0:4]
    oml = small[:, 4:8]
    hgg = small[:, 8:9]
    foxg = small[:, 9:10]
    negb = small[:, 10:11]
    l0 = small[:, 11:15]
    l1 = small[:, 15:19]
    ltmp = small[:, 19:23]
    brt = small[:, 24:44]
    wout = sb(C_WOUT, 16384, BF16).rearrange("p (k c) -> p k c", k=8)
    wr = sb(C_WR, 320, BF16).rearrange("p (k c) -> p k c", k=8)
    stat = sb(C_STAT, 1024, F32)
    gates = sb(C_GATES, 1024, F32).rearrange("p (t e) -> p t e", e=16)
    rt = sb(C_RT, 1024, F32)
    xnT = sb(XNT, 32768, BF16).rearrange("p (k t) -> p k t", k=8)
    ocat = sb(OCAT, 32768, BF16).rearrange("p (k t) -> p k t", k=8)

    def A(eng, fn, reads=(), writes=(), dma=False, tag=None):
        return sch.add(eng, fn, reads, writes, dma, tag)

    def dma(q, out, in_, reads=(), writes=(), **kw):
        A(q, lambda e, o=out, i=in_, kw=kw: e.dma_start(out=o, in_=i, **kw), reads, writes, dma=True)

    def mm(out, lhsT, rhs, start, stop):
        A("pe", lambda e, o=out, l=lhsT, r=rhs, s0=start, s1=stop: e.matmul(o, l, r, start=s0, stop=s1),
          [lhsT, rhs], [out])

    def tr(out, in_, idn):
        A("pe", lambda e, o=out, i=in_, d=idn: e.transpose(o, i, d), [in_, idn], [out])

    def act(out, in_, func, bias=None, scale=None, accum=None):
        rd = [in_]
        kw = {}
        if bias is not None:
            kw["bias"] = bias
            if not isinstance(bias, float):
                rd.append(bias)
        if scale is not None:
            kw["scale"] = scale
            if not isinstance(scale, float):
                rd.append(scale)
        wr_ = [out]
        if accum is not None:
            kw["accum_out"] = accum
            wr_.append(accum)
        A("act", lambda e, o=out, i=in_, f=func, kw=kw: e.activation(o, i, f, **kw), rd, wr_)

    def ts(eng, out, in0, s1, s2, op0, op1=None, accum=None):
        rd = [in0]
        for s_ in (s1, s2):
            if s_ is not None and not isinstance(s_, (float, int)):
                rd.append(s_)
        wr_ = [out]
        kw = {}
        if accum is not None:
            kw["accum_out"] = accum
            wr_.append(accum)
        if op1 is None:
            A(eng, lambda e, o=out, i=in0, a=s1, p=op0, kw=kw: e.tensor_scalar(o, i, a, None, p, **kw), rd, wr_)
        else:
            A(eng, lambda e, o=out, i=in0, a=s1, b=s2, p=op0, q=op1, kw=kw: e.tensor_scalar(o, i, a, b, p, q, **kw), rd, wr_)

    def tt(eng, out, in0, in1, op):
        A(eng, lambda e, o=out, a=in0, b=in1, p=op: e.tensor_tensor(o, a, b, p), [in0, in1], [out])

    def stt(out, in0, scalar, in1, op0, op1, accum=None):
        rd = [in0, in1]
        if not isinstance(scalar, (float, int)):
            rd.append(scalar)
        wr_ = [out]
        kw = {}
        if accum is not None:
            kw["accum_out"] = accum
            wr_.append(accum)
        A("dve", lambda e, o=out, a=in0, s_=scalar, b=in1, p=op0, q=op1, kw=kw:
          e.scalar_tensor_tensor(o, a, s_, b, p, q, **kw), rd, wr_)

    def cp(eng, out, in_):
        if eng == "act":
            A(eng, lambda e, o=out, i=in_: e.copy(o, i), [in_], [out])
        else:
            A(eng, lambda e, o=out, i=in_: e.tensor_copy(o, i), [in_], [out])

    def memset(eng, out, val):
        A(eng, lambda e, o=out, v=val: e.memset(o, v), [], [out])

    def recip(out, in_):
        A("dve", lambda e, o=out, i=in_: e.reciprocal(o, i), [in_], [out])

    dma("sp", ident, ci_d, writes=[ident])
    dma("sp", maskfox, cmf_d, writes=[maskfox])
    dma("sp", maskhg, cmh_d, writes=[maskhg])
    dma("sp", cvecA[0:65, :], cva_d, writes=[cvecA[0:65, :]])
    dma("sp", cvecB, cvb_d, writes=[cvecB])
    dma("sp", sel[0:97, :, :], csel_d.rearrange("p (a b) -> p a b", a=16), writes=[sel[0:97, :, :]])
    dma("sp", gattn, gattn_d.partition_broadcast(128), writes=[gattn])
    dma("sp", gffn, gffn_d.partition_broadcast(128), writes=[gffn])
    dma("sp", gfin, gfin_d.partition_broadcast(128), writes=[gfin])
    dma("sp", brt, br_d.partition_broadcast(128), writes=[brt])
    nonc = dict(allow_slow_non_contiguous=True)
    dma("sp", l0, lbl_d[0:1, :].rearrange("o (h p) -> p (o h)", p=128), writes=[l0], **nonc)
    dma("sp", l1, lbl_d[1:2, :].rearrange("o (h p) -> p (o h)", p=128), writes=[l1], **nonc)
    dma("sp", hgg, hgn_d.rearrange("o p -> p o"), writes=[hgg], **nonc)
    dma("sp", foxg[0:64, :], fxn_d.rearrange("o p -> p o"), writes=[foxg[0:64, :]], **nonc)
    dma("sp", foxg[64:128, :], fxn_d.rearrange("o p -> p o"), writes=[foxg[64:128, :]], **nonc)
    memset("dve", negb, 0.0)
    for g in range(3):
        dma("sp", negb[32 * g:32 * g + 8, :], fb_d.rearrange("o p -> p o"), writes=[negb[32 * g:32 * g + 8, :]], **nonc)
    dma("pool", wout, wout_d.rearrange("(k p) c -> p k c", p=128), writes=[wout])
    dma("pool", wr, wr_d.rearrange("(k p) c -> p k c", p=128), writes=[wr])
    memset("dve", ones, 1.0)
    memset("dve", reset, 1.0)
    memset("dve", reset.rearrange("p (c j) -> p c j", j=64)[:, :, 0:1], 0.0)
    memset("dve", cmathg, 1.0 / 128)
    tt("dve", ltmp, l1, l0, ALU.subtract)
    act(ltmp, ltmp, AF.Exp)
    ts("dve", ltmp, ltmp, 1.0, None, ALU.add)
    recip(lbc, ltmp)
    ts("dve", oml, lbc, -1.0, 1.0, ALU.mult, ALU.add)

    def rms_rstd(src, junk, col):
        ssq = stat[:, col:col + 1]
        var = stat[:, col + 1:col + 2]
        lnv = stat[:, col + 2:col + 3]
        rstd = stat[:, col + 3:col + 4]
        act(junk, src, AF.Square, accum=ssq)
        ts("dve", var, ssq, 1.0 / D, EPS, ALU.mult, ALU.add)
        act(lnv, var, AF.Ln)
        act(rstd, lnv, AF.Exp, scale=-0.5)
        return rstd

    pbank = [0]

    def next_bank(lo, hi):
        b = lo + (pbank[0] % (hi - lo))
        pbank[0] += 1
        return b

    for sq_ in range(nseq):
        xt_s = [sb(W + 77056 + 4096 * i, 4096, F32) for i in range(2)]
        junk = sb(W + 85248, 2048, BF16)
        xs_s = [sb(W + 87296 + 2048 * i, 2048, BF16) for i in range(2)]
        wfox = sb(W, 24704, BF16).rearrange("p (k c) -> p k c", k=8)
        dma("pool", wfox, win_d.rearrange("(k p) c -> p k c", p=128)[:, :, 2048:INC], writes=[wfox])
        for i in range(NT):
            xt = xt_s[i % 2]
            xs = xs_s[i % 2]
            dma("sp", xt, x_d[sq_, i * 128:(i + 1) * 128, :], writes=[xt])
            rstd = rms_rstd(xt, junk, 4 * (i % 8))
            stt(xs, xt, rstd, gattn, ALU.mult, ALU.mult)
            b = next_bank(0, 2)
            pb = bankbf(b).rearrange("p (k t) -> p k t", k=8)
            for kc in range(8):
                tr(pb[:, kc, :], xs[:, kc * 128:(kc + 1) * 128], ident)
            cp("act" if i % 2 else "dve", xnT[:, :, i * 128:(i + 1) * 128], pb)
        if stop_after == "prep":
            break

        vaug = sb(W + 24704, 24704, BF16).rearrange("p (t c) -> p t c", t=NT)
        qa_s = [sb(W + 49408 + 4096 * i, 4096, BF16) for i in range(2)]
        ka_s = [sb(W + 57600 + 4096 * i, 4096, BF16) for i in range(2)]
        parts = sb(W + 65792, 4096, BF16)
        pT_s = [sb(W + 69888 + 1024 * i, 1024, BF16) for i in range(2)]
        sqf = sb(W + 71936, 1024, BF16)
        lnr = sb(W + 72960, 2048, F32)
        rr = sb(W + 75008, 2048, F32)
        fr0 = sb(W + 77056, 8192, F32)
        fr1 = sb(W + 85248, 8192, F32)

        def vcol(h):
            return (h // 2) * 193 + (0 if h % 2 == 0 else 65)

        wf3 = sb(W + 93440, 1152, BF16).rearrange("p (k c) -> p k c", k=8)
        hiT = sb(W + 69888, 4096, BF16)
        memset("dve", parts, 1.0)
        memset("dve", wf3, 0.0)
        for g in range(3):
            cp("dve", wf3[:, :, 32 * g:32 * g + 8], wfox[:, :, 1536:1544])
        for tb in range(4):
            b = next_bank(0, 2)
            pf = banks[b][0:72, :]
            for kc in range(8):
                mm(pf, wf3[:, kc, :], xnT[:, kc, tb * 512:(tb + 1) * 512], kc == 0, kc == 7)
            act(fr0[0:72, tb * 512:(tb + 1) * 512], pf, AF.Identity, bias=negb[0:72, :])
        f0 = fr0[0:72, :]
        f1 = fr1[0:72, :]
        h72 = hiT[0:72, :]
        ts("dve", f1, f0, -80.0, None, ALU.max)
        act(f1, f1, AF.Exp, scale=-1.0)
        act(f1, f1, AF.Ln, bias=1.0)
        ts("dve", f0, f1, -1.0, None, ALU.mult)
        A("dve", lambda e, o=f1, a=ones[0:72, :], b_=f0: e.tensor_tensor_scan(o, a, b_, 0.0, ALU.mult, ALU.add),
          [ones[0:72, :], f0], [f1])
        cp("dve", h72, f1)
        cp("dve", parts[0:8, :], hiT[0:8, :])
        tt("dve", f0, f1, h72, ALU.subtract)
        cp("dve", h72, f0)
        cp("dve", parts[32:40, :], hiT[32:40, :])
        tt("dve", f1, f0, h72, ALU.subtract)
        cp("dve", parts[64:72, :], fr1[64:72, :])

        memset("dve", vaug, 0.0)
        for h in range(8):
            c1 = vcol(h) + (64 if h % 2 == 0 else 0)
            memset("dve", vaug[:, :, c1:c1 + 1], 1.0)
        for i in range(NT):
            b = next_bank(0, 2)
            pv = banks[b]
            for kc in range(8):
                mm(pv[:, :], xnT[:, kc, i * 128:(i + 1) * 128], wfox[:, kc, 1024:1536], kc == 0, kc == 7)
            pv4 = pv[:, :].rearrange("p (h two d) -> p h two d", two=2, d=64)
            vg = vaug[:, i, :].rearrange("p (h c) -> p h c", c=193)
            cp("dve", vg[:, :, 0:64], pv4[:, :, 0, :])
            cp("act", vg[:, :, 129:193], pv4[:, :, 1, :])

        for h in range(8):
            qa = qa_s[h % 2]
            ka = ka_s[h % 2]
            odd = h % 2
            for tb in range(4):
                cs = slice(tb * 512, (tb + 1) * 512)
                for which, dst, scl in ((0, qa, 0.125), (1, ka, 1.0)):
                    b = next_bank(0, 2)
                    pq = banks[b]
                    mm(pq[0:70, :], sel[0:97, which * 8 + h, :], parts[0:97, cs], True, False)
                    wc = which * 512 + h * 64
                    for kc in range(8):
                        mm(pq[0:64, :], wfox[:, kc, wc:wc + 64], xnT[:, kc, cs], False, kc == 7)
                    if which == 0:
                        act(dst[0:70, cs], pq[0:70, :], AF.Copy, scale=scl)
                    else:
                        cp("dve", dst[0:70, cs], pq[0:70, :])
            vc = vcol(h)
            vw = 65 if not odd else 128
            for tb in range(4):
                t0 = tb * 512
                ob = 5 + (h * 4 + tb) % 2
                n_s = 4 * (tb + 1)
                orow = slice(0, 65) if not odd else slice(0, 128)
                for j in range(n_s):
                    s0 = j * 128
                    c0 = max(0, s0 - t0)
                    diag = s0 >= t0
                    lb_ = next_bank(2, 5)
                    lg = banks[lb_]
                    mm(lg[:, c0:512], ka[0:70, s0:s0 + 128], qa[0:70, t0 + c0:t0 + 512], True, not diag)
                    if diag:
                        mm(lg[:, c0:c0 + 128], ident, maskfox, False, True)
                    pT = pT_s[j % 2]
                    act(pT[:, c0:512], lg[:, c0:512], AF.Exp)
                    mm(banks[ob][orow, c0:512], vaug[:, j, vc:vc + vw], pT[:, c0:512], j == 0, j == n_s - 1)
                po = banks[ob]
                if not odd:
                    act(sqf[0:65, :], po[0:65, :], AF.Square)
                    mm(banks[7][0:64, :], cvecA[0:65, :], sqf[0:65, :], True, True)
                    prow = slice(0, 64)
                else:
                    act(sqf[:, :], po[:, :], AF.Square)
                    mm(banks[7][:, :], cvecB[:, :], sqf[:, :], True, True)
                    prow = slice(64, 128)
                act(lnr[prow, :], banks[7][prow, :], AF.Ln)
                act(rr[prow, :], lnr[prow, :], AF.Exp, scale=-0.5)
                stt(ocat[prow, 4 + h // 2, t0:t0 + 512], po[prow, :], foxg[prow, :], rr[prow, :], ALU.mult, ALU.mult)
        if stop_after == "fox":
            break

        whg = sb(W, 32768, BF16).rearrange("p (k c) -> p k c", k=8)
        dma("pool", whg, win_d.rearrange("(k p) c -> p k c", p=128)[:, :, 0:2048], writes=[whg])
        T0 = sb(W + 32768, 8192, F32)
        T1 = sb(W + 40960, 8192, F32)
        T2 = sb(W + 49152, 8192, F32)
        T3 = sb(W + 57344, 8192, F32)
        qtl = sb(W + 65536, 4096, BF16)
        ktl = sb(W + 69632, 4096, BF16)
        ktok = sb(W + 73728, 4096, BF16).rearrange("p (t k) -> p t k", t=NT)
        vtok = sb(W + 77824, 4096, BF16).rearrange("p (t k) -> p t k", t=NT)
        sgT = sb(W + 81920, 4096, BF16)
        Sbf = sb(W + 86016, 8192, BF16).rearrange("p (c v) -> p c v", c=32)
        U_s = [sb(W + 94208 + 512 * i, 512, F32) for i in range(2)]
        scm_s = [sb(W + 95232 + 256 * i, 256, BF16) for i in range(2)]
        lnr2 = sb(W + 40960, 2048, F32)
        rr2 = sb(W + 40960 + 2048, 2048, F32)
        t1b = sb(W + 40960 + 4096, 2048, F32)
        sqh = sb(W + 40960 + 6144, 1024, BF16)
        for h in range(4):
            hs = slice(h * 128, (h + 1) * 128)
            for tb in range(4):
                cs = slice(tb * 512, (tb + 1) * 512)
                for (coff, dst, fn_) in ((0, T0, AF.Silu), (512, T1, AF.Sigmoid), (1536, sgT, AF.Silu)):
                    b = next_bank(0, 2)
                    pp = banks[b]
                    for kc in range(8):
                        mm(pp[:, :], whg[:, kc, coff + h * 128:coff + (h + 1) * 128], xnT[:, kc, cs], kc == 0, kc == 7)
                    act(dst[:, cs], pp[:, :], fn_)
            for i4 in range(4):
                b = next_bank(0, 2)
                pp = banks[b]
                for ii in range(4):
                    i = i4 * 4 + ii
                    for kc in range(8):
                        mm(pp[:, ii * 128:(ii + 1) * 128], xnT[:, kc, i * 128:(i + 1) * 128],
                           whg[:, kc, 1024 + h * 128:1024 + (h + 1) * 128], kc == 0, kc == 7)
                cp("dve", vtok[:, i4 * 4:(i4 + 1) * 4, :], pp[:, :].rearrange("p (t k) -> p t k", t=4))
            ts("dve", T1, T1, oml[:, h:h + 1], lbc[:, h:h + 1], ALU.mult, ALU.add)
            act(T2, T1, AF.Ln)
            ts("dve", T1, T1, -1.0, 1.0, ALU.mult, ALU.add)
            A("dve", lambda e, o=T3, a=reset, b_=T2: e.tensor_tensor_scan(o, a, b_, 0.0, ALU.mult, ALU.add),
              [reset, T2], [T3])
            act(T2, T3, AF.Exp)
            tt("dve", qtl, T0, T2, ALU.mult)
            act(T0, T3, AF.Exp, scale=-1.0)
            tt("dve", ktl, T1, T0, ALU.mult)
            for i8 in range(2):
                b = next_bank(0, 2)
                pb = bankbf(b).rearrange("p (t k) -> p t k", t=8)
                for ii in range(8):
                    i = i8 * 8 + ii
                    tr(pb[:, ii, :], ktl[:, i * 128:(i + 1) * 128], ident)
                cp("act", ktok[:, i8 * 8:(i8 + 1) * 8, :], pb)
            for c in range(32):
                i, par = c // 2, c % 2
                if c % 4 == 0:
                    sb_ = next_bank(2, 5)
                dS = banks[sb_][:, (c % 4) * 128:(c % 4 + 1) * 128]
                ps_ = slice(par * 64, par * 64 + 64)
                mm(dS, ktok[ps_, i, :], vtok[ps_, i, :], True, True)
                Uc = U_s[c % 2]
                Up = U_s[(c + 1) % 2]
                if c == 0:
                    memset("pool", Sbf[:, 0, :], 0.0)
                    cp("dve", Uc, dS)
                else:
                    Dp = T2[:, (c - 1) * 64 + 63:(c - 1) * 64 + 64]
                    ts("pool", Sbf[:, c, :], Up, Dp, None, ALU.mult)
                    stt(Uc, Up, Dp, dS, ALU.mult, ALU.add)
            for tb in range(4):
                ob = 5 + (h * 4 + tb) % 2
                po = banks[ob]
                for ii in range(4):
                    i = tb * 4 + ii
                    cs = slice(i * 128, (i + 1) * 128)
                    lb_ = next_bank(2, 5)
                    sc = banks[lb_][:, 0:128]
                    mm(sc, ktl[:, cs], qtl[:, cs], True, True)
                    scm = scm_s[i % 2]
                    tt("dve", scm, sc, maskhg, ALU.mult)
                    oo = po[:, ii * 128:(ii + 1) * 128]
                    mm(oo, vtok[:, i, :], scm, True, False)
                    mm(oo[:, 0:64], Sbf[:, 2 * i, :], qtl[:, i * 128:i * 128 + 64], False, False)
                    mm(oo[:, 64:128], Sbf[:, 2 * i + 1, :], qtl[:, i * 128 + 64:i * 128 + 128], False, True)
                act(sqh, po[:, :], AF.Square)
                mm(banks[7][:, :], cmathg, sqh, True, True)
                act(lnr2, banks[7][:, :], AF.Ln, bias=EPS)
                act(rr2, lnr2, AF.Exp, scale=-0.5)
                stt(t1b, po[:, :], hgg, rr2, ALU.mult, ALU.mult)
                tt("pool", ocat[:, h, tb * 512:(tb + 1) * 512], t1b, sgT[:, tb * 512:(tb + 1) * 512], ALU.mult)
        if stop_after == "hg":
            break

        hbuf = sb(W, 65536, F32).rearrange("p (t c) -> p t c", t=NT)
        xt2_s = [sb(TT + 4096 * i, 4096, F32) for i in range(2)]
        junk2 = sb(TT + 8192, 2048, BF16)
        hn_s = [sb(TT + 10240 + 2048 * i, 2048, BF16) for i in range(2)]
        hnT = xnT
        for i in range(NT):
            xt = xt2_s[i % 2]
            dma("sp", xt, x_d[sq_, i * 128:(i + 1) * 128, :], writes=[xt])
            for half in range(2):
                b = next_bank(0, 2)
                ph = banks[b]
                for fc in range(8):
                    mm(ph[:, :], ocat[:, fc, i * 128:(i + 1) * 128], wout[:, fc, half * 512:(half + 1) * 512], fc == 0, fc == 7)
                tt("dve", hbuf[:, i, half * 512:(half + 1) * 512], ph[:, :], xt[:, half * 512:(half + 1) * 512], ALU.add)
            rstd = rms_rstd(hbuf[:, i, :], junk2, 32 + 4 * (i % 8))
            hn = hn_s[i % 2]
            stt(hn, hbuf[:, i, :], rstd, gffn, ALU.mult, ALU.mult)
            b = next_bank(2, 4)
            pb = bankbf(b).rearrange("p (k t) -> p k t", k=8)
            for kc in range(8):
                tr(pb[:, kc, :], hn[:, kc * 128:(kc + 1) * 128], ident)
            cp("act", hnT[:, :, i * 128:(i + 1) * 128], pb)
            b = next_bank(4, 6)
            pr = banks[b][:, 0:20]
            for kc in range(8):
                mm(pr, hnT[:, kc, i * 128:(i + 1) * 128], wr[:, kc, :], kc == 0, kc == 7)
            ro = (i % 2) * 128
            lgt = rt[:, ro:ro + 20]
            gmax = rt[:, ro + 20:ro + 21]
            ngmax = rt[:, ro + 21:ro + 22]
            gm = rt[:, ro + 22:ro + 26]
            eg = rt[:, ro + 26:ro + 30]
            sumg = rt[:, ro + 30:ro + 31]
            pgs = rt[:, ro + 31:ro + 32]
            pen = rt[:, ro + 32:ro + 48]
            elm = rt[:, ro + 48:ro + 64]
            top8 = rt[:, ro + 64:ro + 72]
            nm1 = rt[:, ro + 72:ro + 73]
            selm = rt[:, ro + 73:ro + 89]
            den2 = rt[:, ro + 89:ro + 90]
            wsc = rt[:, ro + 90:ro + 91]
            ex = rt[:, ro + 91:ro + 107]
            tt("dve", lgt, pr, brt, ALU.add)
            A("dve", lambda e, o=gmax, i_=lgt[:, 0:4]: e.reduce_max(o, i_, AX.X), [lgt[:, 0:4]], [gmax])
            ts("dve", gm, lgt[:, 0:4], gmax, None, ALU.is_equal)
            ts("dve", ngmax, gmax, -1.0, None, ALU.mult)
            act(eg, lgt[:, 0:4], AF.Exp, bias=ngmax, accum=sumg)
            recip(pgs, sumg)
            gm_b = bass.AP(gm.tensor, gm.offset, [list(gm.ap[0]), [1, 4], [0, 4]])
            pen3 = pen.rearrange("p (g j) -> p g j", j=4)
            ts("dve", pen3, gm_b, -1.0, 1e30, ALU.add, ALU.mult)
            tt("dve", elm, lgt[:, 4:20], pen, ALU.add)
            A("dve", lambda e, o=top8, i_=elm: e.max(o, i_), [elm], [top8])
            ts("dve", selm, elm, top8[:, 1:2], None, ALU.is_ge)
            ts("dve", nm1, top8[:, 0:1], -1.0, None, ALU.mult)
            act(ex, elm, AF.Exp, bias=nm1)
            stt(ex, ex, 1.0, selm, ALU.mult, ALU.mult, accum=den2)
            recip(den2, den2)
            tt("dve", wsc, den2, pgs, ALU.mult)
            ts("dve", gates[:, i, :], ex, wsc, None, ALU.mult)
        if stop_after == "wout":
            if dbg:
                for i in range(NT):
                    dma("sp", dbg_d[i * 128:(i + 1) * 128, :], hbuf[:, i, :], reads=[hbuf[:, i, :]])
            break

        wslot = [OCAT, TT]
        hact_s = [sb(OCAT + 24576 + 4096 * i, 4096, BF16).rearrange("p (c t) -> p c t", c=4) for i in range(2)]
        sgt_s = [sb(TT + 24576 + 2048 * i, 2048, F32) for i in range(2)]
        blk = 0
        for e in range(NE):
            wo = wslot[e % 2]
            wg = sb(wo, 8192, BF16).rearrange("p (k c) -> p k c", k=8)
            wu = sb(wo + 8192, 8192, BF16).rearrange("p (k c) -> p k c", k=8)
            wd = sb(wo + 16384, 8192, BF16).rearrange("p (k c) -> p k c", k=4)
            dma("pool", wg, wg_d[e].rearrange("(k p) c -> p k c", p=128), writes=[wg])
            dma("pool", wu, wu_d[e].rearrange("(k p) c -> p k c", p=128), writes=[wu])
            dma("pool", wd, wd_d[e].rearrange("(k p) c -> p k c", p=128), writes=[wd])
            for tb in range(4):
                cs = slice(tb * 512, (tb + 1) * 512)
                hact = hact_s[blk % 2]
                blk += 1
                for hc in range(4):
                    gb = next_bank(0, 2)
                    ub = 2 + next_bank(0, 2) % 2
                    for kc in range(8):
                        mm(banks[gb][:, :], wg[:, kc, hc * 128:(hc + 1) * 128], hnT[:, kc, cs], kc == 0, kc == 7)
                    for kc in range(8):
                        mm(banks[ub][:, :], wu[:, kc, hc * 128:(hc + 1) * 128], hnT[:, kc, cs], kc == 0, kc == 7)
                    sgt = sgt_s[hc % 2]
                    act(sgt, banks[gb][:, :], AF.Silu)
                    tt("dve", hact[:, hc, :], sgt, banks[ub][:, :], ALU.mult)
                for ii in range(4):
                    i = tb * 4 + ii
                    for half in range(2):
                        yb = next_bank(4, 8)
                        for hc in range(4):
                            mm(banks[yb][:, :], hact[:, hc, ii * 128:(ii + 1) * 128], wd[:, hc, half * 512:(half + 1) * 512], hc == 0, hc == 3)
                        hh = hbuf[:, i, half * 512:(half + 1) * 512]
                        stt(hh, banks[yb][:, :], gates[:, i, e:e + 1], hh, ALU.mult, ALU.add)
        for i in range(NT):
            rstd = rms_rstd(hbuf[:, i, :], junk2, 64 + 4 * (i % 8))
            stt(hbuf[:, i, :], hbuf[:, i, :], rstd, gfin, ALU.mult, ALU.mult)
            dma("sp", out_d[sq_, i * 128:(i + 1) * 128, :], hbuf[:, i, :], reads=[hbuf[:, i, :]])

    sch.prepare()
    sems = {k: es.enter_context(nc.semaphore(f"s_{k}")) for k in ("pe", "act", "dve", "pool")}
    dsems = {}
    for q in ("sp", "pool", "act"):
        for k in range(sch.n_dma_sems):
            dsems[(q, k)] = es.enter_context(nc.semaphore(f"d_{q}{k}"))
    with nc.Block() as block:
        @block.tensor
        def _(eng):
            sch.emit_engine("pe", eng, sems, dsems)

        @block.scalar
        def _(eng):
            sch.emit_engine("act", eng, sems, dsems)

        @block.vector
        def _(eng):
            sch.emit_engine("dve", eng, sems, dsems)

        @block.gpsimd
        def _(eng):
            sch.emit_engine("pool", eng, sems, dsems)

        @block.sync
        def _(eng):
            sch.emit_engine("sp", eng, sems, dsems)
    es.close()
    return nc, sch


_CACHE = {}


def _get_nc(nseq):
    if nseq not in _CACHE:
        _CACHE[nseq] = build(nseq, Sched, F32, BF16, U8)[0]
    return _CACHE[nseq]


def kernel(x, attn_norm, w_in, hg_lb_logits, hg_norm, fox_f_bias, fox_norm, w_out, ffn_norm,
           w_group, b_group, w_expert, b_expert, w_gate, w_up, w_down, final_norm):
    f = lambda a: np.ascontiguousarray(np.asarray(a, dtype=np.float32))
    x = f(x)
    n_cores = 8
    B = x.shape[0]
    nseq = B // n_cores
    shared = {
        "w_in": f(w_in)[0], "w_out": f(w_out)[0], "w_gate": f(w_gate)[0], "w_up": f(w_up)[0],
        "w_down": f(w_down)[0],
        "w_router": np.ascontiguousarray(np.concatenate([f(w_group)[0], f(w_expert)[0]], axis=1)),
        "b_router": np.ascontiguousarray(np.concatenate([f(b_group)[0], f(b_expert)[0]])[None, :]),
        "attn_norm": f(attn_norm), "ffn_norm": f(ffn_norm), "final_norm": f(final_norm).reshape(1, -1),
        "hg_lb_logits": f(hg_lb_logits), "hg_norm": f(hg_norm), "fox_f_bias": f(fox_f_bias),
        "fox_norm": f(fox_norm),
    }
    shared.update(make_consts())
    in_maps = []
    for c in range(n_cores):
        m = dict(shared)
        m["x"] = np.ascontiguousarray(x[c * nseq:(c + 1) * nseq])
        in_maps.append(m)
    nc = _get_nc(nseq)
    res = run_bass_kernel_spmd(nc, in_maps, core_ids=list(range(n_cores)))
    return np.concatenate([r["out"] for r in res.results], axis=0).astype(np.float32)
```

```python
import contextlib
import numpy as np
import ml_dtypes
import concourse.bass as bass
import concourse.mybir as mybir

F32 = mybir.dt.float32
BF16 = mybir.dt.bfloat16
U8 = mybir.dt.uint8
_ES = {F32: 4, BF16: 2, U8: 1, mybir.dt.int32: 4, mybir.dt.uint32: 4, mybir.dt.uint16: 2}


class Op:
    __slots__ = ("eng", "fn", "deps", "cdeps", "seq", "ms", "needed", "dma", "dsem", "dval", "dprev", "tag")

    def __init__(self, eng, fn, dma, tag):
        self.eng = eng
        self.fn = fn
        self.deps = set()
        self.cdeps = {}
        self.seq = 0
        self.ms = None
        self.needed = False
        self.dma = dma
        self.dsem = None
        self.dval = None
        self.dprev = 0
        self.tag = tag


class Sched:
    def __init__(self, n_dma_sems=8, same_engine_sync=True):
        self.ops = []
        self.recs = {}
        self.n_dma_sems = n_dma_sems
        self.same_engine_sync = same_engine_sync

    def _regions(self, ap):
        t = ap.tensor
        cls = type(t).__name__
        if cls.startswith("DRam"):
            return None
        psum = cls.startswith("PSum") or cls.startswith("Psum")
        name = ap.name
        if psum:
            return name, True, 0, 128, [(0, 1 << 20)]
        es = _ES[ap.dtype]
        pat = ap.ap
        off = int(ap.offset)
        pstride, pcnt = pat[0]
        p0 = off // pstride
        c0 = off % pstride
        free = list(pat[1:])
        if not free:
            return name, False, p0, p0 + pcnt, [(c0 * es, (c0 + 1) * es)]
        ls, ln = free[-1]
        run = (ln - 1) * abs(ls) + 1
        outer = free[:-1]
        nout = 1
        for s, n in outer:
            nout *= n
        ivs = []
        if nout <= 64:
            idx = [0] * len(outer)
            while True:
                st = c0 + sum(i * s for i, (s, n) in zip(idx, outer))
                ivs.append((st * es, (st + run) * es))
                k = len(outer) - 1
                while k >= 0:
                    idx[k] += 1
                    if idx[k] < outer[k][1]:
                        break
                    idx[k] = 0
                    k -= 1
                if k < 0:
                    break
        else:
            hi = c0 + sum((n - 1) * abs(s) for s, n in outer) + run
            ivs.append((c0 * es, hi * es))
        ivs.sort()
        out = [ivs[0]]
        for a, b in ivs[1:]:
            if a <= out[-1][1]:
                out[-1] = (out[-1][0], max(out[-1][1], b))
            else:
                out.append((a, b))
        return name, False, p0, p0 + pcnt, out

    def _access(self, op, ap, is_write):
        r = self._regions(ap)
        if r is None:
            return
        name, psum, p0, p1, ivs = r
        tab = self.recs.setdefault(name, {})
        w = is_write or psum
        SH = 11
        for (b0, b1) in ivs:
            newrec = (p0, p1, b0, b1, op, w)
            for bk in range(b0 >> SH, ((b1 - 1) >> SH) + 1):
                lst = tab.get(bk)
                if lst is None:
                    tab[bk] = [newrec]
                    continue
                keep = []
                for rec in lst:
                    rp0, rp1, rb0, rb1, rop, rw = rec
                    ov = rp0 < p1 and p0 < rp1 and rb0 < b1 and b0 < rb1
                    if ov and (rw or w) and rop is not op:
                        if rop.dma:
                            op.deps.add(rop)
                        else:
                            c = op.cdeps.get(rop.eng)
                            if c is None or c.seq < rop.seq:
                                op.cdeps[rop.eng] = rop
                    if ov and w and rp0 >= p0 and rp1 <= p1 and rb0 >= b0 and rb1 <= b1:
                        continue
                    if (not w) and (not rw) and rop.eng == op.eng and not rop.dma and not op.dma \
                            and rp0 == p0 and rp1 == p1 and rb0 == b0 and rb1 == b1:
                        continue
                    keep.append(rec)
                keep.append(newrec)
                tab[bk] = keep

    def add(self, eng, fn, reads=(), writes=(), dma=False, tag=None):
        op = Op(eng, fn, dma, tag)
        op.seq = len(self.ops)
        for ap in reads:
            self._access(op, ap, False)
        for ap in writes:
            self._access(op, ap, True)
        op.deps.update(op.cdeps.values())
        op.cdeps = None
        self.ops.append(op)
        return op

    def prepare(self):
        for op in self.ops:
            for d in op.deps:
                if d.dma:
                    continue
                if d.eng == op.eng and (d.eng == "pe" or not self.same_engine_sync) and not op.dma:
                    continue
                d.needed = True
        cnt = {}
        dcnt = {}
        dslot = {}
        for op in self.ops:
            if op.dma:
                k = dslot.get(op.eng, 0)
                dslot[op.eng] = k + 1
                slot = (op.eng, k % self.n_dma_sems)
                prev = dcnt.get(slot, 0)
                op.dsem = slot
                op.dprev = prev
                op.dval = prev + 16
                dcnt[slot] = op.dval
            elif op.needed:
                cnt[op.eng] = cnt.get(op.eng, 0) + 1
                op.ms = cnt[op.eng]
        self.final_dma = dcnt
        self.ms_total = cnt

    def emit_engine(self, eng_name, eng, sems, dsems):
        waited = {}

        def need(sem_key, sem, val):
            if val <= 0:
                return
            if waited.get(sem_key, 0) >= val:
                return
            waited[sem_key] = val
            eng.wait_ge(sem, val)

        n = 0
        for op in self.ops:
            if op.eng != eng_name:
                continue
            for d in op.deps:
                if d.dma:
                    need(d.dsem, dsems[d.dsem], d.dval)
                else:
                    if d.eng == eng_name and not op.dma and (eng_name == "pe" or not self.same_engine_sync):
                        continue
                    need(d.eng, sems[d.eng], d.ms)
            if op.dma:
                need(op.dsem, dsems[op.dsem], op.dprev)
            ins = op.fn(eng)
            if op.dma:
                ins.then_inc(dsems[op.dsem], 16)
            elif op.needed:
                ins.then_inc(sems[eng_name], 1)
            n += 1
        for slot, val in self.final_dma.items():
            if slot[0] == eng_name:
                need(slot, dsems[slot], val)
        return n


from concourse.bass_utils import run_bass_kernel_spmd

AF = mybir.ActivationFunctionType
ALU = mybir.AluOpType
AX = mybir.AxisListType
I32 = mybir.dt.int32

D = 1024
S = 2048
NT = S // 128
HGW = 512
FOXW = 512
INC = 3592
NE = 16
EH = 512
EPS = 1e-6

ARENA = 208000
C_IDENT = 0
C_MASKFOX = 256
C_MASKHG = 512
C_CMATHG = 768
C_CVECA = 1024
C_CVECB = 1152
C_SEL = 1408
C_ONES = 3648
C_RESET = 7744
C_GATTN = 11840
C_GFFN = 15936
C_GFIN = 20032
C_SMALL = 24128
C_WOUT = 24640
C_WR = 41024
C_STAT = 41344
C_GATES = 42368
C_RT = 43392
XNT = 44544
OCAT = XNT + 32768
W = OCAT + 32768
WSIZE = ARENA - W
TT = W + 65536


def make_consts():
    bf = ml_dtypes.bfloat16
    ident = np.eye(128, dtype=np.float32).astype(bf)
    s_ = np.arange(128)[:, None]
    t_ = np.arange(128)[None, :]
    maskfox = np.where(t_ >= s_, 0.0, -30000.0).astype(np.float32).astype(bf)
    maskhg = ((t_ >= s_) & ((t_ // 64) == (s_ // 64))).astype(np.float32).astype(bf)
    cvecA = np.zeros((65, 64), np.float32)
    cvecA[0:64, :] = 1.0 / 64
    cvecA[64, :] = EPS
    cvecB = np.zeros((128, 128), np.float32)
    cvecB[0, 64:128] = EPS
    cvecB[64:128, 64:128] = 1.0 / 64
    sel = np.zeros((97, 16, 70), np.float32)
    for h in range(8):
        sel[h, h, 64] = 8.0
        sel[32 + h, h, 65] = 8.0
        sel[64 + h, h, 66] = 8.0
        sel[96, h, 67:70] = 8.0
        sel[96, 8 + h, 64:67] = 1.0
        sel[h, 8 + h, 67] = -1.0
        sel[32 + h, 8 + h, 68] = -1.0
        sel[64 + h, 8 + h, 69] = -1.0
    return {
        "c_ident": ident,
        "c_maskfox": maskfox,
        "c_maskhg": maskhg,
        "c_cveca": cvecA.astype(bf),
        "c_cvecb": cvecB.astype(bf),
        "c_sel": sel.reshape(97, 16 * 70).astype(bf),
    }


def build(nseq, Sched, F32, BF16, U8, stop_after=None, dbg=False):
    nc = bass.Bass("TRN2", target_bir_lowering=False)
    dt_ = nc.dram_tensor
    x_d = dt_("x", [nseq, S, D], F32, kind="ExternalInput").ap()
    win_d = dt_("w_in", [D, INC], F32, kind="ExternalInput").ap()
    wout_d = dt_("w_out", [D, D], F32, kind="ExternalInput").ap()
    wg_d = dt_("w_gate", [NE, D, EH], F32, kind="ExternalInput").ap()
    wu_d = dt_("w_up", [NE, D, EH], F32, kind="ExternalInput").ap()
    wd_d = dt_("w_down", [NE, EH, D], F32, kind="ExternalInput").ap()
    wr_d = dt_("w_router", [D, 20], F32, kind="ExternalInput").ap()
    br_d = dt_("b_router", [1, 20], F32, kind="ExternalInput").ap()
    gattn_d = dt_("attn_norm", [1, D], F32, kind="ExternalInput").ap()
    gffn_d = dt_("ffn_norm", [1, D], F32, kind="ExternalInput").ap()
    gfin_d = dt_("final_norm", [1, D], F32, kind="ExternalInput").ap()
    lbl_d = dt_("hg_lb_logits", [2, HGW], F32, kind="ExternalInput").ap()
    hgn_d = dt_("hg_norm", [1, 128], F32, kind="ExternalInput").ap()
    fb_d = dt_("fox_f_bias", [1, 8], F32, kind="ExternalInput").ap()
    fxn_d = dt_("fox_norm", [1, 64], F32, kind="ExternalInput").ap()
    ci_d = dt_("c_ident", [128, 128], BF16, kind="ExternalInput").ap()
    cmf_d = dt_("c_maskfox", [128, 128], BF16, kind="ExternalInput").ap()
    cmh_d = dt_("c_maskhg", [128, 128], BF16, kind="ExternalInput").ap()
    cva_d = dt_("c_cveca", [65, 64], BF16, kind="ExternalInput").ap()
    cvb_d = dt_("c_cvecb", [128, 128], BF16, kind="ExternalInput").ap()
    csel_d = dt_("c_sel", [97, 16 * 70], BF16, kind="ExternalInput").ap()
    out_d = dt_("out", [nseq, S, D], F32, kind="ExternalOutput").ap()
    dbg_d = None
    if dbg:
        dbg_d = dt_("dbg", [S, D], F32, kind="ExternalOutput").ap()

    sch = Sched()
    es = contextlib.ExitStack()
    arena = es.enter_context(nc.sbuf_tensor("arena", [128, ARENA], U8))
    banks = [es.enter_context(nc.psum_tensor(f"bank{i}", [128, 512], F32)) for i in range(8)]

    def sb(off, nbytes, dt):
        return arena[:, off:off + nbytes].bitcast(dt)

    def bankbf(i):
        return banks[i][:, :].bitcast(BF16)

    ident = sb(C_IDENT, 256, BF16)
    maskfox = sb(C_MASKFOX, 256, BF16)
    maskhg = sb(C_MASKHG, 256, BF16)
    cmathg = sb(C_CMATHG, 256, BF16)
    cvecA = sb(C_CVECA, 128, BF16)
    cvecB = sb(C_CVECB, 256, BF16)
    sel = sb(C_SEL, 2240, BF16).rearrange("p (a b) -> p a b", a=16)
    ones = sb(C_ONES, 4096, BF16)
    reset = sb(C_RESET, 4096, BF16)
    gattn = sb(C_GATTN, 4096, F32)
    gffn = sb(C_GFFN, 4096, F32)
    gfin = sb(C_GFIN, 4096, F32)
    small = sb(C_SMALL, 512, F32)
    lbc = small[:, 0:4]
    oml = small[:, 4:8]
    hgg = small[:, 8:9]
    foxg = small[:, 9:10]
    negb = small[:, 10:11]
    l0 = small[:, 11:15]
    l1 = small[:, 15:19]
    ltmp = small[:, 19:23]
    brt = small[:, 24:44]
    wout = sb(C_WOUT, 16384, BF16).rearrange("p (k c) -> p k c", k=8)
    wr = sb(C_WR, 320, BF16).rearrange("p (k c) -> p k c", k=8)
    stat = sb(C_STAT, 1024, F32)
    gates = sb(C_GATES, 1024, F32).rearrange("p (t e) -> p t e", e=16)
    rt = sb(C_RT, 1024, F32)
    xnT = sb(XNT, 32768, BF16).rearrange("p (k t) -> p k t", k=8)
    ocat = sb(OCAT, 32768, BF16).rearrange("p (k t) -> p k t", k=8)

    def A(eng, fn, reads=(), writes=(), dma=False, tag=None):
        return sch.add(eng, fn, reads, writes, dma, tag)

    def dma(q, out, in_, reads=(), writes=(), **kw):
        A(q, lambda e, o=out, i=in_, kw=kw: e.dma_start(out=o, in_=i, **kw), reads, writes, dma=True)

    def mm(out, lhsT, rhs, start, stop):
        A("pe", lambda e, o=out, l=lhsT, r=rhs, s0=start, s1=stop: e.matmul(o, l, r, start=s0, stop=s1),
          [lhsT, rhs], [out])

    def tr(out, in_, idn):
        A("pe", lambda e, o=out, i=in_, d=idn: e.transpose(o, i, d), [in_, idn], [out])

    def act(out, in_, func, bias=None, scale=None, accum=None):
        rd = [in_]
        kw = {}
        if bias is not None:
            kw["bias"] = bias
            if not isinstance(bias, float):
                rd.append(bias)
        if scale is not None:
            kw["scale"] = scale
            if not isinstance(scale, float):
                rd.append(scale)
        wr_ = [out]
        if accum is not None:
            kw["accum_out"] = accum
            wr_.append(accum)
        A("act", lambda e, o=out, i=in_, f=func, kw=kw: e.activation(o, i, f, **kw), rd, wr_)

    def ts(eng, out, in0, s1, s2, op0, op1=None, accum=None):
        rd = [in0]
        for s_ in (s1, s2):
            if s_ is not None and not isinstance(s_, (float, int)):
                rd.append(s_)
        wr_ = [out]
        kw = {}
        if accum is not None:
            kw["accum_out"] = accum
            wr_.append(accum)
        if op1 is None:
            A(eng, lambda e, o=out, i=in0, a=s1, p=op0, kw=kw: e.tensor_scalar(o, i, a, None, p, **kw), rd, wr_)
        else:
            A(eng, lambda e, o=out, i=in0, a=s1, b=s2, p=op0, q=op1, kw=kw: e.tensor_scalar(o, i, a, b, p, q, **kw), rd, wr_)

    def tt(eng, out, in0, in1, op):
        A(eng, lambda e, o=out, a=in0, b=in1, p=op: e.tensor_tensor(o, a, b, p), [in0, in1], [out])

    def stt(out, in0, scalar, in1, op0, op1, accum=None):
        rd = [in0, in1]
        if not isinstance(scalar, (float, int)):
            rd.append(scalar)
        wr_ = [out]
        kw = {}
        if accum is not None:
            kw["accum_out"] = accum
            wr_.append(accum)
        A("dve", lambda e, o=out, a=in0, s_=scalar, b=in1, p=op0, q=op1, kw=kw:
          e.scalar_tensor_tensor(o, a, s_, b, p, q, **kw), rd, wr_)

    def cp(eng, out, in_):
        if eng == "act":
            A(eng, lambda e, o=out, i=in_: e.copy(o, i), [in_], [out])
        else:
            A(eng, lambda e, o=out, i=in_: e.tensor_copy(o, i), [in_], [out])

    def memset(eng, out, val):
        A(eng, lambda e, o=out, v=val: e.memset(o, v), [], [out])

    def recip(out, in_):
        A("dve", lambda e, o=out, i=in_: e.reciprocal(o, i), [in_], [out])

    dma("sp", ident, ci_d, writes=[ident])
    dma("sp", maskfox, cmf_d, writes=[maskfox])
    dma("sp", maskhg, cmh_d, writes=[maskhg])
    dma("sp", cvecA[0:65, :], cva_d, writes=[cvecA[0:65, :]])
    dma("sp", cvecB, cvb_d, writes=[cvecB])
    dma("sp", sel[0:97, :, :], csel_d.rearrange("p (a b) -> p a b", a=16), writes=[sel[0:97, :, :]])
    dma("sp", gattn, gattn_d.partition_broadcast(128), writes=[gattn])
    dma("sp", gffn, gffn_d.partition_broadcast(128), writes=[gffn])
    dma("sp", gfin, gfin_d.partition_broadcast(128), writes=[gfin])
    dma("sp", brt, br_d.partition_broadcast(128), writes=[brt])
    nonc = dict(allow_slow_non_contiguous=True)
    dma("sp", l0, lbl_d[0:1, :].rearrange("o (h p) -> p (o h)", p=128), writes=[l0], **nonc)
    dma("sp", l1, lbl_d[1:2, :].rearrange("o (h p) -> p (o h)", p=128), writes=[l1], **nonc)
    dma("sp", hgg, hgn_d.rearrange("o p -> p o"), writes=[hgg], **nonc)
    dma("sp", foxg[0:64, :], fxn_d.rearrange("o p -> p o"), writes=[foxg[0:64, :]], **nonc)
    dma("sp", foxg[64:128, :], fxn_d.rearrange("o p -> p o"), writes=[foxg[64:128, :]], **nonc)
    memset("dve", negb, 0.0)
    for g in range(3):
        dma("sp", negb[32 * g:32 * g + 8, :], fb_d.rearrange("o p -> p o"), writes=[negb[32 * g:32 * g + 8, :]], **nonc)
    dma("pool", wout, wout_d.rearrange("(k p) c -> p k c", p=128), writes=[wout])
    dma("pool", wr, wr_d.rearrange("(k p) c -> p k c", p=128), writes=[wr])
    memset("dve", ones, 1.0)
    memset("dve", reset, 1.0)
    memset("dve", reset.rearrange("p (c j) -> p c j", j=64)[:, :, 0:1], 0.0)
    memset("dve", cmathg, 1.0 / 128)
    tt("dve", ltmp, l1, l0, ALU.subtract)
    act(ltmp, ltmp, AF.Exp)
    ts("dve", ltmp, ltmp, 1.0, None, ALU.add)
    recip(lbc, ltmp)
    ts("dve", oml, lbc, -1.0, 1.0, ALU.mult, ALU.add)

    def rms_rstd(src, junk, col):
        ssq = stat[:, col:col + 1]
        var = stat[:, col + 1:col + 2]
        lnv = stat[:, col + 2:col + 3]
        rstd = stat[:, col + 3:col + 4]
        act(junk, src, AF.Square, accum=ssq)
        ts("dve", var, ssq, 1.0 / D, EPS, ALU.mult, ALU.add)
        act(lnv, var, AF.Ln)
        act(rstd, lnv, AF.Exp, scale=-0.5)
        return rstd

    pbank = [0]

    def next_bank(lo, hi):
        b = lo + (pbank[0] % (hi - lo))
        pbank[0] += 1
        return b

    for sq_ in range(nseq):
        xt_s = [sb(W + 77056 + 4096 * i, 4096, F32) for i in range(2)]
        junk = sb(W + 85248, 2048, BF16)
        xs_s = [sb(W + 87296 + 2048 * i, 2048, BF16) for i in range(2)]
        wfox = sb(W, 24704, BF16).rearrange("p (k c) -> p k c", k=8)
        dma("pool", wfox, win_d.rearrange("(k p) c -> p k c", p=128)[:, :, 2048:INC], writes=[wfox])
        for i in range(NT):
            xt = xt_s[i % 2]
            xs = xs_s[i % 2]
            dma("sp", xt, x_d[sq_, i * 128:(i + 1) * 128, :], writes=[xt])
            rstd = rms_rstd(xt, junk, 4 * (i % 8))
            stt(xs, xt, rstd, gattn, ALU.mult, ALU.mult)
            b = next_bank(0, 2)
            pb = bankbf(b).rearrange("p (k t) -> p k t", k=8)
            for kc in range(8):
                tr(pb[:, kc, :], xs[:, kc * 128:(kc + 1) * 128], ident)
            cp("act" if i % 2 else "dve", xnT[:, :, i * 128:(i + 1) * 128], pb)
        if stop_after == "prep":
            break

        vaug = sb(W + 24704, 24704, BF16).rearrange("p (t c) -> p t c", t=NT)
        qa_s = [sb(W + 49408 + 4096 * i, 4096, BF16) for i in range(2)]
        ka_s = [sb(W + 57600 + 4096 * i, 4096, BF16) for i in range(2)]
        parts = sb(W + 65792, 4096, BF16)
        pT_s = [sb(W + 69888 + 1024 * i, 1024, BF16) for i in range(2)]
        sqf = sb(W + 71936, 1024, BF16)
        lnr = sb(W + 72960, 2048, F32)
        rr = sb(W + 75008, 2048, F32)
        fr0 = sb(W + 77056, 8192, F32)
        fr1 = sb(W + 85248, 8192, F32)

        def vcol(h):
            return (h // 2) * 193 + (0 if h % 2 == 0 else 65)

        wf3 = sb(W + 93440, 1152, BF16).rearrange("p (k c) -> p k c", k=8)
        hiT = sb(W + 69888, 4096, BF16)
        memset("dve", parts, 1.0)
        memset("dve", wf3, 0.0)
        for g in range(3):
            cp("dve", wf3[:, :, 32 * g:32 * g + 8], wfox[:, :, 1536:1544])
        for tb in range(4):
            b = next_bank(0, 2)
            pf = banks[b][0:72, :]
            for kc in range(8):
                mm(pf, wf3[:, kc, :], xnT[:, kc, tb * 512:(tb + 1) * 512], kc == 0, kc == 7)
            act(fr0[0:72, tb * 512:(tb + 1) * 512], pf, AF.Identity, bias=negb[0:72, :])
        f0 = fr0[0:72, :]
        f1 = fr1[0:72, :]
        h72 = hiT[0:72, :]
        ts("dve", f1, f0, -80.0, None, ALU.max)
        act(f1, f1, AF.Exp, scale=-1.0)
        act(f1, f1, AF.Ln, bias=1.0)
        ts("dve", f0, f1, -1.0, None, ALU.mult)
        A("dve", lambda e, o=f1, a=ones[0:72, :], b_=f0: e.tensor_tensor_scan(o, a, b_, 0.0, ALU.mult, ALU.add),
          [ones[0:72, :], f0], [f1])
        cp("dve", h72, f1)
        cp("dve", parts[0:8, :], hiT[0:8, :])
        tt("dve", f0, f1, h72, ALU.subtract)
        cp("dve", h72, f0)
        cp("dve", parts[32:40, :], hiT[32:40, :])
        tt("dve", f1, f0, h72, ALU.subtract)
        cp("dve", parts[64:72, :], fr1[64:72, :])

        memset("dve", vaug, 0.0)
        for h in range(8):
            c1 = vcol(h) + (64 if h % 2 == 0 else 0)
            memset("dve", vaug[:, :, c1:c1 + 1], 1.0)
        for i in range(NT):
            b = next_bank(0, 2)
            pv = banks[b]
            for kc in range(8):
                mm(pv[:, :], xnT[:, kc, i * 128:(i + 1) * 128], wfox[:, kc, 1024:1536], kc == 0, kc == 7)
            pv4 = pv[:, :].rearrange("p (h two d) -> p h two d", two=2, d=64)
            vg = vaug[:, i, :].rearrange("p (h c) -> p h c", c=193)
            cp("dve", vg[:, :, 0:64], pv4[:, :, 0, :])
            cp("act", vg[:, :, 129:193], pv4[:, :, 1, :])

        fox_pending = []
        for h in range(8):
            qa = qa_s[h % 2]
            ka = ka_s[h % 2]
            odd = h % 2
            for tb in range(4):
                cs = slice(tb * 512, (tb + 1) * 512)
                for which, dst, scl in ((0, qa, 0.125), (1, ka, 1.0)):
                    b = next_bank(0, 2)
                    pq = banks[b]
                    mm(pq[0:70, :], sel[0:97, which * 8 + h, :], parts[0:97, cs], True, False)
                    wc = which * 512 + h * 64
                    for kc in range(8):
                        mm(pq[0:64, :], wfox[:, kc, wc:wc + 64], xnT[:, kc, cs], False, kc == 7)
                    if which == 0:
                        act(dst[0:70, cs], pq[0:70, :], AF.Copy, scale=scl)
                    else:
                        cp("dve", dst[0:70, cs], pq[0:70, :])
            vc = vcol(h)
            vw = 65 if not odd else 128
            for tb in range(4):
                t0 = tb * 512
                ob = 5 + (h * 4 + tb) % 2
                n_s = 4 * (tb + 1)
                orow = slice(0, 65) if not odd else slice(0, 128)
                lgb = {}

                def emit_qk(j, t0=t0, ka=ka, qa=qa, lgb=lgb):
                    s0 = j * 128
                    c0 = max(0, s0 - t0)
                    diag = s0 >= t0
                    lb_ = next_bank(2, 5)
                    lgb[j] = lb_
                    lg = banks[lb_]
                    mm(lg[:, c0:512], ka[0:70, s0:s0 + 128], qa[0:70, t0 + c0:t0 + 512], True, not diag)
                    if diag:
                        mm(lg[:, c0:c0 + 128], ident, maskfox, False, True)

                emit_qk(0)
                for j in range(n_s):
                    if j + 1 < n_s:
                        emit_qk(j + 1)
                    s0 = j * 128
                    c0 = max(0, s0 - t0)
                    lg = banks[lgb[j]]
                    pT = pT_s[j % 2]
                    act(pT[:, c0:512], lg[:, c0:512], AF.Exp)
                    mm(banks[ob][orow, c0:512], vaug[:, j, vc:vc + vw], pT[:, c0:512], j == 0, j == n_s - 1)
                    if j == min(1, n_s - 1) and fox_pending:
                        fox_pending.pop()()

                def norm_ops(ob=ob, odd=odd, h=h, t0=t0):
                    po = banks[ob]
                    if not odd:
                        act(sqf[0:65, :], po[0:65, :], AF.Square)
                        mm(banks[7][0:64, :], cvecA[0:65, :], sqf[0:65, :], True, True)
                        prow = slice(0, 64)
                    else:
                        act(sqf[:, :], po[:, :], AF.Square)
                        mm(banks[7][:, :], cvecB[:, :], sqf[:, :], True, True)
                        prow = slice(64, 128)
                    act(lnr[prow, :], banks[7][prow, :], AF.Ln)
                    act(rr[prow, :], lnr[prow, :], AF.Exp, scale=-0.5)
                    stt(ocat[prow, 4 + h // 2, t0:t0 + 512], po[prow, :], foxg[prow, :], rr[prow, :], ALU.mult, ALU.mult)

                fox_pending.append(norm_ops)
        while fox_pending:
            fox_pending.pop()()
        if stop_after == "fox":
            break

        whg = sb(W, 32768, BF16).rearrange("p (k c) -> p k c", k=8)
        dma("pool", whg, win_d.rearrange("(k p) c -> p k c", p=128)[:, :, 0:2048], writes=[whg])
        T0 = sb(W + 32768, 8192, F32)
        T1 = sb(W + 40960, 8192, F32)
        T2 = sb(W + 49152, 8192, F32)
        T3 = sb(W + 57344, 8192, F32)
        qtl = sb(W + 65536, 4096, BF16)
        ktl = sb(W + 69632, 4096, BF16)
        ktok = sb(W + 73728, 4096, BF16).rearrange("p (t k) -> p t k", t=NT)
        vtok = sb(W + 77824, 4096, BF16).rearrange("p (t k) -> p t k", t=NT)
        sgT = sb(W + 81920, 4096, BF16)
        Sbf = sb(W + 86016, 8192, BF16).rearrange("p (c v) -> p c v", c=32)
        U_s = [sb(W + 94208 + 512 * i, 512, F32) for i in range(2)]
        scm_s = [sb(W + 95232 + 256 * i, 256, BF16) for i in range(2)]
        lnr2 = sb(W + 40960, 2048, F32)
        rr2 = sb(W + 40960 + 2048, 2048, F32)
        t1b = sb(W + 40960 + 4096, 2048, F32)
        sqh = sb(W + 40960 + 6144, 1024, BF16)
        for h in range(4):
            hs = slice(h * 128, (h + 1) * 128)
            for (coff, dst, fn_) in ((0, T0, AF.Silu), (1536, sgT, AF.Silu), (512, T1, AF.Sigmoid)):
                for tb in range(4):
                    cs = slice(tb * 512, (tb + 1) * 512)
                    b = next_bank(0, 2)
                    pp = banks[b]
                    for kc in range(8):
                        mm(pp[:, :], whg[:, kc, coff + h * 128:coff + (h + 1) * 128], xnT[:, kc, cs], kc == 0, kc == 7)
                    act(dst[:, cs], pp[:, :], fn_)
            for i4 in range(4):
                b = next_bank(0, 2)
                pp = banks[b]
                for ii in range(4):
                    i = i4 * 4 + ii
                    for kc in range(8):
                        mm(pp[:, ii * 128:(ii + 1) * 128], xnT[:, kc, i * 128:(i + 1) * 128],
                           whg[:, kc, 1024 + h * 128:1024 + (h + 1) * 128], kc == 0, kc == 7)
                cp("dve", vtok[:, i4 * 4:(i4 + 1) * 4, :], pp[:, :].rearrange("p (t k) -> p t k", t=4))
            ts("dve", T1, T1, oml[:, h:h + 1], lbc[:, h:h + 1], ALU.mult, ALU.add)
            act(T2, T1, AF.Ln)
            ts("dve", T1, T1, -1.0, 1.0, ALU.mult, ALU.add)
            A("dve", lambda e, o=T3, a=reset, b_=T2: e.tensor_tensor_scan(o, a, b_, 0.0, ALU.mult, ALU.add),
              [reset, T2], [T3])
            act(T2, T3, AF.Exp)
            tt("dve", qtl, T0, T2, ALU.mult)
            act(T0, T3, AF.Exp, scale=-1.0)
            tt("dve", ktl, T1, T0, ALU.mult)
            for i8 in range(2):
                b = next_bank(0, 2)
                pb = bankbf(b).rearrange("p (t k) -> p t k", t=8)
                for ii in range(8):
                    i = i8 * 8 + ii
                    tr(pb[:, ii, :], ktl[:, i * 128:(i + 1) * 128], ident)
                cp("act", ktok[:, i8 * 8:(i8 + 1) * 8, :], pb)
            for c in range(32):
                i, par = c // 2, c % 2
                if c % 4 == 0:
                    sb_ = next_bank(2, 5)
                dS = banks[sb_][:, (c % 4) * 128:(c % 4 + 1) * 128]
                ps_ = slice(par * 64, par * 64 + 64)
                mm(dS, ktok[ps_, i, :], vtok[ps_, i, :], True, True)
                Uc = U_s[c % 2]
                Up = U_s[(c + 1) % 2]
                if c == 0:
                    memset("pool", Sbf[:, 0, :], 0.0)
                    cp("dve", Uc, dS)
                else:
                    Dp = T2[:, (c - 1) * 64 + 63:(c - 1) * 64 + 64]
                    act(Sbf[:, c, :], Up, AF.Copy, scale=Dp)
                    stt(Uc, Up, Dp, dS, ALU.mult, ALU.add)
            for tb in range(4):
                ob = 5 + (h * 4 + tb) % 2
                po = banks[ob]
                scb = {}

                def emit_sc(ii, tb=tb, scb=scb):
                    i = tb * 4 + ii
                    cs = slice(i * 128, (i + 1) * 128)
                    lb_ = next_bank(2, 5)
                    sc = banks[lb_][:, 0:128]
                    mm(sc, ktl[:, cs], qtl[:, cs], True, True)
                    scm = scm_s[i % 2]
                    tt("dve", scm, sc, maskhg, ALU.mult)
                    scb[ii] = scm

                emit_sc(0)
                for ii in range(4):
                    i = tb * 4 + ii
                    if ii + 1 < 4:
                        emit_sc(ii + 1)
                    scm = scb[ii]
                    oo = po[:, ii * 128:(ii + 1) * 128]
                    mm(oo, vtok[:, i, :], scm, True, False)
                    mm(oo[:, 0:64], Sbf[:, 2 * i, :], qtl[:, i * 128:i * 128 + 64], False, False)
                    mm(oo[:, 64:128], Sbf[:, 2 * i + 1, :], qtl[:, i * 128 + 64:i * 128 + 128], False, True)
                act(sqh, po[:, :], AF.Square)
                mm(banks[7][:, :], cmathg, sqh, True, True)
                act(lnr2, banks[7][:, :], AF.Ln, bias=EPS)
                act(rr2, lnr2, AF.Exp, scale=-0.5)
                stt(t1b, po[:, :], hgg, rr2, ALU.mult, ALU.mult)
                tt("pool", ocat[:, h, tb * 512:(tb + 1) * 512], t1b, sgT[:, tb * 512:(tb + 1) * 512], ALU.mult)
        if stop_after == "hg":
            break

        hbuf = sb(W, 65536, F32).rearrange("p (t c) -> p t c", t=NT)
        xt2_s = [sb(TT + 4096 * i, 4096, F32) for i in range(2)]
        junk2 = sb(TT + 8192, 2048, BF16)
        hn_s = [sb(TT + 10240 + 2048 * i, 2048, BF16) for i in range(2)]
        hnT = xnT
        for i in range(NT):
            xt = xt2_s[i % 2]
            dma("sp", xt, x_d[sq_, i * 128:(i + 1) * 128, :], writes=[xt])
            for half in range(2):
                b = next_bank(0, 2)
                ph = banks[b]
                for fc in range(8):
                    mm(ph[:, :], ocat[:, fc, i * 128:(i + 1) * 128], wout[:, fc, half * 512:(half + 1) * 512], fc == 0, fc == 7)
                tt("dve", hbuf[:, i, half * 512:(half + 1) * 512], ph[:, :], xt[:, half * 512:(half + 1) * 512], ALU.add)
            rstd = rms_rstd(hbuf[:, i, :], junk2, 32 + 4 * (i % 8))
            hn = hn_s[i % 2]
            stt(hn, hbuf[:, i, :], rstd, gffn, ALU.mult, ALU.mult)
            b = next_bank(2, 4)
            pb = bankbf(b).rearrange("p (k t) -> p k t", k=8)
            for kc in range(8):
                tr(pb[:, kc, :], hn[:, kc * 128:(kc + 1) * 128], ident)
            cp("act", hnT[:, :, i * 128:(i + 1) * 128], pb)
            b = next_bank(4, 6)
            pr = banks[b][:, 0:20]
            for kc in range(8):
                mm(pr, hnT[:, kc, i * 128:(i + 1) * 128], wr[:, kc, :], kc == 0, kc == 7)
            ro = (i % 2) * 128
            lgt = rt[:, ro:ro + 20]
            gmax = rt[:, ro + 20:ro + 21]
            ngmax = rt[:, ro + 21:ro + 22]
            gm = rt[:, ro + 22:ro + 26]
            eg = rt[:, ro + 26:ro + 30]
            sumg = rt[:, ro + 30:ro + 31]
            pgs = rt[:, ro + 31:ro + 32]
            pen = rt[:, ro + 32:ro + 48]
            elm = rt[:, ro + 48:ro + 64]
            top8 = rt[:, ro + 64:ro + 72]
            nm1 = rt[:, ro + 72:ro + 73]
            selm = rt[:, ro + 73:ro + 89]
            den2 = rt[:, ro + 89:ro + 90]
            wsc = rt[:, ro + 90:ro + 91]
            ex = rt[:, ro + 91:ro + 107]
            tt("dve", lgt, pr, brt, ALU.add)
            A("dve", lambda e, o=gmax, i_=lgt[:, 0:4]: e.reduce_max(o, i_, AX.X), [lgt[:, 0:4]], [gmax])
            ts("dve", gm, lgt[:, 0:4], gmax, None, ALU.is_equal)
            ts("dve", ngmax, gmax, -1.0, None, ALU.mult)
            act(eg, lgt[:, 0:4], AF.Exp, bias=ngmax, accum=sumg)
            recip(pgs, sumg)
            gm_b = bass.AP(gm.tensor, gm.offset, [list(gm.ap[0]), [1, 4], [0, 4]])
            pen3 = pen.rearrange("p (g j) -> p g j", j=4)
            ts("dve", pen3, gm_b, -1.0, 1e30, ALU.add, ALU.mult)
            tt("dve", elm, lgt[:, 4:20], pen, ALU.add)
            A("dve", lambda e, o=top8, i_=elm: e.max(o, i_), [elm], [top8])
            ts("dve", selm, elm, top8[:, 1:2], None, ALU.is_ge)
            ts("dve", nm1, top8[:, 0:1], -1.0, None, ALU.mult)
            act(ex, elm, AF.Exp, bias=nm1)
            stt(ex, ex, 1.0, selm, ALU.mult, ALU.mult, accum=den2)
            recip(den2, den2)
            tt("dve", wsc, den2, pgs, ALU.mult)
            ts("dve", gates[:, i, :], ex, wsc, None, ALU.mult)
        if stop_after == "wout":
            if dbg:
                for i in range(NT):
                    dma("sp", dbg_d[i * 128:(i + 1) * 128, :], hbuf[:, i, :], reads=[hbuf[:, i, :]])
            break

        wslot = [OCAT, TT]
        hact_s = [sb(OCAT + 24576 + 4096 * i, 4096, BF16).rearrange("p (c t) -> p c t", c=4) for i in range(2)]
        sgt_s = [sb(TT + 24576 + 2048 * i, 2048, F32) for i in range(2)]
        blk = 0
        for e in range(NE):
            wo = wslot[e % 2]
            wg = sb(wo, 8192, BF16).rearrange("p (k c) -> p k c", k=8)
            wu = sb(wo + 8192, 8192, BF16).rearrange("p (k c) -> p k c", k=8)
            wd = sb(wo + 16384, 8192, BF16).rearrange("p (k c) -> p k c", k=4)
            dma("pool", wg, wg_d[e].rearrange("(k p) c -> p k c", p=128), writes=[wg])
            dma("pool", wu, wu_d[e].rearrange("(k p) c -> p k c", p=128), writes=[wu])
            dma("pool", wd, wd_d[e].rearrange("(k p) c -> p k c", p=128), writes=[wd])
            for tb in range(4):
                cs = slice(tb * 512, (tb + 1) * 512)
                hact = hact_s[blk % 2]
                blk += 1
                for hc in range(4):
                    gb = next_bank(0, 2)
                    ub = 2 + next_bank(0, 2) % 2
                    for kc in range(8):
                        mm(banks[gb][:, :], wg[:, kc, hc * 128:(hc + 1) * 128], hnT[:, kc, cs], kc == 0, kc == 7)
                    for kc in range(8):
                        mm(banks[ub][:, :], wu[:, kc, hc * 128:(hc + 1) * 128], hnT[:, kc, cs], kc == 0, kc == 7)
                    sgt = sgt_s[hc % 2]
                    act(sgt, banks[gb][:, :], AF.Silu)
                    tt("dve", hact[:, hc, :], sgt, banks[ub][:, :], ALU.mult)
                for ii in range(4):
                    i = tb * 4 + ii
                    for half in range(2):
                        yb = next_bank(4, 8)
                        for hc in range(4):
                            mm(banks[yb][:, :], hact[:, hc, ii * 128:(ii + 1) * 128], wd[:, hc, half * 512:(half + 1) * 512], hc == 0, hc == 3)
                        hh = hbuf[:, i, half * 512:(half + 1) * 512]
                        stt(hh, banks[yb][:, :], gates[:, i, e:e + 1], hh, ALU.mult, ALU.add)
        for i in range(NT):
            rstd = rms_rstd(hbuf[:, i, :], junk2, 64 + 4 * (i % 8))
            stt(hbuf[:, i, :], hbuf[:, i, :], rstd, gfin, ALU.mult, ALU.mult)
            dma("sp", out_d[sq_, i * 128:(i + 1) * 128, :], hbuf[:, i, :], reads=[hbuf[:, i, :]])

    sch.prepare()
    sems = {k: es.enter_context(nc.semaphore(f"s_{k}")) for k in ("pe", "act", "dve", "pool")}
    dsems = {}
    for q in ("sp", "pool", "act"):
        for k in range(sch.n_dma_sems):
            dsems[(q, k)] = es.enter_context(nc.semaphore(f"d_{q}{k}"))
    with nc.Block() as block:
        @block.tensor
        def _(eng):
            sch.emit_engine("pe", eng, sems, dsems)

        @block.scalar
        def _(eng):
            sch.emit_engine("act", eng, sems, dsems)

        @block.vector
        def _(eng):
            sch.emit_engine("dve", eng, sems, dsems)

        @block.gpsimd
        def _(eng):
            sch.emit_engine("pool", eng, sems, dsems)

        @block.sync
        def _(eng):
            sch.emit_engine("sp", eng, sems, dsems)
    es.close()
    return nc, sch


_CACHE = {}


def _get_nc(nseq):
    if nseq not in _CACHE:
        _CACHE[nseq] = build(nseq, Sched, F32, BF16, U8)[0]
    return _CACHE[nseq]


def kernel(x, attn_norm, w_in, hg_lb_logits, hg_norm, fox_f_bias, fox_norm, w_out, ffn_norm,
           w_group, b_group, w_expert, b_expert, w_gate, w_up, w_down, final_norm):
    f = lambda a: np.ascontiguousarray(np.asarray(a, dtype=np.float32))
    x = f(x)
    n_cores = 8
    B = x.shape[0]
    nseq = B // n_cores
    shared = {
        "w_in": f(w_in)[0], "w_out": f(w_out)[0], "w_gate": f(w_gate)[0], "w_up": f(w_up)[0],
        "w_down": f(w_down)[0],
        "w_router": np.ascontiguousarray(np.concatenate([f(w_group)[0], f(w_expert)[0]], axis=1)),
        "b_router": np.ascontiguousarray(np.concatenate([f(b_group)[0], f(b_expert)[0]])[None, :]),
        "attn_norm": f(attn_norm), "ffn_norm": f(ffn_norm), "final_norm": f(final_norm).reshape(1, -1),
        "hg_lb_logits": f(hg_lb_logits), "hg_norm": f(hg_norm), "fox_f_bias": f(fox_f_bias),
        "fox_norm": f(fox_norm),
    }
    shared.update(make_consts())
    in_maps = []
    for c in range(n_cores):
        m = dict(shared)
        m["x"] = np.ascontiguousarray(x[c * nseq:(c + 1) * nseq])
        in_maps.append(m)
    nc = _get_nc(nseq)
    res = run_bass_kernel_spmd(nc, in_maps, core_ids=list(range(n_cores)))
    return np.concatenate([r["out"] for r in res.results], axis=0).astype(np.float32)
```

```python
import contextlib
import numpy as np
import ml_dtypes
import concourse.bass as bass
import concourse.mybir as mybir

F32 = mybir.dt.float32
BF16 = mybir.dt.bfloat16
U8 = mybir.dt.uint8
_ES = {F32: 4, BF16: 2, U8: 1, mybir.dt.int32: 4, mybir.dt.uint32: 4, mybir.dt.uint16: 2}


class Op:
    __slots__ = ("eng", "fn", "deps", "cdeps", "seq", "ms", "needed", "dma", "dsem", "dval", "dprev", "tag")

    def __init__(self, eng, fn, dma, tag):
        self.eng = eng
        self.fn = fn
        self.deps = set()
        self.cdeps = {}
        self.seq = 0
        self.ms = None
        self.needed = False
        self.dma = dma
        self.dsem = None
        self.dval = None
        self.dprev = 0
        self.tag = tag


class Sched:
    def __init__(self, n_dma_sems=8, same_engine_sync=True):
        self.ops = []
        self.recs = {}
        self.n_dma_sems = n_dma_sems
        self.same_engine_sync = same_engine_sync
        self.dram_track = set()
        self.whole = {}

    def _regions(self, ap):
        t = ap.tensor
        cls = type(t).__name__
        if cls.startswith("DRam"):
            name = ap.name
            if name not in self.dram_track:
                return None
            es = _ES[ap.dtype]
            pat = ap.ap
            off = int(ap.offset)
            hi = off + sum((n - 1) * abs(st) for st, n in pat) + 1
            return name, False, 0, 1, [(off * es, hi * es)]
        psum = cls.startswith("PSum") or cls.startswith("Psum")
        name = ap.name
        if psum:
            return name, True, 0, 128, [(0, 1 << 20)]
        es = _ES[ap.dtype]
        pat = ap.ap
        off = int(ap.offset)
        pstride, pcnt = pat[0]
        p0 = off // pstride
        c0 = off % pstride
        free = list(pat[1:])
        if not free:
            return name, False, p0, p0 + pcnt, [(c0 * es, (c0 + 1) * es)]
        ls, ln = free[-1]
        run = (ln - 1) * abs(ls) + 1
        outer = free[:-1]
        nout = 1
        for s, n in outer:
            nout *= n
        ivs = []
        if nout <= 64:
            idx = [0] * len(outer)
            while True:
                st = c0 + sum(i * s for i, (s, n) in zip(idx, outer))
                ivs.append((st * es, (st + run) * es))
                k = len(outer) - 1
                while k >= 0:
                    idx[k] += 1
                    if idx[k] < outer[k][1]:
                        break
                    idx[k] = 0
                    k -= 1
                if k < 0:
                    break
        else:
            hi = c0 + sum((n - 1) * abs(s) for s, n in outer) + run
            ivs.append((c0 * es, hi * es))
        ivs.sort()
        out = [ivs[0]]
        for a, b in ivs[1:]:
            if a <= out[-1][1]:
                out[-1] = (out[-1][0], max(out[-1][1], b))
            else:
                out.append((a, b))
        return name, False, p0, p0 + pcnt, out

    def _access(self, op, ap, is_write):
        r = self._regions(ap)
        if r is None:
            return
        name, psum, p0, p1, ivs = r
        tab = self.recs.setdefault(name, {})
        w = is_write or psum
        SH = 20 if name in self.dram_track else 11
        for (b0, b1) in ivs:
            newrec = (p0, p1, b0, b1, op, w)
            for bk in range(b0 >> SH, ((b1 - 1) >> SH) + 1):
                lst = tab.get(bk)
                if lst is None:
                    tab[bk] = [newrec]
                    continue
                keep = []
                for rec in lst:
                    rp0, rp1, rb0, rb1, rop, rw = rec
                    ov = rp0 < p1 and p0 < rp1 and rb0 < b1 and b0 < rb1
                    if ov and (rw or w) and rop is not op:
                        if rop.dma:
                            op.deps.add(rop)
                        else:
                            c = op.cdeps.get(rop.eng)
                            if c is None or c.seq < rop.seq:
                                op.cdeps[rop.eng] = rop
                    if ov and w and rp0 >= p0 and rp1 <= p1 and rb0 >= b0 and rb1 <= b1:
                        continue
                    if (not w) and (not rw) and rop.eng == op.eng and not rop.dma and not op.dma \
                            and rp0 == p0 and rp1 == p1 and rb0 == b0 and rb1 == b1:
                        continue
                    keep.append(rec)
                keep.append(newrec)
                tab[bk] = keep

    def add(self, eng, fn, reads=(), writes=(), dma=False, tag=None):
        op = Op(eng, fn, dma, tag)
        op.seq = len(self.ops)
        for ap in reads:
            self._access(op, ap, False)
        for ap in writes:
            self._access(op, ap, True)
        op.deps.update(op.cdeps.values())
        op.cdeps = None
        self.ops.append(op)
        return op

    def prepare(self):
        for op in self.ops:
            for d in op.deps:
                if d.dma:
                    continue
                if d.eng == op.eng and (d.eng == "pe" or not self.same_engine_sync) and not op.dma:
                    continue
                d.needed = True
        cnt = {}
        dcnt = {}
        dslot = {}
        for op in self.ops:
            if op.dma:
                k = dslot.get(op.eng, 0)
                dslot[op.eng] = k + 1
                slot = (op.eng, k % self.n_dma_sems)
                prev = dcnt.get(slot, 0)
                op.dsem = slot
                op.dprev = prev
                op.dval = prev + 16
                dcnt[slot] = op.dval
            elif op.needed:
                cnt[op.eng] = cnt.get(op.eng, 0) + 1
                op.ms = cnt[op.eng]
        self.final_dma = dcnt
        self.ms_total = cnt

    def emit_engine(self, eng_name, eng, sems, dsems):
        waited = {}

        def need(sem_key, sem, val):
            if val <= 0:
                return
            if waited.get(sem_key, 0) >= val:
                return
            waited[sem_key] = val
            eng.wait_ge(sem, val)

        n = 0
        for op in self.ops:
            if op.eng != eng_name:
                continue
            for d in op.deps:
                if d.dma:
                    need(d.dsem, dsems[d.dsem], d.dval)
                else:
                    if d.eng == eng_name and not op.dma and (eng_name == "pe" or not self.same_engine_sync):
                        continue
                    need(d.eng, sems[d.eng], d.ms)
            if op.dma:
                need(op.dsem, dsems[op.dsem], op.dprev)
            ins = op.fn(eng)
            if op.dma:
                ins.then_inc(dsems[op.dsem], 16)
            elif op.needed:
                ins.then_inc(sems[eng_name], 1)
            n += 1
        for slot, val in self.final_dma.items():
            if slot[0] == eng_name:
                need(slot, dsems[slot], val)
        return n


from concourse.bass_utils import run_bass_kernel_spmd

AF = mybir.ActivationFunctionType
ALU = mybir.AluOpType
AX = mybir.AxisListType
I32 = mybir.dt.int32

D = 1024
S = 2048
NT = S // 128
HGW = 512
FOXW = 512
INC = 3592
NE = 16
EH = 512
EPS = 1e-6

ARENA = 212000
PERS = 208000
C_IDENT = 0
C_MASKFOX = 256
C_MASKHG = 512
C_CMATHG = 768
C_CVECA = 1024
C_CVECB = 1152
C_SEL = 1408
C_ONES = 3648
C_RESET = 7744
C_GATTN = 11840
C_GFFN = 15936
C_GFIN = 20032
C_SMALL = 24128
C_WOUT = 24640
C_WR = 41024
C_STAT = 41344
C_GATES = 42368
C_RT = 43392
XNT = 44544
OCAT = XNT + 32768
W = OCAT + 32768
WSIZE = PERS - W
TT = W + 65536


def make_consts():
    bf = ml_dtypes.bfloat16
    ident = np.eye(128, dtype=np.float32).astype(bf)
    s_ = np.arange(128)[:, None]
    t_ = np.arange(128)[None, :]
    maskfox = np.where(t_ >= s_, 0.0, -30000.0).astype(np.float32).astype(bf)
    maskhg = ((t_ >= s_) & ((t_ // 64) == (s_ // 64))).astype(np.float32).astype(bf)
    cvecA = np.zeros((65, 64), np.float32)
    cvecA[0:64, :] = 1.0 / 64
    cvecA[64, :] = EPS
    cvecB = np.zeros((128, 128), np.float32)
    cvecB[0, 64:128] = EPS
    cvecB[64:128, 64:128] = 1.0 / 64
    sel = np.zeros((97, 16, 70), np.float32)
    for h in range(8):
        sel[h, h, 64] = 8.0
        sel[32 + h, h, 65] = 8.0
        sel[64 + h, h, 66] = 8.0
        sel[96, h, 67:70] = 8.0
        sel[96, 8 + h, 64:67] = 1.0
        sel[h, 8 + h, 67] = -1.0
        sel[32 + h, 8 + h, 68] = -1.0
        sel[64 + h, 8 + h, 69] = -1.0
    lstrict = (s_ < t_).astype(np.float32).astype(bf)
    thr = np.tile((512.0 * np.arange(16, dtype=np.float32))[None, :], (128, 1))
    jt = np.tile(np.arange(64, dtype=np.float32)[None, :], (128, 1))
    pcol2 = (2.0 * np.arange(128, dtype=np.float32)).reshape(128, 1)
    return {
        "c_lstrict": lstrict, "c_thr": thr, "c_jt": jt, "c_pcol2": pcol2,
        "c_ident": ident,
        "c_maskfox": maskfox,
        "c_maskhg": maskhg,
        "c_cveca": cvecA.astype(bf),
        "c_cvecb": cvecB.astype(bf),
        "c_sel": sel.reshape(97, 16 * 70).astype(bf),
    }


def build(nseq, Sched, F32, BF16, U8, stop_after=None, dbg=False):
    nc = bass.Bass("TRN2", target_bir_lowering=False)
    dt_ = nc.dram_tensor
    x_d = dt_("x", [nseq, S, D], F32, kind="ExternalInput").ap()
    win_d = dt_("w_in", [D, INC], F32, kind="ExternalInput").ap()
    wout_d = dt_("w_out", [D, D], F32, kind="ExternalInput").ap()
    wg_d = dt_("w_gate", [NE * 256, 2048], F32, kind="ExternalInput").ap()
    wu_d = dt_("w_up", [NE * 256, 2048], F32, kind="ExternalInput").ap()
    wd_d = dt_("w_down", [NE * 256, 2048], F32, kind="ExternalInput").ap()
    cls_d = dt_("c_lstrict", [128, 128], BF16, kind="ExternalInput").ap()
    cthr_d = dt_("c_thr", [128, 16], F32, kind="ExternalInput").ap()
    cjt_d = dt_("c_jt", [128, 64], F32, kind="ExternalInput").ap()
    cp2_d = dt_("c_pcol2", [128, 1], F32, kind="ExternalInput").ap()
    NTT = nseq * NT
    NSLOT = nseq * 8 + 16
    NROW = NSLOT * 512
    h_scr = dt_("h_scr", [NTT * 128, D], F32, kind="Internal").ap()
    hn_scr = dt_("hn_scr", [NTT * 128, D], BF16, kind="Internal").ap()
    so_scr = dt_("so_scr", [NROW, D], BF16, kind="Internal").ap()
    y_scr = dt_("y_scr", [NROW, D], F32, kind="Internal").ap()
    wr_d = dt_("w_router", [D, 20], F32, kind="ExternalInput").ap()
    br_d = dt_("b_router", [1, 20], F32, kind="ExternalInput").ap()
    gattn_d = dt_("attn_norm", [1, D], F32, kind="ExternalInput").ap()
    gffn_d = dt_("ffn_norm", [1, D], F32, kind="ExternalInput").ap()
    gfin_d = dt_("final_norm", [1, D], F32, kind="ExternalInput").ap()
    lbl_d = dt_("hg_lb_logits", [2, HGW], F32, kind="ExternalInput").ap()
    hgn_d = dt_("hg_norm", [1, 128], F32, kind="ExternalInput").ap()
    fb_d = dt_("fox_f_bias", [1, 8], F32, kind="ExternalInput").ap()
    fxn_d = dt_("fox_norm", [1, 64], F32, kind="ExternalInput").ap()
    ci_d = dt_("c_ident", [128, 128], BF16, kind="ExternalInput").ap()
    cmf_d = dt_("c_maskfox", [128, 128], BF16, kind="ExternalInput").ap()
    cmh_d = dt_("c_maskhg", [128, 128], BF16, kind="ExternalInput").ap()
    cva_d = dt_("c_cveca", [65, 64], BF16, kind="ExternalInput").ap()
    cvb_d = dt_("c_cvecb", [128, 128], BF16, kind="ExternalInput").ap()
    csel_d = dt_("c_sel", [97, 16 * 70], BF16, kind="ExternalInput").ap()
    out_d = dt_("out", [nseq, S, D], F32, kind="ExternalOutput").ap()
    dbg_d = None
    if dbg:
        dbg_d = dt_("dbg", [S, D], F32, kind="ExternalOutput").ap()

    sch = Sched()
    sch.dram_track.update(["h_scr", "hn_scr", "so_scr", "y_scr"])
    es = contextlib.ExitStack()
    arena = es.enter_context(nc.sbuf_tensor("arena", [128, ARENA], U8))
    banks = [es.enter_context(nc.psum_tensor(f"bank{i}", [128, 512], F32)) for i in range(8)]

    def sb(off, nbytes, dt):
        return arena[:, off:off + nbytes].bitcast(dt)

    def bankbf(i):
        return banks[i][:, :].bitcast(BF16)

    ident = sb(C_IDENT, 256, BF16)
    maskfox = sb(C_MASKFOX, 256, BF16)
    maskhg = sb(C_MASKHG, 256, BF16)
    cmathg = sb(C_CMATHG, 256, BF16)
    cvecA = sb(C_CVECA, 128, BF16)
    cvecB = sb(C_CVECB, 256, BF16)
    sel = sb(C_SEL, 2240, BF16).rearrange("p (a b) -> p a b", a=16)
    ones = sb(C_ONES, 4096, BF16)
    reset = sb(C_RESET, 4096, BF16)
    gattn = sb(C_GATTN, 4096, F32)
    gffn = sb(C_GFFN, 4096, F32)
    M2b = sb(C_GFIN, 2048, BF16)
    M1b = sb(C_GFIN + 2048, 2048, BF16)
    small = sb(C_SMALL, 512, F32)
    lbc = small[:, 0:4]
    oml = small[:, 4:8]
    hgg = small[:, 8:9]
    foxg = small[:, 9:10]
    negb = small[:, 10:11]
    l0 = small[:, 11:15]
    l1 = small[:, 15:19]
    ltmp = small[:, 19:23]
    brt = small[:, 24:44]
    wout = sb(C_WOUT, 16384, BF16).rearrange("p (k c) -> p k c", k=8)
    wr = sb(C_WR, 320, BF16).rearrange("p (k c) -> p k c", k=8)
    stat = sb(C_STAT, 1024, F32)
    w1s = sb(C_GATES, 256, F32)
    w2s = sb(C_GATES + 256, 256, F32)
    pos1i = sb(C_GATES + 512, 256, I32)
    pos2i = sb(C_GATES + 768, 256, I32)
    lstrict = sb(PERS, 256, BF16)
    thr = sb(PERS + 256, 64, F32)
    jt = sb(PERS + 320, 256, F32)
    pcol2 = sb(PERS + 576, 4, F32)
    idxW0 = sb(PERS + 640, 256, I32)
    idxW1 = sb(PERS + 896, 256, I32)
    rt = sb(C_RT, 1024, F32)
    xnT = sb(XNT, 32768, BF16).rearrange("p (k t) -> p k t", k=8)
    ocat = sb(OCAT, 32768, BF16).rearrange("p (k t) -> p k t", k=8)

    def A(eng, fn, reads=(), writes=(), dma=False, tag=None):
        return sch.add(eng, fn, reads, writes, dma, tag)

    def dma(q, out, in_, reads=(), writes=(), **kw):
        A(q, lambda e, o=out, i=in_, kw=kw: e.dma_start(out=o, in_=i, **kw), reads, writes, dma=True)

    def mm(out, lhsT, rhs, start, stop):
        A("pe", lambda e, o=out, l=lhsT, r=rhs, s0=start, s1=stop: e.matmul(o, l, r, start=s0, stop=s1),
          [lhsT, rhs], [out])

    def tr(out, in_, idn):
        A("pe", lambda e, o=out, i=in_, d=idn: e.transpose(o, i, d), [in_, idn], [out])

    def act(out, in_, func, bias=None, scale=None, accum=None):
        rd = [in_]
        kw = {}
        if bias is not None:
            kw["bias"] = bias
            if not isinstance(bias, float):
                rd.append(bias)
        if scale is not None:
            kw["scale"] = scale
            if not isinstance(scale, float):
                rd.append(scale)
        wr_ = [out]
        if accum is not None:
            kw["accum_out"] = accum
            wr_.append(accum)
        A("act", lambda e, o=out, i=in_, f=func, kw=kw: e.activation(o, i, f, **kw), rd, wr_)

    def ts(eng, out, in0, s1, s2, op0, op1=None, accum=None):
        rd = [in0]
        for s_ in (s1, s2):
            if s_ is not None and not isinstance(s_, (float, int)):
                rd.append(s_)
        wr_ = [out]
        kw = {}
        if accum is not None:
            kw["accum_out"] = accum
            wr_.append(accum)
        if op1 is None:
            A(eng, lambda e, o=out, i=in0, a=s1, p=op0, kw=kw: e.tensor_scalar(o, i, a, None, p, **kw), rd, wr_)
        else:
            A(eng, lambda e, o=out, i=in0, a=s1, b=s2, p=op0, q=op1, kw=kw: e.tensor_scalar(o, i, a, b, p, q, **kw), rd, wr_)

    def tt(eng, out, in0, in1, op):
        A(eng, lambda e, o=out, a=in0, b=in1, p=op: e.tensor_tensor(o, a, b, p), [in0, in1], [out])

    def stt(out, in0, scalar, in1, op0, op1, accum=None):
        rd = [in0, in1]
        if not isinstance(scalar, (float, int)):
            rd.append(scalar)
        wr_ = [out]
        kw = {}
        if accum is not None:
            kw["accum_out"] = accum
            wr_.append(accum)
        A("dve", lambda e, o=out, a=in0, s_=scalar, b=in1, p=op0, q=op1, kw=kw:
          e.scalar_tensor_tensor(o, a, s_, b, p, q, **kw), rd, wr_)

    def cp(eng, out, in_):
        if eng == "act":
            A(eng, lambda e, o=out, i=in_: e.copy(o, i), [in_], [out])
        else:
            A(eng, lambda e, o=out, i=in_: e.tensor_copy(o, i), [in_], [out])

    def memset(eng, out, val):
        A(eng, lambda e, o=out, v=val: e.memset(o, v), [], [out])

    def recip(out, in_):
        A("dve", lambda e, o=out, i=in_: e.reciprocal(o, i), [in_], [out])

    dma("sp", ident, ci_d, writes=[ident])
    dma("sp", maskfox, cmf_d, writes=[maskfox])
    dma("sp", maskhg, cmh_d, writes=[maskhg])
    dma("sp", cvecA[0:65, :], cva_d, writes=[cvecA[0:65, :]])
    dma("sp", cvecB, cvb_d, writes=[cvecB])
    dma("sp", sel[0:97, :, :], csel_d.rearrange("p (a b) -> p a b", a=16), writes=[sel[0:97, :, :]])
    dma("sp", gattn, gattn_d.partition_broadcast(128), writes=[gattn])
    dma("sp", gffn, gffn_d.partition_broadcast(128), writes=[gffn])
    dma("sp", lstrict, cls_d, writes=[lstrict])
    dma("sp", thr, cthr_d, writes=[thr])
    dma("sp", jt, cjt_d, writes=[jt])
    dma("sp", pcol2, cp2_d, writes=[pcol2])
    dma("sp", brt, br_d.partition_broadcast(128), writes=[brt])
    nonc = dict(allow_slow_non_contiguous=True)
    dma("sp", l0, lbl_d[0:1, :].rearrange("o (h p) -> p (o h)", p=128), writes=[l0], **nonc)
    dma("sp", l1, lbl_d[1:2, :].rearrange("o (h p) -> p (o h)", p=128), writes=[l1], **nonc)
    dma("sp", hgg, hgn_d.rearrange("o p -> p o"), writes=[hgg], **nonc)
    dma("sp", foxg[0:64, :], fxn_d.rearrange("o p -> p o"), writes=[foxg[0:64, :]], **nonc)
    dma("sp", foxg[64:128, :], fxn_d.rearrange("o p -> p o"), writes=[foxg[64:128, :]], **nonc)
    memset("dve", negb, 0.0)
    for g in range(3):
        dma("sp", negb[32 * g:32 * g + 8, :], fb_d.rearrange("o p -> p o"), writes=[negb[32 * g:32 * g + 8, :]], **nonc)
    dma("pool", wout, wout_d.rearrange("(k p) c -> p k c", p=128), writes=[wout])
    dma("pool", wr, wr_d.rearrange("(k p) c -> p k c", p=128), writes=[wr])
    memset("dve", ones, 1.0)
    memset("dve", reset, 1.0)
    memset("dve", reset.rearrange("p (c j) -> p c j", j=64)[:, :, 0:1], 0.0)
    memset("dve", cmathg, 1.0 / 128)
    tt("dve", ltmp, l1, l0, ALU.subtract)
    act(ltmp, ltmp, AF.Exp)
    ts("dve", ltmp, ltmp, 1.0, None, ALU.add)
    recip(lbc, ltmp)
    ts("dve", oml, lbc, -1.0, 1.0, ALU.mult, ALU.add)

    def rms_rstd(src, junk, col):
        ssq = stat[:, col:col + 1]
        var = stat[:, col + 1:col + 2]
        lnv = stat[:, col + 2:col + 3]
        rstd = stat[:, col + 3:col + 4]
        act(junk, src, AF.Square, accum=ssq)
        ts("dve", var, ssq, 1.0 / D, EPS, ALU.mult, ALU.add)
        act(lnv, var, AF.Ln)
        act(rstd, lnv, AF.Exp, scale=-0.5)
        return rstd

    pbank = [0]

    def next_bank(lo, hi):
        b = lo + (pbank[0] % (hi - lo))
        pbank[0] += 1
        return b

    for sq_ in range(nseq):
        xt_s = [sb(W + 77056 + 4096 * i, 4096, F32) for i in range(2)]
        junk = sb(W + 85248, 2048, BF16)
        xs_s = [sb(W + 87296 + 2048 * i, 2048, BF16) for i in range(2)]
        wfox = sb(W, 24704, BF16).rearrange("p (k c) -> p k c", k=8)
        dma("pool", wfox, win_d.rearrange("(k p) c -> p k c", p=128)[:, :, 2048:INC], writes=[wfox])
        for i in range(NT):
            xt = xt_s[i % 2]
            xs = xs_s[i % 2]
            dma("sp", xt, x_d[sq_, i * 128:(i + 1) * 128, :], writes=[xt])
            rstd = rms_rstd(xt, junk, 4 * (i % 8))
            stt(xs, xt, rstd, gattn, ALU.mult, ALU.mult)
            b = next_bank(0, 2)
            pb = bankbf(b).rearrange("p (k t) -> p k t", k=8)
            for kc in range(8):
                tr(pb[:, kc, :], xs[:, kc * 128:(kc + 1) * 128], ident)
            cp("act" if i % 2 else "dve", xnT[:, :, i * 128:(i + 1) * 128], pb)
        if stop_after == "prep":
            break

        vaug = sb(W + 24704, 24704, BF16).rearrange("p (t c) -> p t c", t=NT)
        qa_s = [sb(W + 49408 + 4096 * i, 4096, BF16) for i in range(2)]
        ka_s = [sb(W + 57600 + 4096 * i, 4096, BF16) for i in range(2)]
        parts = sb(W + 65792, 4096, BF16)
        pT_s = [sb(W + 69888 + 1024 * i, 1024, BF16) for i in range(2)]
        sqf = sb(W + 71936, 1024, BF16)
        lnr = sb(W + 72960, 2048, F32)
        rr = sb(W + 75008, 2048, F32)
        fr0 = sb(W + 77056, 8192, F32)
        fr1 = sb(W + 85248, 8192, F32)

        def vcol(h):
            return (h // 2) * 193 + (0 if h % 2 == 0 else 65)

        wf3 = sb(W + 93440, 1152, BF16).rearrange("p (k c) -> p k c", k=8)
        hiT = sb(W + 69888, 4096, BF16)
        memset("dve", parts, 1.0)
        memset("dve", wf3, 0.0)
        for g in range(3):
            cp("dve", wf3[:, :, 32 * g:32 * g + 8], wfox[:, :, 1536:1544])
        for tb in range(4):
            b = next_bank(0, 2)
            pf = banks[b][0:72, :]
            for kc in range(8):
                mm(pf, wf3[:, kc, :], xnT[:, kc, tb * 512:(tb + 1) * 512], kc == 0, kc == 7)
            act(fr0[0:72, tb * 512:(tb + 1) * 512], pf, AF.Identity, bias=negb[0:72, :])
        f0 = fr0[0:72, :]
        f1 = fr1[0:72, :]
        h72 = hiT[0:72, :]
        ts("dve", f1, f0, -80.0, None, ALU.max)
        act(f1, f1, AF.Exp, scale=-1.0)
        act(f1, f1, AF.Ln, bias=1.0)
        ts("dve", f0, f1, -1.0, None, ALU.mult)
        A("dve", lambda e, o=f1, a=ones[0:72, :], b_=f0: e.tensor_tensor_scan(o, a, b_, 0.0, ALU.mult, ALU.add),
          [ones[0:72, :], f0], [f1])
        cp("dve", h72, f1)
        cp("dve", parts[0:8, :], hiT[0:8, :])
        tt("dve", f0, f1, h72, ALU.subtract)
        cp("dve", h72, f0)
        cp("dve", parts[32:40, :], hiT[32:40, :])
        tt("dve", f1, f0, h72, ALU.subtract)
        cp("dve", parts[64:72, :], fr1[64:72, :])

        memset("dve", vaug, 0.0)
        for h in range(8):
            c1 = vcol(h) + (64 if h % 2 == 0 else 0)
            memset("dve", vaug[:, :, c1:c1 + 1], 1.0)
        for i in range(NT):
            b = next_bank(0, 2)
            pv = banks[b]
            for kc in range(8):
                mm(pv[:, :], xnT[:, kc, i * 128:(i + 1) * 128], wfox[:, kc, 1024:1536], kc == 0, kc == 7)
            pv4 = pv[:, :].rearrange("p (h two d) -> p h two d", two=2, d=64)
            vg = vaug[:, i, :].rearrange("p (h c) -> p h c", c=193)
            cp("dve", vg[:, :, 0:64], pv4[:, :, 0, :])
            cp("act", vg[:, :, 129:193], pv4[:, :, 1, :])

        fox_pending = []
        for h in range(8):
            qa = qa_s[h % 2]
            ka = ka_s[h % 2]
            odd = h % 2
            for tb in range(4):
                cs = slice(tb * 512, (tb + 1) * 512)
                for which, dst, scl in ((0, qa, 0.125), (1, ka, 1.0)):
                    b = next_bank(0, 2)
                    pq = banks[b]
                    mm(pq[0:70, :], sel[0:97, which * 8 + h, :], parts[0:97, cs], True, False)
                    wc = which * 512 + h * 64
                    for kc in range(8):
                        mm(pq[0:64, :], wfox[:, kc, wc:wc + 64], xnT[:, kc, cs], False, kc == 7)
                    if which == 0:
                        act(dst[0:70, cs], pq[0:70, :], AF.Copy, scale=scl)
                    else:
                        cp("dve", dst[0:70, cs], pq[0:70, :])
            vc = vcol(h)
            vw = 65 if not odd else 128
            for tb in range(4):
                t0 = tb * 512
                ob = 5 + (h * 4 + tb) % 2
                n_s = 4 * (tb + 1)
                orow = slice(0, 65) if not odd else slice(0, 128)
                lgb = {}

                def emit_qk(j, t0=t0, ka=ka, qa=qa, lgb=lgb):
                    s0 = j * 128
                    c0 = max(0, s0 - t0)
                    diag = s0 >= t0
                    lb_ = next_bank(2, 5)
                    lgb[j] = lb_
                    lg = banks[lb_]
                    mm(lg[:, c0:512], ka[0:70, s0:s0 + 128], qa[0:70, t0 + c0:t0 + 512], True, not diag)
                    if diag:
                        mm(lg[:, c0:c0 + 128], ident, maskfox, False, True)

                emit_qk(0)
                for j in range(n_s):
                    if j + 1 < n_s:
                        emit_qk(j + 1)
                    s0 = j * 128
                    c0 = max(0, s0 - t0)
                    lg = banks[lgb[j]]
                    pT = pT_s[j % 2]
                    act(pT[:, c0:512], lg[:, c0:512], AF.Exp)
                    mm(banks[ob][orow, c0:512], vaug[:, j, vc:vc + vw], pT[:, c0:512], j == 0, j == n_s - 1)
                    if j == min(1, n_s - 1) and fox_pending:
                        fox_pending.pop()()

                def norm_ops(ob=ob, odd=odd, h=h, t0=t0):
                    po = banks[ob]
                    if not odd:
                        act(sqf[0:65, :], po[0:65, :], AF.Square)
                        mm(banks[7][0:64, :], cvecA[0:65, :], sqf[0:65, :], True, True)
                        prow = slice(0, 64)
                    else:
                        act(sqf[:, :], po[:, :], AF.Square)
                        mm(banks[7][:, :], cvecB[:, :], sqf[:, :], True, True)
                        prow = slice(64, 128)
                    act(lnr[prow, :], banks[7][prow, :], AF.Ln)
                    act(rr[prow, :], lnr[prow, :], AF.Exp, scale=-0.5)
                    stt(ocat[prow, 4 + h // 2, t0:t0 + 512], po[prow, :], foxg[prow, :], rr[prow, :], ALU.mult, ALU.mult)

                fox_pending.append(norm_ops)
        while fox_pending:
            fox_pending.pop()()
        if stop_after == "fox":
            break

        whg = sb(W, 32768, BF16).rearrange("p (k c) -> p k c", k=8)
        dma("pool", whg, win_d.rearrange("(k p) c -> p k c", p=128)[:, :, 0:2048], writes=[whg])
        T0 = sb(W + 32768, 8192, F32)
        T1 = sb(W + 40960, 8192, F32)
        T2 = sb(W + 49152, 8192, F32)
        T3 = sb(W + 57344, 8192, F32)
        qtl = sb(W + 65536, 4096, BF16)
        ktl = sb(W + 69632, 4096, BF16)
        ktok = sb(W + 73728, 4096, BF16).rearrange("p (t k) -> p t k", t=NT)
        vtok = sb(W + 77824, 4096, BF16).rearrange("p (t k) -> p t k", t=NT)
        sgT = sb(W + 81920, 4096, BF16)
        Sbf = sb(W + 86016, 8192, BF16).rearrange("p (c v) -> p c v", c=32)
        U_s = [sb(W + 94208 + 512 * i, 512, F32) for i in range(2)]
        scm_s = [sb(W + 95232 + 256 * i, 256, BF16) for i in range(2)]
        lnr2 = sb(W + 40960, 2048, F32)
        rr2 = sb(W + 40960 + 2048, 2048, F32)
        t1b = sb(W + 40960 + 4096, 2048, F32)
        sqh = sb(W + 40960 + 6144, 1024, BF16)
        for h in range(4):
            hs = slice(h * 128, (h + 1) * 128)
            for (coff, dst, fn_) in ((0, T0, AF.Silu), (1536, sgT, AF.Silu), (512, T1, AF.Sigmoid)):
                for tb in range(4):
                    cs = slice(tb * 512, (tb + 1) * 512)
                    b = next_bank(0, 2)
                    pp = banks[b]
                    for kc in range(8):
                        mm(pp[:, :], whg[:, kc, coff + h * 128:coff + (h + 1) * 128], xnT[:, kc, cs], kc == 0, kc == 7)
                    act(dst[:, cs], pp[:, :], fn_)
            for i4 in range(4):
                b = next_bank(0, 2)
                pp = banks[b]
                for ii in range(4):
                    i = i4 * 4 + ii
                    for kc in range(8):
                        mm(pp[:, ii * 128:(ii + 1) * 128], xnT[:, kc, i * 128:(i + 1) * 128],
                           whg[:, kc, 1024 + h * 128:1024 + (h + 1) * 128], kc == 0, kc == 7)
                cp("dve", vtok[:, i4 * 4:(i4 + 1) * 4, :], pp[:, :].rearrange("p (t k) -> p t k", t=4))
            ts("dve", T1, T1, oml[:, h:h + 1], lbc[:, h:h + 1], ALU.mult, ALU.add)
            act(T2, T1, AF.Ln)
            ts("dve", T1, T1, -1.0, 1.0, ALU.mult, ALU.add)
            A("dve", lambda e, o=T3, a=reset, b_=T2: e.tensor_tensor_scan(o, a, b_, 0.0, ALU.mult, ALU.add),
              [reset, T2], [T3])
            act(T2, T3, AF.Exp)
            tt("dve", qtl, T0, T2, ALU.mult)
            act(T0, T3, AF.Exp, scale=-1.0)
            tt("dve", ktl, T1, T0, ALU.mult)
            for i8 in range(2):
                b = next_bank(0, 2)
                pb = bankbf(b).rearrange("p (t k) -> p t k", t=8)
                for ii in range(8):
                    i = i8 * 8 + ii
                    tr(pb[:, ii, :], ktl[:, i * 128:(i + 1) * 128], ident)
                cp("act", ktok[:, i8 * 8:(i8 + 1) * 8, :], pb)
            for c in range(32):
                i, par = c // 2, c % 2
                if c % 4 == 0:
                    sb_ = next_bank(2, 5)
                dS = banks[sb_][:, (c % 4) * 128:(c % 4 + 1) * 128]
                ps_ = slice(par * 64, par * 64 + 64)
                mm(dS, ktok[ps_, i, :], vtok[ps_, i, :], True, True)
                Uc = U_s[c % 2]
                Up = U_s[(c + 1) % 2]
                if c == 0:
                    memset("pool", Sbf[:, 0, :], 0.0)
                    cp("dve", Uc, dS)
                else:
                    Dp = T2[:, (c - 1) * 64 + 63:(c - 1) * 64 + 64]
                    act(Sbf[:, c, :], Up, AF.Copy, scale=Dp)
                    stt(Uc, Up, Dp, dS, ALU.mult, ALU.add)
            for tb in range(4):
                ob = 5 + (h * 4 + tb) % 2
                po = banks[ob]
                scb = {}

                def emit_sc(ii, tb=tb, scb=scb):
                    i = tb * 4 + ii
                    cs = slice(i * 128, (i + 1) * 128)
                    lb_ = next_bank(2, 5)
                    sc = banks[lb_][:, 0:128]
                    mm(sc, ktl[:, cs], qtl[:, cs], True, True)
                    scm = scm_s[i % 2]
                    tt("dve", scm, sc, maskhg, ALU.mult)
                    scb[ii] = scm

                emit_sc(0)
                for ii in range(4):
                    i = tb * 4 + ii
                    if ii + 1 < 4:
                        emit_sc(ii + 1)
                    scm = scb[ii]
                    oo = po[:, ii * 128:(ii + 1) * 128]
                    mm(oo, vtok[:, i, :], scm, True, False)
                    mm(oo[:, 0:64], Sbf[:, 2 * i, :], qtl[:, i * 128:i * 128 + 64], False, False)
                    mm(oo[:, 64:128], Sbf[:, 2 * i + 1, :], qtl[:, i * 128 + 64:i * 128 + 128], False, True)
                act(sqh, po[:, :], AF.Square)
                mm(banks[7][:, :], cmathg, sqh, True, True)
                act(lnr2, banks[7][:, :], AF.Ln, bias=EPS)
                act(rr2, lnr2, AF.Exp, scale=-0.5)
                stt(t1b, po[:, :], hgg, rr2, ALU.mult, ALU.mult)
                tt("pool", ocat[:, h, tb * 512:(tb + 1) * 512], t1b, sgT[:, tb * 512:(tb + 1) * 512], ALU.mult)
        if stop_after == "hg":
            break

        hb_s = [sb(W + 4096 * i, 4096, F32) for i in range(2)]
        xt2_s = [sb(W + 8192 + 4096 * i, 4096, F32) for i in range(2)]
        junk2 = sb(W + 16384, 2048, BF16)
        hn_s = [sb(W + 18432 + 2048 * i, 2048, BF16) for i in range(2)]
        hnt_s = [sb(W + 22528 + 2048 * i, 2048, BF16).rearrange("p (k t) -> p k t", k=8) for i in range(2)]
        for i in range(NT):
            gi = sq_ * NT + i
            rows = slice(gi * 128, (gi + 1) * 128)
            xt = xt2_s[i % 2]
            hb = hb_s[i % 2]
            if i == 0:
                dma("sp", xt, x_d[sq_, 0:128, :], writes=[xt])
            if i + 1 < NT:
                dma("sp", xt2_s[(i + 1) % 2], x_d[sq_, (i + 1) * 128:(i + 2) * 128, :], writes=[xt2_s[(i + 1) % 2]])
            for half in range(2):
                b = next_bank(0, 2)
                ph = banks[b]
                for fc in range(8):
                    mm(ph[:, :], ocat[:, fc, i * 128:(i + 1) * 128], wout[:, fc, half * 512:(half + 1) * 512], fc == 0, fc == 7)
                tt("dve", hb[:, half * 512:(half + 1) * 512], ph[:, :], xt[:, half * 512:(half + 1) * 512], ALU.add)
            dma("sp", h_scr[rows, :], hb, reads=[hb], writes=[h_scr[rows, :]])
            rstd = rms_rstd(hb, junk2, 32 + 4 * (i % 8))
            hn = hn_s[i % 2]
            stt(hn, hb, rstd, gffn, ALU.mult, ALU.mult)
            dma("sp", hn_scr[rows, :], hn, reads=[hn], writes=[hn_scr[rows, :]])
            b = next_bank(2, 4)
            pb = bankbf(b).rearrange("p (k t) -> p k t", k=8)
            for kc in range(8):
                tr(pb[:, kc, :], hn[:, kc * 128:(kc + 1) * 128], ident)
            hnt = hnt_s[i % 2]
            cp("act", hnt, pb)
            b = next_bank(4, 6)
            pr = banks[b][:, 0:20]
            for kc in range(8):
                mm(pr, hnt[:, kc, :], wr[:, kc, :], kc == 0, kc == 7)
            ro = (i % 2) * 128
            lgt = rt[:, ro:ro + 20]
            gmax = rt[:, ro + 20:ro + 21]
            ngmax = rt[:, ro + 21:ro + 22]
            gm = rt[:, ro + 22:ro + 26]
            eg = rt[:, ro + 26:ro + 30]
            sumg = rt[:, ro + 30:ro + 31]
            pgs = rt[:, ro + 31:ro + 32]
            pen = rt[:, ro + 32:ro + 48]
            elm = rt[:, ro + 48:ro + 64]
            top8 = rt[:, ro + 64:ro + 72]
            nm1 = rt[:, ro + 72:ro + 73]
            selm = rt[:, ro + 73:ro + 89]
            den2 = rt[:, ro + 89:ro + 90]
            ex = rt[:, ro + 91:ro + 107]
            tt("dve", lgt, pr, brt, ALU.add)
            A("dve", lambda e, o=gmax, i_=lgt[:, 0:4]: e.reduce_max(o, i_, AX.X), [lgt[:, 0:4]], [gmax])
            ts("dve", gm, lgt[:, 0:4], gmax, None, ALU.is_equal)
            ts("dve", ngmax, gmax, -1.0, None, ALU.mult)
            act(eg, lgt[:, 0:4], AF.Exp, bias=ngmax, accum=sumg)
            recip(pgs, sumg)
            gm_b = bass.AP(gm.tensor, gm.offset, [list(gm.ap[0]), [1, 4], [0, 4]])
            pen3 = pen.rearrange("p (g j) -> p g j", j=4)
            ts("dve", pen3, gm_b, -1.0, 1e30, ALU.add, ALU.mult)
            tt("dve", elm, lgt[:, 4:20], pen, ALU.add)
            A("dve", lambda e, o=top8, i_=elm: e.max(o, i_), [elm], [top8])
            ts("dve", selm, elm, top8[:, 1:2], None, ALU.is_ge)
            cp("dve", M2b[:, gi * 16:(gi + 1) * 16], selm)
            ts("dve", M1b[:, gi * 16:(gi + 1) * 16], elm, top8[:, 0:1], None, ALU.is_equal)
            ts("dve", nm1, top8[:, 0:1], -1.0, None, ALU.mult)
            act(ex, elm, AF.Exp, bias=nm1)
            stt(ex, ex, 1.0, selm, ALU.mult, ALU.mult, accum=den2)
            recip(den2, den2)
            tt("dve", w1s[:, gi:gi + 1], den2, pgs, ALU.mult)
            tt("dve", w2s[:, gi:gi + 1], pgs, w1s[:, gi:gi + 1], ALU.subtract)

    if stop_after is None:
        NTT16 = NTT * 16
        def f32t(k):
            return sb(XNT + 4096 * k, 4096, F32)[:, 0:NTT16]
        TotS, RS, CI, EX, PP, TMP = [f32t(k) for k in range(6)]
        small2 = sb(XNT + 4096 * 6, 4096, F32)
        ne = small2[:, 0:16]
        ke = small2[:, 16:32]
        cki = small2[:, 32:48]
        base = small2[:, 48:64]
        cmp_ = small2[:, 64:320]
        ejf = small2[:, 320:384]
        cmp2 = sb(XNT + 4096 * 7, 4096, F32)
        pos1f = small2[:, 384:448]
        pos2f = small2[:, 448:512]
        idf0 = small2[:, 512:576]
        idf1 = small2[:, 576:640]
        for half in range(0, NTT16, 512):
            n_ = min(512, NTT16 - half)
            mm(banks[0][:, 0:n_], lstrict, M2b[:, half:half + n_], True, True)
            cp("dve", RS[:, half:half + n_], banks[0][:, 0:n_])
            mm(banks[1][:, 0:n_], ones[:, 0:128], M2b[:, half:half + n_], True, True)
            cp("dve", TotS[:, half:half + n_], banks[1][:, 0:n_])
        TotS3 = TotS.rearrange("p (t e) -> p t e", e=16)
        CI3 = CI.rearrange("p (t e) -> p t e", e=16)
        for e_ in range(16):
            A("dve", lambda e, o=CI3[:, :, e_], a=ones[:, 0:NTT], b_=TotS3[:, :, e_]:
              e.tensor_tensor_scan(o, a, b_, 0.0, ALU.mult, ALU.add), [ones[:, 0:NTT], TotS3[:, :, e_]], [CI3[:, :, e_]])
        tt("dve", EX, CI, TotS, ALU.subtract)
        cp("dve", ne, CI3[:, NTT - 1, :])
        ne_b = bass.AP(ne.tensor, ne.offset, [list(ne.ap[0]), [1, 16], [0, 16]])
        thr_b = bass.AP(thr.tensor, thr.offset, [list(thr.ap[0]), [0, 16], [1, 16]])
        tt("dve", cmp_.rearrange("p (a b) -> p a b", b=16), ne_b, thr_b, ALU.is_gt)
        A("dve", lambda e, o=ke, i_=cmp_.rearrange("p (a b) -> p a b", b=16): e.reduce_sum(o, i_, AX.X),
          [cmp_], [ke])
        A("dve", lambda e, o=cki, a=ones[:, 0:16], b_=ke: e.tensor_tensor_scan(o, a, b_, 0.0, ALU.mult, ALU.add),
          [ones[:, 0:16], ke], [cki])
        tt("dve", base, cki, ke, ALU.subtract)
        ts("dve", base, base, 512.0, None, ALU.mult)
        base_b = bass.AP(base.tensor, base.offset, [list(base.ap[0]), [0, NTT], [1, 16]])
        tt("dve", PP, RS, EX, ALU.add)
        PP3 = PP.rearrange("p (t e) -> p t e", e=16)
        tt("dve", PP3, PP3, base_b, ALU.add)
        tt("dve", TMP, PP, M1b[:, 0:NTT16], ALU.mult)
        A("dve", lambda e, o=pos1f[:, 0:NTT], i_=TMP.rearrange("p (t e) -> p t e", e=16): e.reduce_sum(o, i_, AX.X),
          [TMP], [pos1f[:, 0:NTT]])
        tt("dve", TMP, PP, M2b[:, 0:NTT16], ALU.mult)
        A("dve", lambda e, o=pos2f[:, 0:NTT], i_=TMP.rearrange("p (t e) -> p t e", e=16): e.reduce_sum(o, i_, AX.X),
          [TMP], [pos2f[:, 0:NTT]])
        tt("dve", pos2f[:, 0:NTT], pos2f[:, 0:NTT], pos1f[:, 0:NTT], ALU.subtract)
        cp("dve", pos1i[:, 0:NTT], pos1f[:, 0:NTT])
        cp("dve", pos2i[:, 0:NTT], pos2f[:, 0:NTT])
        cki_b = bass.AP(cki.tensor, cki.offset, [list(cki.ap[0]), [0, NSLOT], [1, 16]])
        jt_b = bass.AP(jt.tensor, jt.offset, [list(jt.ap[0]), [1, NSLOT], [0, 16]])
        c23 = cmp2[:, 0:NSLOT * 16].rearrange("p (j e) -> p j e", e=16)
        tt("dve", c23, cki_b, jt_b, ALU.is_le)
        A("dve", lambda e, o=ejf[:, 0:NSLOT], i_=c23: e.reduce_sum(o, i_, AX.X), [cmp2[:, 0:NSLOT * 16]], [ejf[:, 0:NSLOT]])
        ts("dve", ejf[:, 0:NSLOT], ejf[:, 0:NSLOT], 15.0, None, ALU.min)
        ts("dve", idf0[:, 0:NSLOT], ejf[:, 0:NSLOT], 256.0, pcol2, ALU.mult, ALU.add)
        ts("dve", idf1[:, 0:NSLOT], idf0[:, 0:NSLOT], 1.0, None, ALU.add)
        cp("dve", idxW0[:, 0:NSLOT], idf0[:, 0:NSLOT])
        cp("dve", idxW1[:, 0:NSLOT], idf1[:, 0:NSLOT])

        def idma(out, in_, out_off=None, in_off=None, reads=(), writes=()):
            def fn(e, o=out, i=in_, oo=out_off, io=in_off):
                return e.indirect_dma_start(
                    out=o, out_offset=(bass.IndirectOffsetOnAxis(ap=oo, axis=0) if oo is not None else None),
                    in_=i, in_offset=(bass.IndirectOffsetOnAxis(ap=io, axis=0) if io is not None else None))
            A("pool", fn, reads, writes, dma=True)

        hnc_s = [sb(XNT + 131072 + 2048 * i, 2048, BF16) for i in range(8)]
        for gi in range(NTT):
            hnc = hnc_s[gi % 8]
            rows = slice(gi * 128, (gi + 1) * 128)
            dma("sp", hnc, hn_scr[rows, :], reads=[hn_scr[rows, :]], writes=[hnc])
            idma(so_scr[:, :], hnc, out_off=pos1i[:, gi:gi + 1], reads=[hnc, pos1i[:, gi:gi + 1]], writes=[so_scr[:, :]])
            idma(so_scr[:, :], hnc, out_off=pos2i[:, gi:gi + 1], reads=[hnc, pos2i[:, gi:gi + 1]], writes=[so_scr[:, :]])

        D0 = XNT

        def d_views(j):
            wo = D0 + 24576 * (j % 2)
            wg = sb(wo, 8192, BF16)
            wu = sb(wo + 8192, 8192, BF16)
            wd = sb(wo + 16384, 8192, BF16)
            xtok = sb(D0 + 49152 + 8192 * (j % 2), 8192, BF16).rearrange("p (a c) -> p a c", a=4)
            return wg, wu, wd, xtok

        def d_load(j):
            wg, wu, wd, xtok = d_views(j)
            srows = so_scr[j * 512:(j + 1) * 512, :]
            dma("sp", xtok, srows.rearrange("(a p) c -> p a c", p=128), reads=[srows], writes=[xtok])
            for (dst, src) in ((wg, wg_d), (wu, wu_d), (wd, wd_d)):
                idma(dst[:, 0:2048], src[:, :], in_off=idxW0[:, j:j + 1], reads=[idxW0[:, j:j + 1]], writes=[dst[:, 0:2048]])
                idma(dst[:, 2048:4096], src[:, :], in_off=idxW1[:, j:j + 1], reads=[idxW1[:, j:j + 1]], writes=[dst[:, 2048:4096]])

        d_load(0)
        for j in range(NSLOT):
            if j + 1 < NSLOT:
                d_load(j + 1)
            wg, wu, wd, xtok = d_views(j)
            wg3 = wg.rearrange("p (k c) -> p k c", k=8)
            wu3 = wu.rearrange("p (k c) -> p k c", k=8)
            wd3 = wd.rearrange("p (k c) -> p k c", k=4)
            hsT = sb(D0 + 65536 + 8192 * (j % 2), 8192, BF16).rearrange("p (k t) -> p k t", k=8)
            hact = sb(D0 + 81920 + 4096 * (j % 2), 4096, BF16).rearrange("p (c t) -> p c t", c=4)
            ybuf = sb(D0 + 94208 + 16384 * (j % 2), 16384, F32).rearrange("p (a c) -> p a c", a=4)
            for a in range(4):
                b = 6 + a % 2
                pb = bankbf(b).rearrange("p (k t) -> p k t", k=8)
                for kc in range(8):
                    tr(pb[:, kc, :], xtok[:, a, kc * 128:(kc + 1) * 128], ident)
                cp("act" if a % 2 else "dve", hsT[:, :, a * 128:(a + 1) * 128], pb)
            for hc in range(4):
                gb = hc % 2
                ub = 2 + hc % 2
                for kc in range(8):
                    mm(banks[gb][:, :], wg3[:, kc, hc * 128:(hc + 1) * 128], hsT[:, kc, :], kc == 0, kc == 7)
                for kc in range(8):
                    mm(banks[ub][:, :], wu3[:, kc, hc * 128:(hc + 1) * 128], hsT[:, kc, :], kc == 0, kc == 7)
                sgt = sb(D0 + 90112 + 2048 * (hc % 2), 2048, F32)
                act(sgt, banks[gb][:, :], AF.Silu)
                tt("dve", hact[:, hc, :], sgt, banks[ub][:, :], ALU.mult)
            for a in range(4):
                for half in range(2):
                    yb = 4 + (a * 2 + half) % 2
                    for hc in range(4):
                        mm(banks[yb][:, :], hact[:, hc, a * 128:(a + 1) * 128], wd3[:, hc, half * 512:(half + 1) * 512], hc == 0, hc == 3)
                    cp("act" if half else "dve", ybuf[:, a, half * 512:(half + 1) * 512], banks[yb][:, :])
            yrows = y_scr[j * 512:(j + 1) * 512, :]
            dma("sp", yrows.rearrange("(a p) c -> p a c", p=128), ybuf, reads=[ybuf], writes=[yrows])

        gfin = sb(XNT + 151552, 4096, F32)
        dma("sp", gfin, gfin_d.partition_broadcast(128), writes=[gfin])
        junk3 = sb(XNT + 155648, 2048, BF16)
        def e_views(gi):
            eo = XNT + 12288 * (gi % 4)
            return sb(eo, 4096, F32), sb(eo + 4096, 4096, F32), sb(eo + 8192, 4096, F32)

        def e_load(gi):
            y1, y2, hb = e_views(gi)
            rows = slice(gi * 128, (gi + 1) * 128)
            idma(y1, y_scr[:, :], in_off=pos1i[:, gi:gi + 1], reads=[y_scr[:, :], pos1i[:, gi:gi + 1]], writes=[y1])
            idma(y2, y_scr[:, :], in_off=pos2i[:, gi:gi + 1], reads=[y_scr[:, :], pos2i[:, gi:gi + 1]], writes=[y2])
            dma("sp", hb, h_scr[rows, :], reads=[h_scr[rows, :]], writes=[hb])

        for g0 in range(min(3, NTT)):
            e_load(g0)
        for gi in range(NTT):
            sq_, i = gi // NT, gi % NT
            if gi + 3 < NTT:
                e_load(gi + 3)
            y1, y2, hb = e_views(gi)
            stt(hb, y1, w1s[:, gi:gi + 1], hb, ALU.mult, ALU.add)
            stt(hb, y2, w2s[:, gi:gi + 1], hb, ALU.mult, ALU.add)
            rstd = rms_rstd(hb, junk3, 64 + 4 * (gi % 8))
            stt(hb, hb, rstd, gfin, ALU.mult, ALU.mult)
            dma("sp", out_d[sq_, i * 128:(i + 1) * 128, :], hb, reads=[hb])

    sch.prepare()
    sems = {k: es.enter_context(nc.semaphore(f"s_{k}")) for k in ("pe", "act", "dve", "pool")}
    dsems = {}
    for q in ("sp", "pool", "act"):
        for k in range(sch.n_dma_sems):
            dsems[(q, k)] = es.enter_context(nc.semaphore(f"d_{q}{k}"))
    with nc.Block() as block:
        @block.tensor
        def _(eng):
            sch.emit_engine("pe", eng, sems, dsems)

        @block.scalar
        def _(eng):
            sch.emit_engine("act", eng, sems, dsems)

        @block.vector
        def _(eng):
            sch.emit_engine("dve", eng, sems, dsems)

        @block.gpsimd
        def _(eng):
            sch.emit_engine("pool", eng, sems, dsems)

        @block.sync
        def _(eng):
            sch.emit_engine("sp", eng, sems, dsems)
    es.close()
    return nc, sch


_CACHE = {}


def _get_nc(nseq):
    if nseq not in _CACHE:
        _CACHE[nseq] = build(nseq, Sched, F32, BF16, U8)[0]
    return _CACHE[nseq]


def kernel(x, attn_norm, w_in, hg_lb_logits, hg_norm, fox_f_bias, fox_norm, w_out, ffn_norm,
           w_group, b_group, w_expert, b_expert, w_gate, w_up, w_down, final_norm):
    f = lambda a: np.ascontiguousarray(np.asarray(a, dtype=np.float32))
    x = f(x)
    n_cores = 8
    B = x.shape[0]
    nseq = B // n_cores
    shared = {
        "w_in": f(w_in)[0], "w_out": f(w_out)[0],
        "w_gate": np.ascontiguousarray(f(w_gate)[0].reshape(16, 8, 128, 512).transpose(0, 2, 1, 3)).reshape(16 * 256, 2048),
        "w_up": np.ascontiguousarray(f(w_up)[0].reshape(16, 8, 128, 512).transpose(0, 2, 1, 3)).reshape(16 * 256, 2048),
        "w_down": np.ascontiguousarray(f(w_down)[0].reshape(16, 4, 128, 1024).transpose(0, 2, 1, 3)).reshape(16 * 256, 2048),
        "w_router": np.ascontiguousarray(np.concatenate([f(w_group)[0], f(w_expert)[0]], axis=1)),
        "b_router": np.ascontiguousarray(np.concatenate([f(b_group)[0], f(b_expert)[0]])[None, :]),
        "attn_norm": f(attn_norm), "ffn_norm": f(ffn_norm), "final_norm": f(final_norm).reshape(1, -1),
        "hg_lb_logits": f(hg_lb_logits), "hg_norm": f(hg_norm), "fox_f_bias": f(fox_f_bias),
        "fox_norm": f(fox_norm),
    }
    shared.update(make_consts())
    in_maps = []
    for c in range(n_cores):
        m = dict(shared)
        m["x"] = np.ascontiguousarray(x[c * nseq:(c + 1) * nseq])
        in_maps.append(m)
    nc = _get_nc(nseq)
    res = run_bass_kernel_spmd(nc, in_maps, core_ids=list(range(n_cores)))
    return np.concatenate([r["out"] for r in res.results], axis=0).astype(np.float32)
```

```python
import contextlib
import numpy as np
import ml_dtypes
import concourse.bass as bass
import concourse.mybir as mybir

F32 = mybir.dt.float32
BF16 = mybir.dt.bfloat16
U8 = mybir.dt.uint8
_ES = {F32: 4, BF16: 2, U8: 1, mybir.dt.int32: 4, mybir.dt.uint32: 4, mybir.dt.uint16: 2}


class Op:
    __slots__ = ("eng", "fn", "deps", "cdeps", "seq", "ms", "needed", "dma", "dsem", "dval", "dprev", "tag")

    def __init__(self, eng, fn, dma, tag):
        self.eng = eng
        self.fn = fn
        self.deps = set()
        self.cdeps = {}
        self.seq = 0
        self.ms = None
        self.needed = False
        self.dma = dma
        self.dsem = None
        self.dval = None
        self.dprev = 0
        self.tag = tag


class Sched:
    def __init__(self, n_dma_sems=8, same_engine_sync=True):
        self.ops = []
        self.recs = {}
        self.n_dma_sems = n_dma_sems
        self.same_engine_sync = same_engine_sync
        self.dram_track = set()
        self.whole = {}

    def _regions(self, ap):
        t = ap.tensor
        cls = type(t).__name__
        if cls.startswith("DRam"):
            name = ap.name
            if name not in self.dram_track:
                return None
            es = _ES[ap.dtype]
            pat = ap.ap
            off = int(ap.offset)
            hi = off + sum((n - 1) * abs(st) for st, n in pat) + 1
            return name, False, 0, 1, [(off * es, hi * es)]
        psum = cls.startswith("PSum") or cls.startswith("Psum")
        name = ap.name
        if psum:
            return name, True, 0, 128, [(0, 1 << 20)]
        es = _ES[ap.dtype]
        pat = ap.ap
        off = int(ap.offset)
        pstride, pcnt = pat[0]
        p0 = off // pstride
        c0 = off % pstride
        free = list(pat[1:])
        if not free:
            return name, False, p0, p0 + pcnt, [(c0 * es, (c0 + 1) * es)]
        ls, ln = free[-1]
        run = (ln - 1) * abs(ls) + 1
        outer = free[:-1]
        nout = 1
        for s, n in outer:
            nout *= n
        ivs = []
        if nout <= 64:
            idx = [0] * len(outer)
            while True:
                st = c0 + sum(i * s for i, (s, n) in zip(idx, outer))
                ivs.append((st * es, (st + run) * es))
                k = len(outer) - 1
                while k >= 0:
                    idx[k] += 1
                    if idx[k] < outer[k][1]:
                        break
                    idx[k] = 0
                    k -= 1
                if k < 0:
                    break
        else:
            hi = c0 + sum((n - 1) * abs(s) for s, n in outer) + run
            ivs.append((c0 * es, hi * es))
        ivs.sort()
        out = [ivs[0]]
        for a, b in ivs[1:]:
            if a <= out[-1][1]:
                out[-1] = (out[-1][0], max(out[-1][1], b))
            else:
                out.append((a, b))
        return name, False, p0, p0 + pcnt, out

    def _access(self, op, ap, is_write):
        r = self._regions(ap)
        if r is None:
            return
        name, psum, p0, p1, ivs = r
        tab = self.recs.setdefault(name, {})
        w = is_write or psum
        SH = 20 if name in self.dram_track else 11
        for (b0, b1) in ivs:
            newrec = (p0, p1, b0, b1, op, w)
            for bk in range(b0 >> SH, ((b1 - 1) >> SH) + 1):
                lst = tab.get(bk)
                if lst is None:
                    tab[bk] = [newrec]
                    continue
                keep = []
                for rec in lst:
                    rp0, rp1, rb0, rb1, rop, rw = rec
                    ov = rp0 < p1 and p0 < rp1 and rb0 < b1 and b0 < rb1
                    if ov and (rw or w) and rop is not op:
                        if rop.dma:
                            op.deps.add(rop)
                        else:
                            c = op.cdeps.get(rop.eng)
                            if c is None or c.seq < rop.seq:
                                op.cdeps[rop.eng] = rop
                    if ov and w and rp0 >= p0 and rp1 <= p1 and rb0 >= b0 and rb1 <= b1:
                        continue
                    if (not w) and (not rw) and rop.eng == op.eng and not rop.dma and not op.dma \
                            and rp0 == p0 and rp1 == p1 and rb0 == b0 and rb1 == b1:
                        continue
                    keep.append(rec)
                keep.append(newrec)
                tab[bk] = keep

    def add(self, eng, fn, reads=(), writes=(), dma=False, tag=None):
        op = Op(eng, fn, dma, tag)
        op.seq = len(self.ops)
        for ap in reads:
            self._access(op, ap, False)
        for ap in writes:
            self._access(op, ap, True)
        op.deps.update(op.cdeps.values())
        op.cdeps = None
        self.ops.append(op)
        return op

    def prepare(self):
        for op in self.ops:
            for d in op.deps:
                if d.dma:
                    continue
                if d.eng == op.eng and (d.eng == "pe" or not self.same_engine_sync) and not op.dma:
                    continue
                d.needed = True
        cnt = {}
        dcnt = {}
        dslot = {}
        for op in self.ops:
            if op.dma:
                k = dslot.get(op.eng, 0)
                dslot[op.eng] = k + 1
                slot = (op.eng, k % self.n_dma_sems)
                prev = dcnt.get(slot, 0)
                op.dsem = slot
                op.dprev = prev
                op.dval = prev + 16
                dcnt[slot] = op.dval
            elif op.needed:
                cnt[op.eng] = cnt.get(op.eng, 0) + 1
                op.ms = cnt[op.eng]
        self.final_dma = dcnt
        self.ms_total = cnt

    def emit_engine(self, eng_name, eng, sems, dsems):
        waited = {}

        def need(sem_key, sem, val):
            if val <= 0:
                return
            if waited.get(sem_key, 0) >= val:
                return
            waited[sem_key] = val
            eng.wait_ge(sem, val)

        n = 0
        for op in self.ops:
            if op.eng != eng_name:
                continue
            for d in op.deps:
                if d.dma:
                    need(d.dsem, dsems[d.dsem], d.dval)
                else:
                    if d.eng == eng_name and not op.dma and (eng_name == "pe" or not self.same_engine_sync):
                        continue
                    need(d.eng, sems[d.eng], d.ms)
            if op.dma:
                need(op.dsem, dsems[op.dsem], op.dprev)
            ins = op.fn(eng)
            if op.dma:
                ins.then_inc(dsems[op.dsem], 16)
            elif op.needed:
                ins.then_inc(sems[eng_name], 1)
            n += 1
        for slot, val in self.final_dma.items():
            if slot[0] == eng_name:
                need(slot, dsems[slot], val)
        return n


from concourse.bass_utils import run_bass_kernel_spmd

AF = mybir.ActivationFunctionType
ALU = mybir.AluOpType
AX = mybir.AxisListType
I32 = mybir.dt.int32

D = 1024
S = 2048
NT = S // 128
HGW = 512
FOXW = 512
INC = 3592
NE = 16
EH = 512
EPS = 1e-6

ARENA = 212000
PERS = 208000
C_IDENT = 0
C_MASKFOX = 256
C_MASKHG = 512
C_CMATHG = 768
C_CVECA = 1024
C_CVECB = 1152
C_SEL = 1408
C_ONES = 3648
C_RESET = 7744
C_GATTN = 11840
C_GFFN = 15936
C_GFIN = 20032
C_SMALL = 24128
C_WOUT = 24640
C_WR = 41024
C_STAT = 41344
C_GATES = 42368
C_RT = 43392
XNT = 44544
OCAT = XNT + 32768
W = OCAT + 32768
WSIZE = PERS - W
TT = W + 65536


def make_consts():
    bf = ml_dtypes.bfloat16
    ident = np.eye(128, dtype=np.float32).astype(bf)
    s_ = np.arange(128)[:, None]
    t_ = np.arange(128)[None, :]
    maskfox = np.where(t_ >= s_, 0.0, -30000.0).astype(np.float32).astype(bf)
    maskhg = ((t_ >= s_) & ((t_ // 64) == (s_ // 64))).astype(np.float32).astype(bf)
    cvecA = np.zeros((65, 64), np.float32)
    cvecA[0:64, :] = 1.0 / 64
    cvecA[64, :] = EPS
    cvecB = np.zeros((128, 128), np.float32)
    cvecB[0, 64:128] = EPS
    cvecB[64:128, 64:128] = 1.0 / 64
    sel = np.zeros((97, 16, 70), np.float32)
    for h in range(8):
        sel[h, h, 64] = 8.0
        sel[32 + h, h, 65] = 8.0
        sel[64 + h, h, 66] = 8.0
        sel[96, h, 67:70] = 8.0
        sel[96, 8 + h, 64:67] = 1.0
        sel[h, 8 + h, 67] = -1.0
        sel[32 + h, 8 + h, 68] = -1.0
        sel[64 + h, 8 + h, 69] = -1.0
    lstrict = (s_ < t_).astype(np.float32).astype(bf)
    thr = np.tile((512.0 * np.arange(16, dtype=np.float32))[None, :], (128, 1))
    jt = np.tile(np.arange(64, dtype=np.float32)[None, :], (128, 1))
    pcol2 = (2.0 * np.arange(128, dtype=np.float32)).reshape(128, 1)
    return {
        "c_lstrict": lstrict, "c_thr": thr, "c_jt": jt, "c_pcol2": pcol2,
        "c_ident": ident,
        "c_maskfox": maskfox,
        "c_maskhg": maskhg,
        "c_cveca": cvecA.astype(bf),
        "c_cvecb": cvecB.astype(bf),
        "c_sel": sel.reshape(97, 16 * 70).astype(bf),
    }


def build(nseq, Sched, F32, BF16, U8, stop_after=None, dbg=False):
    nc = bass.Bass("TRN2", target_bir_lowering=False)
    dt_ = nc.dram_tensor
    x_d = dt_("x", [nseq, S, D], F32, kind="ExternalInput").ap()
    win_d = dt_("w_in", [D, INC], F32, kind="ExternalInput").ap()
    wout_d = dt_("w_out", [D, D], F32, kind="ExternalInput").ap()
    wg_d = dt_("w_gate", [NE * 256, 2048], F32, kind="ExternalInput").ap()
    wu_d = dt_("w_up", [NE * 256, 2048], F32, kind="ExternalInput").ap()
    wd_d = dt_("w_down", [NE * 256, 2048], F32, kind="ExternalInput").ap()
    cls_d = dt_("c_lstrict", [128, 128], BF16, kind="ExternalInput").ap()
    cthr_d = dt_("c_thr", [128, 16], F32, kind="ExternalInput").ap()
    cjt_d = dt_("c_jt", [128, 64], F32, kind="ExternalInput").ap()
    cp2_d = dt_("c_pcol2", [128, 1], F32, kind="ExternalInput").ap()
    NTT = nseq * NT
    NSLOT = nseq * 8 + 16
    NROW = NSLOT * 512
    h_scr = dt_("h_scr", [NTT * 128, D], F32, kind="Internal").ap()
    hn_scr = dt_("hn_scr", [NTT * 128, D], BF16, kind="Internal").ap()
    so_scr = dt_("so_scr", [NROW, D], BF16, kind="Internal").ap()
    y_scr = dt_("y_scr", [NROW, D], F32, kind="Internal").ap()
    wr_d = dt_("w_router", [D, 20], F32, kind="ExternalInput").ap()
    br_d = dt_("b_router", [1, 20], F32, kind="ExternalInput").ap()
    gattn_d = dt_("attn_norm", [1, D], F32, kind="ExternalInput").ap()
    gffn_d = dt_("ffn_norm", [1, D], F32, kind="ExternalInput").ap()
    gfin_d = dt_("final_norm", [1, D], F32, kind="ExternalInput").ap()
    lbl_d = dt_("hg_lb_logits", [2, HGW], F32, kind="ExternalInput").ap()
    hgn_d = dt_("hg_norm", [1, 128], F32, kind="ExternalInput").ap()
    fb_d = dt_("fox_f_bias", [1, 8], F32, kind="ExternalInput").ap()
    fxn_d = dt_("fox_norm", [1, 64], F32, kind="ExternalInput").ap()
    ci_d = dt_("c_ident", [128, 128], BF16, kind="ExternalInput").ap()
    cmf_d = dt_("c_maskfox", [128, 128], BF16, kind="ExternalInput").ap()
    cmh_d = dt_("c_maskhg", [128, 128], BF16, kind="ExternalInput").ap()
    cva_d = dt_("c_cveca", [65, 64], BF16, kind="ExternalInput").ap()
    cvb_d = dt_("c_cvecb", [128, 128], BF16, kind="ExternalInput").ap()
    csel_d = dt_("c_sel", [97, 16 * 70], BF16, kind="ExternalInput").ap()
    out_d = dt_("out", [nseq, S, D], F32, kind="ExternalOutput").ap()
    dbg_d = None
    if dbg:
        dbg_d = dt_("dbg", [S, D], F32, kind="ExternalOutput").ap()

    sch = Sched()
    sch.dram_track.update(["h_scr", "hn_scr", "so_scr", "y_scr"])
    es = contextlib.ExitStack()
    arena = es.enter_context(nc.sbuf_tensor("arena", [128, ARENA], U8))
    banks = [es.enter_context(nc.psum_tensor(f"bank{i}", [128, 512], F32)) for i in range(8)]

    def sb(off, nbytes, dt):
        return arena[:, off:off + nbytes].bitcast(dt)

    def bankbf(i):
        return banks[i][:, :].bitcast(BF16)

    ident = sb(C_IDENT, 256, BF16)
    maskfox = sb(C_MASKFOX, 256, BF16)
    maskhg = sb(C_MASKHG, 256, BF16)
    cmathg = sb(C_CMATHG, 256, BF16)
    cvecA = sb(C_CVECA, 128, BF16)
    cvecB = sb(C_CVECB, 256, BF16)
    sel = sb(C_SEL, 2240, BF16).rearrange("p (a b) -> p a b", a=16)
    ones = sb(C_ONES, 4096, BF16)
    reset = sb(C_RESET, 4096, BF16)
    gattn = sb(C_GATTN, 4096, F32)
    gffn = sb(C_GFFN, 4096, F32)
    M2b = sb(C_GFIN, 2048, BF16)
    M1b = sb(C_GFIN + 2048, 2048, BF16)
    small = sb(C_SMALL, 512, F32)
    lbc = small[:, 0:4]
    oml = small[:, 4:8]
    hgg = small[:, 8:9]
    foxg = small[:, 9:10]
    negb = small[:, 10:11]
    l0 = small[:, 11:15]
    l1 = small[:, 15:19]
    ltmp = small[:, 19:23]
    brt = small[:, 24:44]
    wout = sb(C_WOUT, 16384, BF16).rearrange("p (k c) -> p k c", k=8)
    wr = sb(C_WR, 320, BF16).rearrange("p (k c) -> p k c", k=8)
    stat = sb(C_STAT, 1024, F32)
    w1s = sb(C_GATES, 256, F32)
    w2s = sb(C_GATES + 256, 256, F32)
    pos1i = sb(C_GATES + 512, 256, I32)
    pos2i = sb(C_GATES + 768, 256, I32)
    lstrict = sb(PERS, 256, BF16)
    thr = sb(PERS + 256, 64, F32)
    jt = sb(PERS + 320, 256, F32)
    pcol2 = sb(PERS + 576, 4, F32)
    idxW0 = sb(PERS + 640, 256, I32)
    idxW1 = sb(PERS + 896, 256, I32)
    rt = sb(C_RT, 1024, F32)
    xnT = sb(XNT, 32768, BF16).rearrange("p (k t) -> p k t", k=8)
    ocat = sb(OCAT, 32768, BF16).rearrange("p (k t) -> p k t", k=8)

    def A(eng, fn, reads=(), writes=(), dma=False, tag=None):
        return sch.add(eng, fn, reads, writes, dma, tag)

    def dma(q, out, in_, reads=(), writes=(), **kw):
        A(q, lambda e, o=out, i=in_, kw=kw: e.dma_start(out=o, in_=i, **kw), reads, writes, dma=True)

    def mm(out, lhsT, rhs, start, stop):
        A("pe", lambda e, o=out, l=lhsT, r=rhs, s0=start, s1=stop: e.matmul(o, l, r, start=s0, stop=s1),
          [lhsT, rhs], [out])

    def tr(out, in_, idn):
        A("pe", lambda e, o=out, i=in_, d=idn: e.transpose(o, i, d), [in_, idn], [out])

    def act(out, in_, func, bias=None, scale=None, accum=None):
        rd = [in_]
        kw = {}
        if bias is not None:
            kw["bias"] = bias
            if not isinstance(bias, float):
                rd.append(bias)
        if scale is not None:
            kw["scale"] = scale
            if not isinstance(scale, float):
                rd.append(scale)
        wr_ = [out]
        if accum is not None:
            kw["accum_out"] = accum
            wr_.append(accum)
        A("act", lambda e, o=out, i=in_, f=func, kw=kw: e.activation(o, i, f, **kw), rd, wr_)

    def ts(eng, out, in0, s1, s2, op0, op1=None, accum=None):
        rd = [in0]
        for s_ in (s1, s2):
            if s_ is not None and not isinstance(s_, (float, int)):
                rd.append(s_)
        wr_ = [out]
        kw = {}
        if accum is not None:
            kw["accum_out"] = accum
            wr_.append(accum)
        if op1 is None:
            A(eng, lambda e, o=out, i=in0, a=s1, p=op0, kw=kw: e.tensor_scalar(o, i, a, None, p, **kw), rd, wr_)
        else:
            A(eng, lambda e, o=out, i=in0, a=s1, b=s2, p=op0, q=op1, kw=kw: e.tensor_scalar(o, i, a, b, p, q, **kw), rd, wr_)

    def tt(eng, out, in0, in1, op):
        A(eng, lambda e, o=out, a=in0, b=in1, p=op: e.tensor_tensor(o, a, b, p), [in0, in1], [out])

    def stt(out, in0, scalar, in1, op0, op1, accum=None):
        rd = [in0, in1]
        if not isinstance(scalar, (float, int)):
            rd.append(scalar)
        wr_ = [out]
        kw = {}
        if accum is not None:
            kw["accum_out"] = accum
            wr_.append(accum)
        A("dve", lambda e, o=out, a=in0, s_=scalar, b=in1, p=op0, q=op1, kw=kw:
          e.scalar_tensor_tensor(o, a, s_, b, p, q, **kw), rd, wr_)

    def cp(eng, out, in_):
        if eng == "act":
            A(eng, lambda e, o=out, i=in_: e.copy(o, i), [in_], [out])
        else:
            A(eng, lambda e, o=out, i=in_: e.tensor_copy(o, i), [in_], [out])

    def memset(eng, out, val):
        A(eng, lambda e, o=out, v=val: e.memset(o, v), [], [out])

    def recip(out, in_):
        A("dve", lambda e, o=out, i=in_: e.reciprocal(o, i), [in_], [out])

    dma("sp", ident, ci_d, writes=[ident])
    dma("sp", maskfox, cmf_d, writes=[maskfox])
    dma("sp", maskhg, cmh_d, writes=[maskhg])
    dma("sp", cvecA[0:65, :], cva_d, writes=[cvecA[0:65, :]])
    dma("sp", cvecB, cvb_d, writes=[cvecB])
    dma("sp", sel[0:97, :, :], csel_d.rearrange("p (a b) -> p a b", a=16), writes=[sel[0:97, :, :]])
    dma("sp", gattn, gattn_d.partition_broadcast(128), writes=[gattn])
    dma("sp", gffn, gffn_d.partition_broadcast(128), writes=[gffn])
    dma("sp", lstrict, cls_d, writes=[lstrict])
    dma("sp", thr, cthr_d, writes=[thr])
    dma("sp", jt, cjt_d, writes=[jt])
    dma("sp", pcol2, cp2_d, writes=[pcol2])
    dma("sp", brt, br_d.partition_broadcast(128), writes=[brt])
    nonc = dict(allow_slow_non_contiguous=True)
    dma("sp", l0, lbl_d[0:1, :].rearrange("o (h p) -> p (o h)", p=128), writes=[l0], **nonc)
    dma("sp", l1, lbl_d[1:2, :].rearrange("o (h p) -> p (o h)", p=128), writes=[l1], **nonc)
    dma("sp", hgg, hgn_d.rearrange("o p -> p o"), writes=[hgg], **nonc)
    dma("sp", foxg[0:64, :], fxn_d.rearrange("o p -> p o"), writes=[foxg[0:64, :]], **nonc)
    dma("sp", foxg[64:128, :], fxn_d.rearrange("o p -> p o"), writes=[foxg[64:128, :]], **nonc)
    memset("dve", negb, 0.0)
    for g in range(3):
        dma("sp", negb[32 * g:32 * g + 8, :], fb_d.rearrange("o p -> p o"), writes=[negb[32 * g:32 * g + 8, :]], **nonc)
    dma("pool", wout, wout_d.rearrange("(k p) c -> p k c", p=128), writes=[wout])
    dma("pool", wr, wr_d.rearrange("(k p) c -> p k c", p=128), writes=[wr])
    memset("dve", ones, 1.0)
    memset("dve", reset, 1.0)
    memset("dve", reset.rearrange("p (c j) -> p c j", j=64)[:, :, 0:1], 0.0)
    memset("dve", cmathg, 1.0 / 128)
    tt("dve", ltmp, l1, l0, ALU.subtract)
    act(ltmp, ltmp, AF.Exp)
    ts("dve", ltmp, ltmp, 1.0, None, ALU.add)
    recip(lbc, ltmp)
    ts("dve", oml, lbc, -1.0, 1.0, ALU.mult, ALU.add)

    def rms_rstd(src, junk, col):
        ssq = stat[:, col:col + 1]
        var = stat[:, col + 1:col + 2]
        lnv = stat[:, col + 2:col + 3]
        rstd = stat[:, col + 3:col + 4]
        act(junk, src, AF.Square, accum=ssq)
        ts("dve", var, ssq, 1.0 / D, EPS, ALU.mult, ALU.add)
        act(lnv, var, AF.Ln)
        act(rstd, lnv, AF.Exp, scale=-0.5)
        return rstd

    pbank = [0]

    def next_bank(lo, hi):
        b = lo + (pbank[0] % (hi - lo))
        pbank[0] += 1
        return b

    for sq_ in range(nseq):
        xt_s = [sb(W + 77056 + 4096 * i, 4096, F32) for i in range(2)]
        junk = sb(W + 85248, 2048, BF16)
        xs_s = [sb(W + 87296 + 2048 * i, 2048, BF16) for i in range(2)]
        wfox = sb(W, 24704, BF16).rearrange("p (k c) -> p k c", k=8)
        dma("pool", wfox, win_d.rearrange("(k p) c -> p k c", p=128)[:, :, 2048:INC], writes=[wfox])
        for i in range(NT):
            xt = xt_s[i % 2]
            xs = xs_s[i % 2]
            dma("sp", xt, x_d[sq_, i * 128:(i + 1) * 128, :], writes=[xt])
            rstd = rms_rstd(xt, junk, 4 * (i % 8))
            stt(xs, xt, rstd, gattn, ALU.mult, ALU.mult)
            b = next_bank(0, 2)
            pb = bankbf(b).rearrange("p (k t) -> p k t", k=8)
            for kc in range(8):
                tr(pb[:, kc, :], xs[:, kc * 128:(kc + 1) * 128], ident)
            cp("act" if i % 2 else "dve", xnT[:, :, i * 128:(i + 1) * 128], pb)
        if stop_after == "prep":
            break

        vaug = sb(W + 24704, 24704, BF16).rearrange("p (t c) -> p t c", t=NT)
        qa_s = [sb(W + 49408 + 4096 * i, 4096, BF16) for i in range(2)]
        ka_s = [sb(W + 57600 + 4096 * i, 4096, BF16) for i in range(2)]
        parts = sb(W + 65792, 4096, BF16)
        pT_s = [sb(W + 69888 + 1024 * i, 1024, BF16) for i in range(2)] + [sb(W + 94592, 1024, BF16)]
        sqf = sb(W + 71936, 1024, BF16)
        lnr = sb(W + 72960, 2048, F32)
        rr = sb(W + 75008, 2048, F32)
        fr0 = sb(W + 77056, 8192, F32)
        fr1 = sb(W + 85248, 8192, F32)

        def vcol(h):
            return (h // 2) * 193 + (0 if h % 2 == 0 else 65)

        wf3 = sb(W + 93440, 1152, BF16).rearrange("p (k c) -> p k c", k=8)
        hiT = sb(W + 69888, 4096, BF16)
        memset("dve", parts, 1.0)
        memset("pool", wf3, 0.0)
        memset("pool", vaug, 0.0)
        for h in range(8):
            c1 = vcol(h) + (64 if h % 2 == 0 else 0)
            memset("dve", vaug[:, :, c1:c1 + 1], 1.0)
        for g in range(3):
            cp("dve", wf3[:, :, 32 * g:32 * g + 8], wfox[:, :, 1536:1544])
        for tb in range(4):
            b = next_bank(0, 2)
            pf = banks[b][0:72, :]
            for kc in range(8):
                mm(pf, wf3[:, kc, :], xnT[:, kc, tb * 512:(tb + 1) * 512], kc == 0, kc == 7)
            act(fr0[0:72, tb * 512:(tb + 1) * 512], pf, AF.Identity, bias=negb[0:72, :])
        f0 = fr0[0:72, :]
        f1 = fr1[0:72, :]
        h72 = hiT[0:72, :]
        chain = [
            lambda: ts("dve", f1, f0, -80.0, None, ALU.max),
            lambda: act(f1, f1, AF.Exp, scale=-1.0),
            lambda: act(f1, f1, AF.Ln, bias=1.0),
            lambda: ts("dve", f0, f1, -1.0, None, ALU.mult),
            lambda: A("dve", lambda e, o=f1, a=ones[0:72, :], b_=f0: e.tensor_tensor_scan(o, a, b_, 0.0, ALU.mult, ALU.add),
                      [ones[0:72, :], f0], [f1]),
            lambda: cp("dve", h72, f1),
            lambda: cp("dve", parts[0:8, :], hiT[0:8, :]),
            lambda: tt("dve", f0, f1, h72, ALU.subtract),
            lambda: cp("dve", h72, f0),
            lambda: cp("dve", parts[32:40, :], hiT[32:40, :]),
            lambda: tt("dve", f1, f0, h72, ALU.subtract),
            lambda: cp("dve", parts[64:72, :], fr1[64:72, :]),
        ]
        for i in range(NT):
            b = next_bank(0, 2)
            pv = banks[b]
            for kc in range(8):
                mm(pv[:, :], xnT[:, kc, i * 128:(i + 1) * 128], wfox[:, kc, 1024:1536], kc == 0, kc == 7)
            pv4 = pv[:, :].rearrange("p (h two d) -> p h two d", two=2, d=64)
            vg = vaug[:, i, :].rearrange("p (h c) -> p h c", c=193)
            cp("dve", vg[:, :, 0:64], pv4[:, :, 0, :])
            cp("act", vg[:, :, 129:193], pv4[:, :, 1, :])
            if chain:
                chain.pop(0)()
        while chain:
            chain.pop(0)()

        fox_pending = []

        def proj_groups(h):
            qa_ = qa_s[h % 2]
            ka_ = ka_s[h % 2]
            out = []
            for tb in range(4):
                for which in (0, 1):
                    st = {}

                    def c0_(tb=tb, which=which, h=h, st=st):
                        cs = slice(tb * 512, (tb + 1) * 512)
                        st["pq"] = banks[next_bank(0, 2)]
                        pq = st["pq"]
                        mm(pq[0:70, :], sel[0:97, which * 8 + h, :], parts[0:97, cs], True, False)
                        wc = which * 512 + h * 64
                        for kc in range(0, 2):
                            mm(pq[0:64, :], wfox[:, kc, wc:wc + 64], xnT[:, kc, cs], False, False)

                    def c1_(tb=tb, which=which, h=h, st=st):
                        cs = slice(tb * 512, (tb + 1) * 512)
                        pq = st["pq"]
                        wc = which * 512 + h * 64
                        for kc in range(2, 5):
                            mm(pq[0:64, :], wfox[:, kc, wc:wc + 64], xnT[:, kc, cs], False, False)

                    def c2_(tb=tb, which=which, h=h, st=st, qa_=qa_, ka_=ka_):
                        cs = slice(tb * 512, (tb + 1) * 512)
                        pq = st["pq"]
                        wc = which * 512 + h * 64
                        for kc in range(5, 8):
                            mm(pq[0:64, :], wfox[:, kc, wc:wc + 64], xnT[:, kc, cs], False, kc == 7)
                        if which == 0:
                            ts("dve", qa_[0:70, cs], pq[0:70, :], 0.125, None, ALU.mult)
                        else:
                            cp("dve", ka_[0:70, cs], pq[0:70, :])
                    out += [c0_, c1_, c2_]
            return out

        for g_ in proj_groups(0):
            g_()
        for h in range(8):
            qa = qa_s[h % 2]
            ka = ka_s[h % 2]
            odd = h % 2
            nxt = proj_groups(h + 1) if h + 1 < 8 else []
            it_cnt = [0]
            vc = vcol(h)
            vw = 65 if not odd else 128
            for tb in range(4):
                t0 = tb * 512
                ob = 5 + (h * 4 + tb) % 2
                n_s = 4 * (tb + 1)
                orow = slice(0, 65) if not odd else slice(0, 128)
                lgb = {}

                def emit_qk(j, t0=t0, ka=ka, qa=qa, lgb=lgb):
                    s0 = j * 128
                    c0 = max(0, s0 - t0)
                    diag = s0 >= t0
                    lb_ = next_bank(2, 5)
                    lgb[j] = lb_
                    lg = banks[lb_]
                    mm(lg[:, c0:512], ka[0:70, s0:s0 + 128], qa[0:70, t0 + c0:t0 + 512], True, not diag)
                    if diag:
                        mm(lg[:, c0:c0 + 128], ident, maskfox, False, True)

                emit_qk(0)
                if n_s > 1:
                    emit_qk(1)
                for j in range(n_s):
                    if j + 2 < n_s:
                        emit_qk(j + 2)
                    s0 = j * 128
                    c0 = max(0, s0 - t0)
                    lg = banks[lgb[j]]
                    pT = pT_s[j % 3]
                    act(pT[:, c0:512], lg[:, c0:512], AF.Exp)
                    mm(banks[ob][orow, c0:512], vaug[:, j, vc:vc + vw], pT[:, c0:512], j == 0, j == n_s - 1)
                    if j == min(1, n_s - 1) and fox_pending:
                        fox_pending.pop()()
                    it_cnt[0] += 1
                    if False and nxt:
                        nxt.pop(0)()

                def norm_ops(ob=ob, odd=odd, h=h, t0=t0):
                    po = banks[ob]
                    if not odd:
                        act(sqf[0:65, :], po[0:65, :], AF.Square)
                        mm(banks[7][0:64, :], cvecA[0:65, :], sqf[0:65, :], True, True)
                        prow = slice(0, 64)
                    else:
                        act(sqf[:, :], po[:, :], AF.Square)
                        mm(banks[7][:, :], cvecB[:, :], sqf[:, :], True, True)
                        prow = slice(64, 128)
                    act(lnr[prow, :], banks[7][prow, :], AF.Ln)
                    act(rr[prow, :], lnr[prow, :], AF.Exp, scale=-0.5)
                    stt(ocat[prow, 4 + h // 2, t0:t0 + 512], po[prow, :], foxg[prow, :], rr[prow, :], ALU.mult, ALU.mult)

                fox_pending.append(norm_ops)
            while nxt:
                nxt.pop(0)()
        while fox_pending:
            fox_pending.pop()()
        if stop_after == "fox":
            break

        whg = sb(W, 32768, BF16).rearrange("p (k c) -> p k c", k=8)
        dma("pool", whg, win_d.rearrange("(k p) c -> p k c", p=128)[:, :, 0:2048], writes=[whg])
        T0 = sb(W + 32768, 8192, F32)
        T1 = sb(W + 40960, 8192, F32)
        T2 = sb(W + 49152, 8192, F32)
        T3 = sb(W + 57344, 8192, F32)
        qtl = sb(W + 65536, 4096, BF16)
        ktl = sb(W + 69632, 4096, BF16)
        ktok = sb(W + 73728, 4096, BF16).rearrange("p (t k) -> p t k", t=NT)
        vtok = sb(W + 77824, 4096, BF16).rearrange("p (t k) -> p t k", t=NT)
        sgT = sb(W + 81920, 4096, BF16)
        Sbf = sb(W + 86016, 8192, BF16).rearrange("p (c v) -> p c v", c=32)
        U_s = [sb(W + 94208 + 512 * i, 512, F32) for i in range(2)]
        scm_s = [sb(W + 95232 + 256 * i, 256, BF16) for i in range(2)]
        lnr2 = sb(W + 40960, 2048, F32)
        rr2 = sb(W + 40960 + 2048, 2048, F32)
        t1b = sb(W + 40960 + 4096, 2048, F32)
        sqh = sb(W + 40960 + 6144, 1024, BF16)
        for h in range(4):
            hs = slice(h * 128, (h + 1) * 128)
            for (coff, dst, fn_) in ((0, T0, AF.Silu), (1536, sgT, AF.Silu), (512, T1, AF.Sigmoid)):
                for tb in range(4):
                    cs = slice(tb * 512, (tb + 1) * 512)
                    b = next_bank(0, 2)
                    pp = banks[b]
                    for kc in range(8):
                        mm(pp[:, :], whg[:, kc, coff + h * 128:coff + (h + 1) * 128], xnT[:, kc, cs], kc == 0, kc == 7)
                    act(dst[:, cs], pp[:, :], fn_)
            for i4 in range(4):
                b = next_bank(0, 2)
                pp = banks[b]
                for ii in range(4):
                    i = i4 * 4 + ii
                    for kc in range(8):
                        mm(pp[:, ii * 128:(ii + 1) * 128], xnT[:, kc, i * 128:(i + 1) * 128],
                           whg[:, kc, 1024 + h * 128:1024 + (h + 1) * 128], kc == 0, kc == 7)
                cp("dve", vtok[:, i4 * 4:(i4 + 1) * 4, :], pp[:, :].rearrange("p (t k) -> p t k", t=4))
            ts("dve", T1, T1, oml[:, h:h + 1], lbc[:, h:h + 1], ALU.mult, ALU.add)
            act(T2, T1, AF.Ln)
            ts("dve", T1, T1, -1.0, 1.0, ALU.mult, ALU.add)
            A("dve", lambda e, o=T3, a=reset, b_=T2: e.tensor_tensor_scan(o, a, b_, 0.0, ALU.mult, ALU.add),
              [reset, T2], [T3])
            act(T2, T3, AF.Exp)
            tt("dve", qtl, T0, T2, ALU.mult)
            act(T0, T3, AF.Exp, scale=-1.0)
            tt("dve", ktl, T1, T0, ALU.mult)
            for i8 in range(2):
                b = next_bank(0, 2)
                pb = bankbf(b).rearrange("p (t k) -> p t k", t=8)
                for ii in range(8):
                    i = i8 * 8 + ii
                    tr(pb[:, ii, :], ktl[:, i * 128:(i + 1) * 128], ident)
                cp("act", ktok[:, i8 * 8:(i8 + 1) * 8, :], pb)
            for c in range(32):
                i, par = c // 2, c % 2
                if c % 4 == 0:
                    sb_ = next_bank(2, 5)
                dS = banks[sb_][:, (c % 4) * 128:(c % 4 + 1) * 128]
                ps_ = slice(par * 64, par * 64 + 64)
                mm(dS, ktok[ps_, i, :], vtok[ps_, i, :], True, True)
                Uc = U_s[c % 2]
                Up = U_s[(c + 1) % 2]
                if c == 0:
                    memset("pool", Sbf[:, 0, :], 0.0)
                    cp("dve", Uc, dS)
                else:
                    Dp = T2[:, (c - 1) * 64 + 63:(c - 1) * 64 + 64]
                    act(Sbf[:, c, :], Up, AF.Copy, scale=Dp)
                    stt(Uc, Up, Dp, dS, ALU.mult, ALU.add)
            for tb in range(4):
                ob = 5 + (h * 4 + tb) % 2
                po = banks[ob]
                scb = {}

                def emit_sc(ii, tb=tb, scb=scb):
                    i = tb * 4 + ii
                    cs = slice(i * 128, (i + 1) * 128)
                    lb_ = next_bank(2, 5)
                    sc = banks[lb_][:, 0:128]
                    mm(sc, ktl[:, cs], qtl[:, cs], True, True)
                    scm = scm_s[i % 2]
                    tt("dve", scm, sc, maskhg, ALU.mult)
                    scb[ii] = scm

                emit_sc(0)
                for ii in range(4):
                    i = tb * 4 + ii
                    if ii + 1 < 4:
                        emit_sc(ii + 1)
                    scm = scb[ii]
                    oo = po[:, ii * 128:(ii + 1) * 128]
                    mm(oo, vtok[:, i, :], scm, True, False)
                    mm(oo[:, 0:64], Sbf[:, 2 * i, :], qtl[:, i * 128:i * 128 + 64], False, False)
                    mm(oo[:, 64:128], Sbf[:, 2 * i + 1, :], qtl[:, i * 128 + 64:i * 128 + 128], False, True)
                act(sqh, po[:, :], AF.Square)
                mm(banks[7][:, :], cmathg, sqh, True, True)
                act(lnr2, banks[7][:, :], AF.Ln, bias=EPS)
                act(rr2, lnr2, AF.Exp, scale=-0.5)
                stt(t1b, po[:, :], hgg, rr2, ALU.mult, ALU.mult)
                tt("pool", ocat[:, h, tb * 512:(tb + 1) * 512], t1b, sgT[:, tb * 512:(tb + 1) * 512], ALU.mult)
        if stop_after == "hg":
            break

        hb_s = [sb(W + 4096 * i, 4096, F32) for i in range(2)]
        xt2_s = [sb(W + 8192 + 4096 * i, 4096, F32) for i in range(2)]
        junk2 = sb(W + 16384, 2048, BF16)
        hn_s = [sb(W + 18432 + 2048 * i, 2048, BF16) for i in range(2)]
        hnt_s = [sb(W + 22528 + 2048 * i, 2048, BF16).rearrange("p (k t) -> p k t", k=8) for i in range(2)]
        for i in range(NT):
            gi = sq_ * NT + i
            rows = slice(gi * 128, (gi + 1) * 128)
            xt = xt2_s[i % 2]
            hb = hb_s[i % 2]
            if i == 0:
                dma("sp", xt, x_d[sq_, 0:128, :], writes=[xt])
            if i + 1 < NT:
                dma("sp", xt2_s[(i + 1) % 2], x_d[sq_, (i + 1) * 128:(i + 2) * 128, :], writes=[xt2_s[(i + 1) % 2]])
            for half in range(2):
                b = next_bank(0, 2)
                ph = banks[b]
                for fc in range(8):
                    mm(ph[:, :], ocat[:, fc, i * 128:(i + 1) * 128], wout[:, fc, half * 512:(half + 1) * 512], fc == 0, fc == 7)
                tt("dve", hb[:, half * 512:(half + 1) * 512], ph[:, :], xt[:, half * 512:(half + 1) * 512], ALU.add)
            dma("sp", h_scr[rows, :], hb, reads=[hb], writes=[h_scr[rows, :]])
            rstd = rms_rstd(hb, junk2, 32 + 4 * (i % 8))
            hn = hn_s[i % 2]
            stt(hn, hb, rstd, gffn, ALU.mult, ALU.mult)
            dma("sp", hn_scr[rows, :], hn, reads=[hn], writes=[hn_scr[rows, :]])
            b = next_bank(2, 4)
            pb = bankbf(b).rearrange("p (k t) -> p k t", k=8)
            for kc in range(8):
                tr(pb[:, kc, :], hn[:, kc * 128:(kc + 1) * 128], ident)
            hnt = hnt_s[i % 2]
            cp("act", hnt, pb)
            b = next_bank(4, 6)
            pr = banks[b][:, 0:20]
            for kc in range(8):
                mm(pr, hnt[:, kc, :], wr[:, kc, :], kc == 0, kc == 7)
            ro = (i % 2) * 128
            lgt = rt[:, ro:ro + 20]
            gmax = rt[:, ro + 20:ro + 21]
            ngmax = rt[:, ro + 21:ro + 22]
            gm = rt[:, ro + 22:ro + 26]
            eg = rt[:, ro + 26:ro + 30]
            sumg = rt[:, ro + 30:ro + 31]
            pgs = rt[:, ro + 31:ro + 32]
            pen = rt[:, ro + 32:ro + 48]
            elm = rt[:, ro + 48:ro + 64]
            top8 = rt[:, ro + 64:ro + 72]
            nm1 = rt[:, ro + 72:ro + 73]
            selm = rt[:, ro + 73:ro + 89]
            den2 = rt[:, ro + 89:ro + 90]
            ex = rt[:, ro + 91:ro + 107]
            tt("dve", lgt, pr, brt, ALU.add)
            A("dve", lambda e, o=gmax, i_=lgt[:, 0:4]: e.reduce_max(o, i_, AX.X), [lgt[:, 0:4]], [gmax])
            ts("dve", gm, lgt[:, 0:4], gmax, None, ALU.is_equal)
            ts("dve", ngmax, gmax, -1.0, None, ALU.mult)
            act(eg, lgt[:, 0:4], AF.Exp, bias=ngmax, accum=sumg)
            recip(pgs, sumg)
            gm_b = bass.AP(gm.tensor, gm.offset, [list(gm.ap[0]), [1, 4], [0, 4]])
            pen3 = pen.rearrange("p (g j) -> p g j", j=4)
            ts("dve", pen3, gm_b, -1.0, 1e30, ALU.add, ALU.mult)
            tt("dve", elm, lgt[:, 4:20], pen, ALU.add)
            A("dve", lambda e, o=top8, i_=elm: e.max(o, i_), [elm], [top8])
            ts("dve", selm, elm, top8[:, 1:2], None, ALU.is_ge)
            cp("dve", M2b[:, gi * 16:(gi + 1) * 16], selm)
            ts("dve", M1b[:, gi * 16:(gi + 1) * 16], elm, top8[:, 0:1], None, ALU.is_equal)
            ts("dve", nm1, top8[:, 0:1], -1.0, None, ALU.mult)
            act(ex, elm, AF.Exp, bias=nm1)
            stt(ex, ex, 1.0, selm, ALU.mult, ALU.mult, accum=den2)
            recip(den2, den2)
            tt("dve", w1s[:, gi:gi + 1], den2, pgs, ALU.mult)
            tt("dve", w2s[:, gi:gi + 1], pgs, w1s[:, gi:gi + 1], ALU.subtract)

    if stop_after is None:
        NTT16 = NTT * 16
        def f32t(k):
            return sb(XNT + 4096 * k, 4096, F32)[:, 0:NTT16]
        TotS, RS, CI, EX, PP, TMP = [f32t(k) for k in range(6)]
        small2 = sb(XNT + 4096 * 6, 4096, F32)
        ne = small2[:, 0:16]
        ke = small2[:, 16:32]
        cki = small2[:, 32:48]
        base = small2[:, 48:64]
        cmp_ = small2[:, 64:320]
        ejf = small2[:, 320:384]
        cmp2 = sb(XNT + 4096 * 7, 4096, F32)
        pos1f = small2[:, 384:448]
        pos2f = small2[:, 448:512]
        idf0 = small2[:, 512:576]
        idf1 = small2[:, 576:640]
        for half in range(0, NTT16, 512):
            n_ = min(512, NTT16 - half)
            mm(banks[0][:, 0:n_], lstrict, M2b[:, half:half + n_], True, True)
            cp("dve", RS[:, half:half + n_], banks[0][:, 0:n_])
            mm(banks[1][:, 0:n_], ones[:, 0:128], M2b[:, half:half + n_], True, True)
            cp("dve", TotS[:, half:half + n_], banks[1][:, 0:n_])
        TotS3 = TotS.rearrange("p (t e) -> p t e", e=16)
        CI3 = CI.rearrange("p (t e) -> p t e", e=16)
        for e_ in range(16):
            A("dve", lambda e, o=CI3[:, :, e_], a=ones[:, 0:NTT], b_=TotS3[:, :, e_]:
              e.tensor_tensor_scan(o, a, b_, 0.0, ALU.mult, ALU.add), [ones[:, 0:NTT], TotS3[:, :, e_]], [CI3[:, :, e_]])
        tt("dve", EX, CI, TotS, ALU.subtract)
        cp("dve", ne, CI3[:, NTT - 1, :])
        ne_b = bass.AP(ne.tensor, ne.offset, [list(ne.ap[0]), [1, 16], [0, 16]])
        thr_b = bass.AP(thr.tensor, thr.offset, [list(thr.ap[0]), [0, 16], [1, 16]])
        tt("dve", cmp_.rearrange("p (a b) -> p a b", b=16), ne_b, thr_b, ALU.is_gt)
        A("dve", lambda e, o=ke, i_=cmp_.rearrange("p (a b) -> p a b", b=16): e.reduce_sum(o, i_, AX.X),
          [cmp_], [ke])
        A("dve", lambda e, o=cki, a=ones[:, 0:16], b_=ke: e.tensor_tensor_scan(o, a, b_, 0.0, ALU.mult, ALU.add),
          [ones[:, 0:16], ke], [cki])
        tt("dve", base, cki, ke, ALU.subtract)
        ts("dve", base, base, 512.0, None, ALU.mult)
        base_b = bass.AP(base.tensor, base.offset, [list(base.ap[0]), [0, NTT], [1, 16]])
        tt("dve", PP, RS, EX, ALU.add)
        PP3 = PP.rearrange("p (t e) -> p t e", e=16)
        tt("dve", PP3, PP3, base_b, ALU.add)
        tt("dve", TMP, PP, M1b[:, 0:NTT16], ALU.mult)
        A("dve", lambda e, o=pos1f[:, 0:NTT], i_=TMP.rearrange("p (t e) -> p t e", e=16): e.reduce_sum(o, i_, AX.X),
          [TMP], [pos1f[:, 0:NTT]])
        tt("dve", TMP, PP, M2b[:, 0:NTT16], ALU.mult)
        A("dve", lambda e, o=pos2f[:, 0:NTT], i_=TMP.rearrange("p (t e) -> p t e", e=16): e.reduce_sum(o, i_, AX.X),
          [TMP], [pos2f[:, 0:NTT]])
        tt("dve", pos2f[:, 0:NTT], pos2f[:, 0:NTT], pos1f[:, 0:NTT], ALU.subtract)
        cp("dve", pos1i[:, 0:NTT], pos1f[:, 0:NTT])
        cp("dve", pos2i[:, 0:NTT], pos2f[:, 0:NTT])
        cki_b = bass.AP(cki.tensor, cki.offset, [list(cki.ap[0]), [0, NSLOT], [1, 16]])
        jt_b = bass.AP(jt.tensor, jt.offset, [list(jt.ap[0]), [1, NSLOT], [0, 16]])
        c23 = cmp2[:, 0:NSLOT * 16].rearrange("p (j e) -> p j e", e=16)
        tt("dve", c23, cki_b, jt_b, ALU.is_le)
        A("dve", lambda e, o=ejf[:, 0:NSLOT], i_=c23: e.reduce_sum(o, i_, AX.X), [cmp2[:, 0:NSLOT * 16]], [ejf[:, 0:NSLOT]])
        ts("dve", ejf[:, 0:NSLOT], ejf[:, 0:NSLOT], 15.0, None, ALU.min)
        ts("dve", idf0[:, 0:NSLOT], ejf[:, 0:NSLOT], 256.0, pcol2, ALU.mult, ALU.add)
        ts("dve", idf1[:, 0:NSLOT], idf0[:, 0:NSLOT], 1.0, None, ALU.add)
        cp("dve", idxW0[:, 0:NSLOT], idf0[:, 0:NSLOT])
        cp("dve", idxW1[:, 0:NSLOT], idf1[:, 0:NSLOT])

        def idma(out, in_, out_off=None, in_off=None, reads=(), writes=()):
            def fn(e, o=out, i=in_, oo=out_off, io=in_off):
                return e.indirect_dma_start(
                    out=o, out_offset=(bass.IndirectOffsetOnAxis(ap=oo, axis=0) if oo is not None else None),
                    in_=i, in_offset=(bass.IndirectOffsetOnAxis(ap=io, axis=0) if io is not None else None))
            A("pool", fn, reads, writes, dma=True)

        hnc_s = [sb(XNT + 131072 + 2048 * i, 2048, BF16) for i in range(8)]
        for gi in range(NTT):
            hnc = hnc_s[gi % 8]
            rows = slice(gi * 128, (gi + 1) * 128)
            dma("sp", hnc, hn_scr[rows, :], reads=[hn_scr[rows, :]], writes=[hnc])
            idma(so_scr[:, :], hnc, out_off=pos1i[:, gi:gi + 1], reads=[hnc, pos1i[:, gi:gi + 1]], writes=[so_scr[:, :]])
            idma(so_scr[:, :], hnc, out_off=pos2i[:, gi:gi + 1], reads=[hnc, pos2i[:, gi:gi + 1]], writes=[so_scr[:, :]])

        D0 = XNT

        def d_views(j):
            wo = D0 + 24576 * (j % 2)
            wg = sb(wo, 8192, BF16)
            wu = sb(wo + 8192, 8192, BF16)
            wd = sb(wo + 16384, 8192, BF16)
            xtok = sb(D0 + 49152 + 8192 * (j % 2), 8192, BF16).rearrange("p (a c) -> p a c", a=4)
            return wg, wu, wd, xtok

        def d_load(j):
            wg, wu, wd, xtok = d_views(j)
            srows = so_scr[j * 512:(j + 1) * 512, :]
            dma("sp", xtok, srows.rearrange("(a p) c -> p a c", p=128), reads=[srows], writes=[xtok])
            for (dst, src) in ((wg, wg_d), (wu, wu_d), (wd, wd_d)):
                idma(dst[:, 0:2048], src[:, :], in_off=idxW0[:, j:j + 1], reads=[idxW0[:, j:j + 1]], writes=[dst[:, 0:2048]])
                idma(dst[:, 2048:4096], src[:, :], in_off=idxW1[:, j:j + 1], reads=[idxW1[:, j:j + 1]], writes=[dst[:, 2048:4096]])

        d_load(0)
        for j in range(NSLOT):
            if j + 1 < NSLOT:
                d_load(j + 1)
            wg, wu, wd, xtok = d_views(j)
            wg3 = wg.rearrange("p (k c) -> p k c", k=8)
            wu3 = wu.rearrange("p (k c) -> p k c", k=8)
            wd3 = wd.rearrange("p (k c) -> p k c", k=4)
            hsT = sb(D0 + 65536 + 8192 * (j % 2), 8192, BF16).rearrange("p (k t) -> p k t", k=8)
            hact = sb(D0 + 81920 + 4096 * (j % 2), 4096, BF16).rearrange("p (c t) -> p c t", c=4)
            ybuf = sb(D0 + 94208 + 16384 * (j % 2), 16384, F32).rearrange("p (a c) -> p a c", a=4)
            for a in range(4):
                b = 6 + a % 2
                pb = bankbf(b).rearrange("p (k t) -> p k t", k=8)
                for kc in range(8):
                    tr(pb[:, kc, :], xtok[:, a, kc * 128:(kc + 1) * 128], ident)
                cp("act" if a % 2 else "dve", hsT[:, :, a * 128:(a + 1) * 128], pb)
            for hc in range(4):
                gb = hc % 2
                ub = 2 + hc % 2
                for kc in range(8):
                    mm(banks[gb][:, :], wg3[:, kc, hc * 128:(hc + 1) * 128], hsT[:, kc, :], kc == 0, kc == 7)
                for kc in range(8):
                    mm(banks[ub][:, :], wu3[:, kc, hc * 128:(hc + 1) * 128], hsT[:, kc, :], kc == 0, kc == 7)
                sgt = sb(D0 + 90112 + 2048 * (hc % 2), 2048, F32)
                act(sgt, banks[gb][:, :], AF.Silu)
                tt("dve", hact[:, hc, :], sgt, banks[ub][:, :], ALU.mult)
            for a in range(4):
                for half in range(2):
                    yb = 4 + (a * 2 + half) % 2
                    for hc in range(4):
                        mm(banks[yb][:, :], hact[:, hc, a * 128:(a + 1) * 128], wd3[:, hc, half * 512:(half + 1) * 512], hc == 0, hc == 3)
                    cp("act" if half else "dve", ybuf[:, a, half * 512:(half + 1) * 512], banks[yb][:, :])
            yrows = y_scr[j * 512:(j + 1) * 512, :]
            dma("sp", yrows.rearrange("(a p) c -> p a c", p=128), ybuf, reads=[ybuf], writes=[yrows])

        gfin = sb(XNT + 151552, 4096, F32)
        dma("sp", gfin, gfin_d.partition_broadcast(128), writes=[gfin])
        junk3 = sb(XNT + 155648, 2048, BF16)
        def e_views(gi):
            eo = XNT + 12288 * (gi % 4)
            return sb(eo, 4096, F32), sb(eo + 4096, 4096, F32), sb(eo + 8192, 4096, F32)

        def e_load(gi):
            y1, y2, hb = e_views(gi)
            rows = slice(gi * 128, (gi + 1) * 128)
            idma(y1, y_scr[:, :], in_off=pos1i[:, gi:gi + 1], reads=[y_scr[:, :], pos1i[:, gi:gi + 1]], writes=[y1])
            idma(y2, y_scr[:, :], in_off=pos2i[:, gi:gi + 1], reads=[y_scr[:, :], pos2i[:, gi:gi + 1]], writes=[y2])
            dma("sp", hb, h_scr[rows, :], reads=[h_scr[rows, :]], writes=[hb])

        for g0 in range(min(3, NTT)):
            e_load(g0)
        for gi in range(NTT):
            sq_, i = gi // NT, gi % NT
            if gi + 3 < NTT:
                e_load(gi + 3)
            y1, y2, hb = e_views(gi)
            stt(hb, y1, w1s[:, gi:gi + 1], hb, ALU.mult, ALU.add)
            stt(hb, y2, w2s[:, gi:gi + 1], hb, ALU.mult, ALU.add)
            rstd = rms_rstd(hb, junk3, 64 + 4 * (gi % 8))
            stt(hb, hb, rstd, gfin, ALU.mult, ALU.mult)
            dma("sp", out_d[sq_, i * 128:(i + 1) * 128, :], hb, reads=[hb])

    sch.prepare()
    sems = {k: es.enter_context(nc.semaphore(f"s_{k}")) for k in ("pe", "act", "dve", "pool")}
    dsems = {}
    for q in ("sp", "pool", "act"):
        for k in range(sch.n_dma_sems):
            dsems[(q, k)] = es.enter_context(nc.semaphore(f"d_{q}{k}"))
    with nc.Block() as block:
        @block.tensor
        def _(eng):
            sch.emit_engine("pe", eng, sems, dsems)

        @block.scalar
        def _(eng):
            sch.emit_engine("act", eng, sems, dsems)

        @block.vector
        def _(eng):
            sch.emit_engine("dve", eng, sems, dsems)

        @block.gpsimd
        def _(eng):
            sch.emit_engine("pool", eng, sems, dsems)

        @block.sync
        def _(eng):
            sch.emit_engine("sp", eng, sems, dsems)
    es.close()
    return nc, sch


_CACHE = {}


def _get_nc(nseq):
    if nseq not in _CACHE:
        _CACHE[nseq] = build(nseq, Sched, F32, BF16, U8)[0]
    return _CACHE[nseq]


def kernel(x, attn_norm, w_in, hg_lb_logits, hg_norm, fox_f_bias, fox_norm, w_out, ffn_norm,
           w_group, b_group, w_expert, b_expert, w_gate, w_up, w_down, final_norm):
    f = lambda a: np.ascontiguousarray(np.asarray(a, dtype=np.float32))
    x = f(x)
    n_cores = 8
    B = x.shape[0]
    nseq = B // n_cores
    shared = {
        "w_in": f(w_in)[0], "w_out": f(w_out)[0],
        "w_gate": np.ascontiguousarray(f(w_gate)[0].reshape(16, 8, 128, 512).transpose(0, 2, 1, 3)).reshape(16 * 256, 2048),
        "w_up": np.ascontiguousarray(f(w_up)[0].reshape(16, 8, 128, 512).transpose(0, 2, 1, 3)).reshape(16 * 256, 2048),
        "w_down": np.ascontiguousarray(f(w_down)[0].reshape(16, 4, 128, 1024).transpose(0, 2, 1, 3)).reshape(16 * 256, 2048),
        "w_router": np.ascontiguousarray(np.concatenate([f(w_group)[0], f(w_expert)[0]], axis=1)),
        "b_router": np.ascontiguousarray(np.concatenate([f(b_group)[0], f(b_expert)[0]])[None, :]),
        "attn_norm": f(attn_norm), "ffn_norm": f(ffn_norm), "final_norm": f(final_norm).reshape(1, -1),
        "hg_lb_logits": f(hg_lb_logits), "hg_norm": f(hg_norm), "fox_f_bias": f(fox_f_bias),
        "fox_norm": f(fox_norm),
    }
    shared.update(make_consts())
    in_maps = []
    for c in range(n_cores):
        m = dict(shared)
        m["x"] = np.ascontiguousarray(x[c * nseq:(c + 1) * nseq])
        in_maps.append(m)
    nc = _get_nc(nseq)
    res = run_bass_kernel_spmd(nc, in_maps, core_ids=list(range(n_cores)))
    return np.concatenate([r["out"] for r in res.results], axis=0).astype(np.float32)
```

```python
import contextlib
import numpy as np
import ml_dtypes
import concourse.bass as bass
import concourse.mybir as mybir

F32 = mybir.dt.float32
BF16 = mybir.dt.bfloat16
U8 = mybir.dt.uint8
_ES = {F32: 4, BF16: 2, U8: 1, mybir.dt.int32: 4, mybir.dt.uint32: 4, mybir.dt.uint16: 2}


class Op:
    __slots__ = ("eng", "fn", "deps", "cdeps", "seq", "ms", "needed", "dma", "dsem", "dval", "dprev", "tag")

    def __init__(self, eng, fn, dma, tag):
        self.eng = eng
        self.fn = fn
        self.deps = set()
        self.cdeps = {}
        self.seq = 0
        self.ms = None
        self.needed = False
        self.dma = dma
        self.dsem = None
        self.dval = None
        self.dprev = 0
        self.tag = tag


class Sched:
    def __init__(self, n_dma_sems=8, same_engine_sync=True):
        self.ops = []
        self.recs = {}
        self.n_dma_sems = n_dma_sems
        self.same_engine_sync = same_engine_sync
        self.dram_track = set()
        self.embed_wait = True
        self.whole = {}

    def _regions(self, ap):
        t = ap.tensor
        cls = type(t).__name__
        if cls.startswith("DRam"):
            name = ap.name
            if name not in self.dram_track:
                return None
            es = _ES[ap.dtype]
            pat = ap.ap
            off = int(ap.offset)
            hi = off + sum((n - 1) * abs(st) for st, n in pat) + 1
            return name, False, 0, 1, [(off * es, hi * es)]
        psum = cls.startswith("PSum") or cls.startswith("Psum")
        name = ap.name
        if psum:
            return name, True, 0, 128, [(0, 1 << 20)]
        es = _ES[ap.dtype]
        pat = ap.ap
        off = int(ap.offset)
        pstride, pcnt = pat[0]
        p0 = off // pstride
        c0 = off % pstride
        free = list(pat[1:])
        if not free:
            return name, False, p0, p0 + pcnt, [(c0 * es, (c0 + 1) * es)]
        ls, ln = free[-1]
        run = (ln - 1) * abs(ls) + 1
        outer = free[:-1]
        nout = 1
        for s, n in outer:
            nout *= n
        ivs = []
        if nout <= 64:
            idx = [0] * len(outer)
            while True:
                st = c0 + sum(i * s for i, (s, n) in zip(idx, outer))
                ivs.append((st * es, (st + run) * es))
                k = len(outer) - 1
                while k >= 0:
                    idx[k] += 1
                    if idx[k] < outer[k][1]:
                        break
                    idx[k] = 0
                    k -= 1
                if k < 0:
                    break
        else:
            hi = c0 + sum((n - 1) * abs(s) for s, n in outer) + run
            ivs.append((c0 * es, hi * es))
        ivs.sort()
        out = [ivs[0]]
        for a, b in ivs[1:]:
            if a <= out[-1][1]:
                out[-1] = (out[-1][0], max(out[-1][1], b))
            else:
                out.append((a, b))
        return name, False, p0, p0 + pcnt, out

    def _access(self, op, ap, is_write):
        r = self._regions(ap)
        if r is None:
            return
        name, psum, p0, p1, ivs = r
        tab = self.recs.setdefault(name, {})
        w = is_write or psum
        SH = 20 if name in self.dram_track else 11
        for (b0, b1) in ivs:
            newrec = (p0, p1, b0, b1, op, w)
            for bk in range(b0 >> SH, ((b1 - 1) >> SH) + 1):
                lst = tab.get(bk)
                if lst is None:
                    tab[bk] = [newrec]
                    continue
                keep = []
                for rec in lst:
                    rp0, rp1, rb0, rb1, rop, rw = rec
                    ov = rp0 < p1 and p0 < rp1 and rb0 < b1 and b0 < rb1
                    if ov and (rw or w) and rop is not op:
                        if rop.dma:
                            op.deps.add(rop)
                        else:
                            c = op.cdeps.get(rop.eng)
                            if c is None or c.seq < rop.seq:
                                op.cdeps[rop.eng] = rop
                    if ov and w and rp0 >= p0 and rp1 <= p1 and rb0 >= b0 and rb1 <= b1:
                        continue
                    if (not w) and (not rw) and rop.eng == op.eng and not rop.dma and not op.dma \
                            and rp0 == p0 and rp1 == p1 and rb0 == b0 and rb1 == b1:
                        continue
                    keep.append(rec)
                keep.append(newrec)
                tab[bk] = keep

    def add(self, eng, fn, reads=(), writes=(), dma=False, tag=None, extra_deps=()):
        op = Op(eng, fn, dma, tag)
        op.seq = len(self.ops)
        for ap in reads:
            self._access(op, ap, False)
        for ap in writes:
            self._access(op, ap, True)
        op.deps.update(op.cdeps.values())
        op.deps.update(extra_deps)
        op.cdeps = None
        self.ops.append(op)
        return op

    def prepare(self):
        for op in self.ops:
            for d in op.deps:
                if d.dma:
                    continue
                if d.eng == op.eng and (d.eng == "pe" or not self.same_engine_sync) and not op.dma:
                    continue
                d.needed = True
        cnt = {}
        dcnt = {}
        dslot = {}
        for op in self.ops:
            if op.dma:
                k = dslot.get(op.eng, 0)
                dslot[op.eng] = k + 1
                slot = (op.eng, k % self.n_dma_sems)
                prev = dcnt.get(slot, 0)
                op.dsem = slot
                op.dprev = prev
                op.dval = prev + 16
                dcnt[slot] = op.dval
            elif op.needed:
                cnt[op.eng] = cnt.get(op.eng, 0) + 1
                op.ms = cnt[op.eng]
        self.final_dma = dcnt
        self.ms_total = cnt

    def emit_engine(self, eng_name, eng, sems, dsems):
        waited = {}
        pend = []

        def need(sem_key, sem, val):
            if val <= 0:
                return
            if waited.get(sem_key, 0) >= val:
                return
            waited[sem_key] = val
            pend.append((sem, val))

        n = 0
        for op in self.ops:
            if op.eng != eng_name:
                continue
            for d in op.deps:
                if d.dma:
                    need(d.dsem, dsems[d.dsem], d.dval)
                else:
                    if d.eng == eng_name and not op.dma and (eng_name == "pe" or not self.same_engine_sync):
                        continue
                    need(d.eng, sems[d.eng], d.ms)
            if op.dma:
                need(op.dsem, dsems[op.dsem], op.dprev)
            embed = None
            if self.embed_wait and pend and not op.dma:
                embed = pend.pop()
            for (sm, vl) in pend:
                eng.wait_ge(sm, vl)
            del pend[:]
            ins = op.fn(eng)
            if embed is not None:
                ins._wait_ge(embed[0], embed[1])
            if op.dma:
                ins.then_inc(dsems[op.dsem], 16)
            elif op.needed:
                ins.then_inc(sems[eng_name], 1)
            n += 1
        for slot, val in self.final_dma.items():
            if slot[0] == eng_name:
                need(slot, dsems[slot], val)
        for (sm, vl) in pend:
            eng.wait_ge(sm, vl)
        return n


from concourse.bass_utils import run_bass_kernel_spmd

AF = mybir.ActivationFunctionType
ALU = mybir.AluOpType
AX = mybir.AxisListType
I32 = mybir.dt.int32

D = 1024
S = 2048
NT = S // 128
HGW = 512
FOXW = 512
INC = 3592
NE = 16
EH = 512
EPS = 1e-6

ARENA = 212000
PERS = 208000
C_IDENT = 0
C_MASKFOX = 256
C_MASKHG = 512
C_CMATHG = 768
C_CVECA = 1024
C_CVECB = 1152
C_SEL = 1408
C_ONES = 3648
C_RESET = 7744
C_GATTN = 11840
C_GFFN = 15936
C_GFIN = 20032
C_SMALL = 24128
C_WOUT = 24640
C_WR = 41024
C_STAT = 41344
C_GATES = 42368
C_RT = 43392
XNT = 44544
OCAT = XNT + 32768
W = OCAT + 32768
WSIZE = PERS - W
TT = W + 65536


def make_consts():
    bf = ml_dtypes.bfloat16
    ident = np.eye(128, dtype=np.float32).astype(bf)
    s_ = np.arange(128)[:, None]
    t_ = np.arange(128)[None, :]
    maskfox = np.where(t_ >= s_, 0.0, -30000.0).astype(np.float32).astype(bf)
    maskhg = ((t_ >= s_) & ((t_ // 64) == (s_ // 64))).astype(np.float32).astype(bf)
    cvecA = np.zeros((65, 64), np.float32)
    cvecA[0:64, :] = 1.0 / 64
    cvecA[64, :] = EPS
    cvecB = np.zeros((128, 128), np.float32)
    cvecB[0, 64:128] = EPS
    cvecB[64:128, 64:128] = 1.0 / 64
    sel = np.zeros((97, 16, 70), np.float32)
    for h in range(8):
        sel[h, h, 64] = 8.0
        sel[32 + h, h, 65] = 8.0
        sel[64 + h, h, 66] = 8.0
        sel[96, h, 67:70] = 8.0
        sel[96, 8 + h, 64:67] = 1.0
        sel[h, 8 + h, 67] = -1.0
        sel[32 + h, 8 + h, 68] = -1.0
        sel[64 + h, 8 + h, 69] = -1.0
    lstrict = (s_ < t_).astype(np.float32).astype(bf)
    thr = np.tile((512.0 * np.arange(16, dtype=np.float32))[None, :], (128, 1))
    jt = np.tile(np.arange(64, dtype=np.float32)[None, :], (128, 1))
    pcol2 = (2.0 * np.arange(128, dtype=np.float32)).reshape(128, 1)
    return {
        "c_lstrict": lstrict, "c_thr": thr, "c_jt": jt, "c_pcol2": pcol2,
        "c_ident": ident,
        "c_maskfox": maskfox,
        "c_maskhg": maskhg,
        "c_cveca": cvecA.astype(bf),
        "c_cvecb": cvecB.astype(bf),
        "c_sel": sel.reshape(97, 16 * 70).astype(bf),
    }


def build(nseq, Sched, F32, BF16, U8, stop_after=None, dbg=False):
    nc = bass.Bass("TRN2", target_bir_lowering=False)
    dt_ = nc.dram_tensor
    x_d = dt_("x", [nseq, S, D], F32, kind="ExternalInput").ap()
    win_d = dt_("w_in", [D, INC], F32, kind="ExternalInput").ap()
    wout_d = dt_("w_out", [D, D], F32, kind="ExternalInput").ap()
    wg_d = dt_("w_gate", [NE * 256, 2048], F32, kind="ExternalInput").ap()
    wu_d = dt_("w_up", [NE * 256, 2048], F32, kind="ExternalInput").ap()
    wd_d = dt_("w_down", [NE * 256, 2048], F32, kind="ExternalInput").ap()
    cls_d = dt_("c_lstrict", [128, 128], BF16, kind="ExternalInput").ap()
    cthr_d = dt_("c_thr", [128, 16], F32, kind="ExternalInput").ap()
    cjt_d = dt_("c_jt", [128, 64], F32, kind="ExternalInput").ap()
    cp2_d = dt_("c_pcol2", [128, 1], F32, kind="ExternalInput").ap()
    NTT = nseq * NT
    NSLOT = nseq * 8 + 16
    NROW = NSLOT * 512
    h_scr = dt_("h_scr", [NTT * 128, D], F32, kind="Internal").ap()
    hn_scr = dt_("hn_scr", [NTT * 128, D], BF16, kind="Internal").ap()
    so_scr = dt_("so_scr", [NROW, D], BF16, kind="Internal").ap()
    y_scr = dt_("y_scr", [NROW, D], F32, kind="Internal").ap()
    wr_d = dt_("w_router", [D, 20], F32, kind="ExternalInput").ap()
    br_d = dt_("b_router", [1, 20], F32, kind="ExternalInput").ap()
    gattn_d = dt_("attn_norm", [1, D], F32, kind="ExternalInput").ap()
    gffn_d = dt_("ffn_norm", [1, D], F32, kind="ExternalInput").ap()
    gfin_d = dt_("final_norm", [1, D], F32, kind="ExternalInput").ap()
    lbl_d = dt_("hg_lb_logits", [2, HGW], F32, kind="ExternalInput").ap()
    hgn_d = dt_("hg_norm", [1, 128], F32, kind="ExternalInput").ap()
    fb_d = dt_("fox_f_bias", [1, 8], F32, kind="ExternalInput").ap()
    fxn_d = dt_("fox_norm", [1, 64], F32, kind="ExternalInput").ap()
    ci_d = dt_("c_ident", [128, 128], BF16, kind="ExternalInput").ap()
    cmf_d = dt_("c_maskfox", [128, 128], BF16, kind="ExternalInput").ap()
    cmh_d = dt_("c_maskhg", [128, 128], BF16, kind="ExternalInput").ap()
    cva_d = dt_("c_cveca", [65, 64], BF16, kind="ExternalInput").ap()
    cvb_d = dt_("c_cvecb", [128, 128], BF16, kind="ExternalInput").ap()
    csel_d = dt_("c_sel", [97, 16 * 70], BF16, kind="ExternalInput").ap()
    out_d = dt_("out", [nseq, S, D], F32, kind="ExternalOutput").ap()
    dbg_d = None
    if dbg:
        dbg_d = dt_("dbg", [S, D], F32, kind="ExternalOutput").ap()

    sch = Sched(n_dma_sems=24)
    sch.dram_track.update(["h_scr", "hn_scr", "so_scr", "y_scr"])
    es = contextlib.ExitStack()
    arena = es.enter_context(nc.sbuf_tensor("arena", [128, ARENA], U8))
    banks = [es.enter_context(nc.psum_tensor(f"bank{i}", [128, 512], F32)) for i in range(8)]

    def sb(off, nbytes, dt):
        return arena[:, off:off + nbytes].bitcast(dt)

    def bankbf(i):
        return banks[i][:, :].bitcast(BF16)

    ident = sb(C_IDENT, 256, BF16)
    maskfox = sb(C_MASKFOX, 256, BF16)
    maskhg = sb(C_MASKHG, 256, BF16)
    cmathg = sb(C_CMATHG, 256, BF16)
    cvecA = sb(C_CVECA, 128, BF16)
    cvecB = sb(C_CVECB, 256, BF16)
    sel = sb(C_SEL, 2240, BF16).rearrange("p (a b) -> p a b", a=16)
    ones = sb(C_ONES, 4096, BF16)
    reset = sb(C_RESET, 4096, BF16)
    gattn = sb(C_GATTN, 4096, F32)
    gffn = sb(C_GFFN, 4096, F32)
    M2b = sb(C_GFIN, 2048, BF16)
    M1b = sb(C_GFIN + 2048, 2048, BF16)
    small = sb(C_SMALL, 512, F32)
    lbc = small[:, 0:4]
    oml = small[:, 4:8]
    hgg = small[:, 8:9]
    foxg = small[:, 9:10]
    negb = small[:, 10:11]
    l0 = small[:, 11:15]
    l1 = small[:, 15:19]
    ltmp = small[:, 19:23]
    brt = small[:, 24:44]
    wout = sb(C_WOUT, 16384, BF16).rearrange("p (k c) -> p k c", k=8)
    wr = sb(C_WR, 320, BF16).rearrange("p (k c) -> p k c", k=8)
    stat = sb(C_STAT, 1024, F32)
    w1s = sb(C_GATES, 256, F32)
    w2s = sb(C_GATES + 256, 256, F32)
    pos1i = sb(C_GATES + 512, 256, I32)
    pos2i = sb(C_GATES + 768, 256, I32)
    lstrict = sb(PERS, 256, BF16)
    thr = sb(PERS + 256, 64, F32)
    jt = sb(PERS + 320, 256, F32)
    pcol2 = sb(PERS + 576, 4, F32)
    idxW0 = sb(PERS + 640, 256, I32)
    idxW1 = sb(PERS + 896, 256, I32)
    rt = sb(C_RT, 1024, F32)
    xnT = sb(XNT, 32768, BF16).rearrange("p (k t) -> p k t", k=8)
    ocat = sb(OCAT, 32768, BF16).rearrange("p (k t) -> p k t", k=8)

    def A(eng, fn, reads=(), writes=(), dma=False, tag=None, extra_deps=()):
        return sch.add(eng, fn, reads, writes, dma, tag, extra_deps)

    def dma(q, out, in_, reads=(), writes=(), extra_deps=(), **kw):
        return A(q, lambda e, o=out, i=in_, kw=kw: e.dma_start(out=o, in_=i, **kw), reads, writes, dma=True, extra_deps=extra_deps)

    def mm(out, lhsT, rhs, start, stop):
        A("pe", lambda e, o=out, l=lhsT, r=rhs, s0=start, s1=stop: e.matmul(o, l, r, start=s0, stop=s1),
          [lhsT, rhs], [out])

    def tr(out, in_, idn):
        A("pe", lambda e, o=out, i=in_, d=idn: e.transpose(o, i, d), [in_, idn], [out])

    def act(out, in_, func, bias=None, scale=None, accum=None):
        rd = [in_]
        kw = {}
        if bias is not None:
            kw["bias"] = bias
            if not isinstance(bias, float):
                rd.append(bias)
        if scale is not None:
            kw["scale"] = scale
            if not isinstance(scale, float):
                rd.append(scale)
        wr_ = [out]
        if accum is not None:
            kw["accum_out"] = accum
            wr_.append(accum)
        A("act", lambda e, o=out, i=in_, f=func, kw=kw: e.activation(o, i, f, **kw), rd, wr_)

    def ts(eng, out, in0, s1, s2, op0, op1=None, accum=None):
        rd = [in0]
        for s_ in (s1, s2):
            if s_ is not None and not isinstance(s_, (float, int)):
                rd.append(s_)
        wr_ = [out]
        kw = {}
        if accum is not None:
            kw["accum_out"] = accum
            wr_.append(accum)
        if op1 is None:
            A(eng, lambda e, o=out, i=in0, a=s1, p=op0, kw=kw: e.tensor_scalar(o, i, a, None, p, **kw), rd, wr_)
        else:
            A(eng, lambda e, o=out, i=in0, a=s1, b=s2, p=op0, q=op1, kw=kw: e.tensor_scalar(o, i, a, b, p, q, **kw), rd, wr_)

    def tt(eng, out, in0, in1, op):
        A(eng, lambda e, o=out, a=in0, b=in1, p=op: e.tensor_tensor(o, a, b, p), [in0, in1], [out])

    def stt(out, in0, scalar, in1, op0, op1, accum=None):
        rd = [in0, in1]
        if not isinstance(scalar, (float, int)):
            rd.append(scalar)
        wr_ = [out]
        kw = {}
        if accum is not None:
            kw["accum_out"] = accum
            wr_.append(accum)
        A("dve", lambda e, o=out, a=in0, s_=scalar, b=in1, p=op0, q=op1, kw=kw:
          e.scalar_tensor_tensor(o, a, s_, b, p, q, **kw), rd, wr_)

    def cp(eng, out, in_):
        if eng == "act":
            A(eng, lambda e, o=out, i=in_: e.copy(o, i), [in_], [out])
        else:
            A(eng, lambda e, o=out, i=in_: e.tensor_copy(o, i), [in_], [out])

    def memset(eng, out, val):
        A(eng, lambda e, o=out, v=val: e.memset(o, v), [], [out])

    def recip(out, in_):
        A("dve", lambda e, o=out, i=in_: e.reciprocal(o, i), [in_], [out])

    dma("sp", ident, ci_d, writes=[ident])
    dma("sp", maskfox, cmf_d, writes=[maskfox])
    dma("sp", maskhg, cmh_d, writes=[maskhg])
    dma("sp", cvecA[0:65, :], cva_d, writes=[cvecA[0:65, :]])
    dma("sp", cvecB, cvb_d, writes=[cvecB])
    dma("sp", sel[0:97, :, :], csel_d.rearrange("p (a b) -> p a b", a=16), writes=[sel[0:97, :, :]])
    dma("sp", gattn, gattn_d.partition_broadcast(128), writes=[gattn])
    dma("sp", gffn, gffn_d.partition_broadcast(128), writes=[gffn])
    dma("sp", lstrict, cls_d, writes=[lstrict])
    dma("sp", thr, cthr_d, writes=[thr])
    dma("sp", jt, cjt_d, writes=[jt])
    dma("sp", pcol2, cp2_d, writes=[pcol2])
    dma("sp", brt, br_d.partition_broadcast(128), writes=[brt])
    nonc = dict(allow_slow_non_contiguous=True)
    dma("sp", l0, lbl_d[0:1, :].rearrange("o (h p) -> p (o h)", p=128), writes=[l0], **nonc)
    dma("sp", l1, lbl_d[1:2, :].rearrange("o (h p) -> p (o h)", p=128), writes=[l1], **nonc)
    dma("sp", hgg, hgn_d.rearrange("o p -> p o"), writes=[hgg], **nonc)
    dma("sp", foxg[0:64, :], fxn_d.rearrange("o p -> p o"), writes=[foxg[0:64, :]], **nonc)
    dma("sp", foxg[64:128, :], fxn_d.rearrange("o p -> p o"), writes=[foxg[64:128, :]], **nonc)
    memset("dve", negb, 0.0)
    for g in range(3):
        dma("sp", negb[32 * g:32 * g + 8, :], fb_d.rearrange("o p -> p o"), writes=[negb[32 * g:32 * g + 8, :]], **nonc)
    dma("pool", wout, wout_d.rearrange("(k p) c -> p k c", p=128), writes=[wout])
    dma("pool", wr, wr_d.rearrange("(k p) c -> p k c", p=128), writes=[wr])
    memset("dve", ones, 1.0)
    memset("dve", reset, 1.0)
    memset("dve", reset.rearrange("p (c j) -> p c j", j=64)[:, :, 0:1], 0.0)
    memset("dve", cmathg, 1.0 / 128)
    tt("dve", ltmp, l1, l0, ALU.subtract)
    act(ltmp, ltmp, AF.Exp)
    ts("dve", ltmp, ltmp, 1.0, None, ALU.add)
    recip(lbc, ltmp)
    ts("dve", oml, lbc, -1.0, 1.0, ALU.mult, ALU.add)

    def rms_rstd(src, junk, col):
        ssq = stat[:, col:col + 1]
        var = stat[:, col + 1:col + 2]
        lnv = stat[:, col + 2:col + 3]
        rstd = stat[:, col + 3:col + 4]
        act(junk, src, AF.Square, accum=ssq)
        ts("dve", var, ssq, 1.0 / D, EPS, ALU.mult, ALU.add)
        act(lnv, var, AF.Ln)
        act(rstd, lnv, AF.Exp, scale=-0.5)
        return rstd

    pbank = [0]

    def next_bank(lo, hi):
        b = lo + (pbank[0] % (hi - lo))
        pbank[0] += 1
        return b

    for sq_ in range(nseq):
        xt_s = [sb(W + 77056 + 4096 * i, 4096, F32) for i in range(2)]
        junk = sb(W + 85248, 2048, BF16)
        xs_s = [sb(W + 87296 + 2048 * i, 2048, BF16) for i in range(2)]
        wfox = sb(W, 24704, BF16).rearrange("p (k c) -> p k c", k=8)
        dma("pool", wfox, win_d.rearrange("(k p) c -> p k c", p=128)[:, :, 2048:INC], writes=[wfox])
        for i in range(NT):
            xt = xt_s[i % 2]
            xs = xs_s[i % 2]
            dma("sp", xt, x_d[sq_, i * 128:(i + 1) * 128, :], writes=[xt])
            rstd = rms_rstd(xt, junk, 4 * (i % 8))
            stt(xs, xt, rstd, gattn, ALU.mult, ALU.mult)
            b = next_bank(0, 2)
            pb = bankbf(b).rearrange("p (k t) -> p k t", k=8)
            for kc in range(8):
                tr(pb[:, kc, :], xs[:, kc * 128:(kc + 1) * 128], ident)
            cp("act" if i % 2 else "dve", xnT[:, :, i * 128:(i + 1) * 128], pb)
        if stop_after == "prep":
            break

        vaug = sb(W + 24704, 24704, BF16).rearrange("p (t c) -> p t c", t=NT)
        qa_s = [sb(W + 49408 + 4096 * i, 4096, BF16) for i in range(2)]
        ka_s = [sb(W + 57600 + 4096 * i, 4096, BF16) for i in range(2)]
        parts = sb(W + 65792, 4096, BF16)
        pT_s = [sb(W + 69888 + 1024 * i, 1024, BF16) for i in range(2)] + [sb(W + 94592, 1024, BF16)]
        sqf = sb(W + 71936, 1024, BF16)
        lnr = sb(W + 72960, 2048, F32)
        rr = sb(W + 75008, 2048, F32)
        fr0 = sb(W + 77056, 8192, F32)
        fr1 = sb(W + 85248, 8192, F32)

        def vcol(h):
            return (h // 2) * 193 + (0 if h % 2 == 0 else 65)

        wf3 = sb(W + 93440, 1152, BF16).rearrange("p (k c) -> p k c", k=8)
        hiT = sb(W + 69888, 4096, BF16)
        memset("dve", parts, 1.0)
        memset("pool", wf3, 0.0)
        memset("pool", vaug, 0.0)
        for h in range(8):
            c1 = vcol(h) + (64 if h % 2 == 0 else 0)
            memset("dve", vaug[:, :, c1:c1 + 1], 1.0)
        for g in range(3):
            cp("dve", wf3[:, :, 32 * g:32 * g + 8], wfox[:, :, 1536:1544])
        for tb in range(4):
            b = next_bank(0, 2)
            pf = banks[b][0:72, :]
            for kc in range(8):
                mm(pf, wf3[:, kc, :], xnT[:, kc, tb * 512:(tb + 1) * 512], kc == 0, kc == 7)
            act(fr0[0:72, tb * 512:(tb + 1) * 512], pf, AF.Identity, bias=negb[0:72, :])
        f0 = fr0[0:72, :]
        f1 = fr1[0:72, :]
        h72 = hiT[0:72, :]
        chain = [
            lambda: ts("dve", f1, f0, -80.0, None, ALU.max),
            lambda: act(f1, f1, AF.Exp, scale=-1.0),
            lambda: act(f1, f1, AF.Ln, bias=1.0),
            lambda: ts("dve", f0, f1, -1.0, None, ALU.mult),
            lambda: A("dve", lambda e, o=f1, a=ones[0:72, :], b_=f0: e.tensor_tensor_scan(o, a, b_, 0.0, ALU.mult, ALU.add),
                      [ones[0:72, :], f0], [f1]),
            lambda: cp("dve", h72, f1),
            lambda: cp("dve", parts[0:8, :], hiT[0:8, :]),
            lambda: tt("dve", f0, f1, h72, ALU.subtract),
            lambda: cp("dve", h72, f0),
            lambda: cp("dve", parts[32:40, :], hiT[32:40, :]),
            lambda: tt("dve", f1, f0, h72, ALU.subtract),
            lambda: cp("dve", parts[64:72, :], fr1[64:72, :]),
        ]
        for i in range(NT):
            b = next_bank(0, 2)
            pv = banks[b]
            for kc in range(8):
                mm(pv[:, :], xnT[:, kc, i * 128:(i + 1) * 128], wfox[:, kc, 1024:1536], kc == 0, kc == 7)
            pv4 = pv[:, :].rearrange("p (h two d) -> p h two d", two=2, d=64)
            vg = vaug[:, i, :].rearrange("p (h c) -> p h c", c=193)
            cp("dve", vg[:, :, 0:64], pv4[:, :, 0, :])
            cp("act", vg[:, :, 129:193], pv4[:, :, 1, :])
            if chain:
                chain.pop(0)()
        while chain:
            chain.pop(0)()

        fox_pending = []
        lgcnt = [0]

        def proj_groups(h):
            qa_ = qa_s[h % 2]
            ka_ = ka_s[h % 2]
            out = []
            for tb in range(4):
                for which in (0, 1):
                    st = {}

                    def c0_(tb=tb, which=which, h=h, st=st):
                        cs = slice(tb * 512, (tb + 1) * 512)
                        st["pq"] = banks[next_bank(0, 2)]
                        pq = st["pq"]
                        mm(pq[0:70, :], sel[0:97, which * 8 + h, :], parts[0:97, cs], True, False)
                        wc = which * 512 + h * 64
                        for kc in range(0, 2):
                            mm(pq[0:64, :], wfox[:, kc, wc:wc + 64], xnT[:, kc, cs], False, False)

                    def c1_(tb=tb, which=which, h=h, st=st):
                        cs = slice(tb * 512, (tb + 1) * 512)
                        pq = st["pq"]
                        wc = which * 512 + h * 64
                        for kc in range(2, 5):
                            mm(pq[0:64, :], wfox[:, kc, wc:wc + 64], xnT[:, kc, cs], False, False)

                    def c2_(tb=tb, which=which, h=h, st=st, qa_=qa_, ka_=ka_):
                        cs = slice(tb * 512, (tb + 1) * 512)
                        pq = st["pq"]
                        wc = which * 512 + h * 64
                        for kc in range(5, 8):
                            mm(pq[0:64, :], wfox[:, kc, wc:wc + 64], xnT[:, kc, cs], False, kc == 7)
                        if which == 0:
                            ts("dve", qa_[0:70, cs], pq[0:70, :], 0.125, None, ALU.mult)
                        else:
                            cp("dve", ka_[0:70, cs], pq[0:70, :])
                    out += [c0_, c1_, c2_]
            return out

        for g_ in proj_groups(0):
            g_()
        for h in range(8):
            qa = qa_s[h % 2]
            ka = ka_s[h % 2]
            odd = h % 2
            nxt = proj_groups(h + 1) if h + 1 < 8 else []
            it_cnt = [0]
            vc = vcol(h)
            vw = 65 if not odd else 128
            for tb in range(4):
                t0 = tb * 512
                ob = 5 + (h * 4 + tb) % 2
                n_s = 4 * (tb + 1)
                orow = slice(0, 65) if not odd else slice(0, 128)
                lgb = {}

                def emit_qk(j, t0=t0, ka=ka, qa=qa, lgb=lgb):
                    s0 = j * 128
                    c0 = max(0, s0 - t0)
                    diag = s0 >= t0
                    lb_ = 2 + lgcnt[0] % 3
                    lgcnt[0] += 1
                    lgb[j] = lb_
                    lg = banks[lb_]
                    mm(lg[:, c0:512], ka[0:70, s0:s0 + 128], qa[0:70, t0 + c0:t0 + 512], True, not diag)
                    if diag:
                        mm(lg[:, c0:c0 + 128], ident, maskfox, False, True)

                emit_qk(0)
                if n_s > 1:
                    emit_qk(1)
                for j in range(n_s):
                    if j + 2 < n_s:
                        emit_qk(j + 2)
                    s0 = j * 128
                    c0 = max(0, s0 - t0)
                    lg = banks[lgb[j]]
                    pT = pT_s[j % 3]
                    act(pT[:, c0:512], lg[:, c0:512], AF.Exp)
                    mm(banks[ob][orow, c0:512], vaug[:, j, vc:vc + vw], pT[:, c0:512], j == 0, j == n_s - 1)
                    if j == min(1, n_s - 1) and fox_pending:
                        fox_pending.pop()()
                    it_cnt[0] += 1
                    if False and nxt:
                        nxt.pop(0)()

                def norm_ops(ob=ob, odd=odd, h=h, t0=t0):
                    po = banks[ob]
                    if not odd:
                        act(sqf[0:65, :], po[0:65, :], AF.Square)
                        mm(banks[7][0:64, :], cvecA[0:65, :], sqf[0:65, :], True, True)
                        prow = slice(0, 64)
                    else:
                        act(sqf[:, :], po[:, :], AF.Square)
                        mm(banks[7][:, :], cvecB[:, :], sqf[:, :], True, True)
                        prow = slice(64, 128)
                    act(lnr[prow, :], banks[7][prow, :], AF.Ln)
                    act(rr[prow, :], lnr[prow, :], AF.Exp, scale=-0.5)
                    stt(ocat[prow, 4 + h // 2, t0:t0 + 512], po[prow, :], foxg[prow, :], rr[prow, :], ALU.mult, ALU.mult)

                fox_pending.append(norm_ops)
            while nxt:
                nxt.pop(0)()
        while fox_pending:
            fox_pending.pop()()
        if stop_after == "fox":
            break

        whg = sb(W, 32768, BF16).rearrange("p (k c) -> p k c", k=8)
        dma("pool", whg, win_d.rearrange("(k p) c -> p k c", p=128)[:, :, 0:2048], writes=[whg])
        T0 = sb(W + 32768, 8192, F32)
        T1 = sb(W + 40960, 8192, F32)
        T2 = sb(W + 49152, 8192, F32)
        T3 = sb(W + 57344, 8192, F32)
        qtl = sb(W + 65536, 4096, BF16)
        ktl = sb(W + 69632, 4096, BF16)
        ktok = sb(W + 73728, 4096, BF16).rearrange("p (t k) -> p t k", t=NT)
        vtok = sb(W + 77824, 4096, BF16).rearrange("p (t k) -> p t k", t=NT)
        sgT = sb(W + 81920, 4096, BF16)
        Sbf = sb(W + 86016, 8192, BF16).rearrange("p (c v) -> p c v", c=32)
        U_s = [sb(W + 95744 + 512 * i, 512, F32) for i in range(4)]
        scm_s = [sb(W + 95232 + 256 * i, 256, BF16) for i in range(2)]
        lnr2 = sb(W + 40960, 2048, F32)
        rr2 = sb(W + 40960 + 2048, 2048, F32)
        t1b = sb(W + 40960 + 4096, 2048, F32)
        sqh = sb(W + 40960 + 6144, 1024, BF16)
        for h in range(4):
            hs = slice(h * 128, (h + 1) * 128)
            def hproj(coff, dst, fn_, h=h):
                for tb in range(4):
                    cs = slice(tb * 512, (tb + 1) * 512)
                    b = next_bank(0, 2)
                    pp = banks[b]
                    for kc in range(8):
                        mm(pp[:, :], whg[:, kc, coff + h * 128:coff + (h + 1) * 128], xnT[:, kc, cs], kc == 0, kc == 7)
                    act(dst[:, cs], pp[:, :], fn_)

            hproj(512, T1, AF.Sigmoid)
            ts("dve", T1, T1, oml[:, h:h + 1], lbc[:, h:h + 1], ALU.mult, ALU.add)
            hproj(0, T0, AF.Silu)
            act(T2, T1, AF.Ln)
            ts("dve", T1, T1, -1.0, 1.0, ALU.mult, ALU.add)
            A("dve", lambda e, o=T3, a=reset, b_=T2: e.tensor_tensor_scan(o, a, b_, 0.0, ALU.mult, ALU.add),
              [reset, T2], [T3])
            hproj(1536, sgT, AF.Silu)
            act(T2, T3, AF.Exp)
            act(T3, T3, AF.Exp, scale=-1.0)
            tt("dve", ktl, T1, T3, ALU.mult)
            for i4 in range(4):
                b = next_bank(0, 2)
                pp = banks[b]
                for ii in range(4):
                    i = i4 * 4 + ii
                    for kc in range(8):
                        mm(pp[:, ii * 128:(ii + 1) * 128], xnT[:, kc, i * 128:(i + 1) * 128],
                           whg[:, kc, 1024 + h * 128:1024 + (h + 1) * 128], kc == 0, kc == 7)
                cp("dve", vtok[:, i4 * 4:(i4 + 1) * 4, :], pp[:, :].rearrange("p (t k) -> p t k", t=4))
            tt("dve", qtl, T0, T2, ALU.mult)
            for i8 in range(2):
                b = next_bank(0, 2)
                pb = bankbf(b).rearrange("p (t k) -> p t k", t=8)
                for ii in range(8):
                    i = i8 * 8 + ii
                    tr(pb[:, ii, :], ktl[:, i * 128:(i + 1) * 128], ident)
                cp("act", ktok[:, i8 * 8:(i8 + 1) * 8, :], pb)
            for c in range(32):
                i, par = c // 2, c % 2
                if c % 4 == 0:
                    sb_ = next_bank(2, 5)
                dS = banks[sb_][:, (c % 4) * 128:(c % 4 + 1) * 128]
                ps_ = slice(par * 64, par * 64 + 64)
                mm(dS, ktok[ps_, i, :], vtok[ps_, i, :], True, True)
                Uc = U_s[c % 4]
                Up = U_s[(c + 3) % 4]
                if c == 0:
                    memset("pool", Sbf[:, 0, :], 0.0)
                    cp("dve", Uc, dS)
                else:
                    Dp = T2[:, (c - 1) * 64 + 63:(c - 1) * 64 + 64]
                    act(Sbf[:, c, :], Up, AF.Copy, scale=Dp)
                    stt(Uc, Up, Dp, dS, ALU.mult, ALU.add)
            for tb in range(4):
                ob = 5 + (h * 4 + tb) % 2
                po = banks[ob]
                scb = {}

                def emit_sc(ii, tb=tb, scb=scb):
                    i = tb * 4 + ii
                    cs = slice(i * 128, (i + 1) * 128)
                    lb_ = next_bank(2, 5)
                    sc = banks[lb_][:, 0:128]
                    mm(sc, ktl[:, cs], qtl[:, cs], True, True)
                    scm = scm_s[i % 2]
                    tt("dve", scm, sc, maskhg, ALU.mult)
                    scb[ii] = scm

                emit_sc(0)
                for ii in range(4):
                    i = tb * 4 + ii
                    if ii + 1 < 4:
                        emit_sc(ii + 1)
                    scm = scb[ii]
                    oo = po[:, ii * 128:(ii + 1) * 128]
                    mm(oo, vtok[:, i, :], scm, True, False)
                    mm(oo[:, 0:64], Sbf[:, 2 * i, :], qtl[:, i * 128:i * 128 + 64], False, False)
                    mm(oo[:, 64:128], Sbf[:, 2 * i + 1, :], qtl[:, i * 128 + 64:i * 128 + 128], False, True)
                act(sqh, po[:, :], AF.Square)
                mm(banks[7][:, :], cmathg, sqh, True, True)
                act(lnr2, banks[7][:, :], AF.Ln, bias=EPS)
                act(rr2, lnr2, AF.Exp, scale=-0.5)
                stt(t1b, po[:, :], hgg, rr2, ALU.mult, ALU.mult)
                tt("pool", ocat[:, h, tb * 512:(tb + 1) * 512], t1b, sgT[:, tb * 512:(tb + 1) * 512], ALU.mult)
        if stop_after == "hg":
            break

        hb_s = [sb(W + 4096 * i, 4096, F32) for i in range(2)]
        xt2_s = [sb(W + 8192 + 4096 * i, 4096, F32) for i in range(2)]
        junk2 = sb(W + 16384, 2048, BF16)
        hn_s = [sb(W + 18432 + 2048 * i, 2048, BF16) for i in range(2)]
        hnt_s = [sb(W + 22528 + 2048 * i, 2048, BF16).rearrange("p (k t) -> p k t", k=8) for i in range(2)]
        for i in range(NT):
            gi = sq_ * NT + i
            rows = slice(gi * 128, (gi + 1) * 128)
            xt = xt2_s[i % 2]
            hb = hb_s[i % 2]
            if i == 0:
                dma("sp", xt, x_d[sq_, 0:128, :], writes=[xt])
            if i + 1 < NT:
                dma("sp", xt2_s[(i + 1) % 2], x_d[sq_, (i + 1) * 128:(i + 2) * 128, :], writes=[xt2_s[(i + 1) % 2]])
            for half in range(2):
                b = next_bank(0, 2)
                ph = banks[b]
                for fc in range(8):
                    mm(ph[:, :], ocat[:, fc, i * 128:(i + 1) * 128], wout[:, fc, half * 512:(half + 1) * 512], fc == 0, fc == 7)
                tt("dve", hb[:, half * 512:(half + 1) * 512], ph[:, :], xt[:, half * 512:(half + 1) * 512], ALU.add)
            dma("sp", h_scr[rows, :], hb, reads=[hb], writes=[h_scr[rows, :]])
            rstd = rms_rstd(hb, junk2, 32 + 4 * (i % 8))
            hn = hn_s[i % 2]
            stt(hn, hb, rstd, gffn, ALU.mult, ALU.mult)
            dma("sp", hn_scr[rows, :], hn, reads=[hn], writes=[hn_scr[rows, :]])
            b = next_bank(2, 4)
            pb = bankbf(b).rearrange("p (k t) -> p k t", k=8)
            for kc in range(8):
                tr(pb[:, kc, :], hn[:, kc * 128:(kc + 1) * 128], ident)
            hnt = hnt_s[i % 2]
            cp("act", hnt, pb)
            b = next_bank(4, 6)
            pr = banks[b][:, 0:20]
            for kc in range(8):
                mm(pr, hnt[:, kc, :], wr[:, kc, :], kc == 0, kc == 7)
            ro = (i % 2) * 128
            lgt = rt[:, ro:ro + 20]
            gmax = rt[:, ro + 20:ro + 21]
            ngmax = rt[:, ro + 21:ro + 22]
            gm = rt[:, ro + 22:ro + 26]
            eg = rt[:, ro + 26:ro + 30]
            sumg = rt[:, ro + 30:ro + 31]
            pgs = rt[:, ro + 31:ro + 32]
            pen = rt[:, ro + 32:ro + 48]
            elm = rt[:, ro + 48:ro + 64]
            top8 = rt[:, ro + 64:ro + 72]
            nm1 = rt[:, ro + 72:ro + 73]
            selm = rt[:, ro + 73:ro + 89]
            den2 = rt[:, ro + 89:ro + 90]
            ex = rt[:, ro + 91:ro + 107]
            tt("dve", lgt, pr, brt, ALU.add)
            A("dve", lambda e, o=gmax, i_=lgt[:, 0:4]: e.reduce_max(o, i_, AX.X), [lgt[:, 0:4]], [gmax])
            ts("dve", gm, lgt[:, 0:4], gmax, None, ALU.is_equal)
            ts("dve", ngmax, gmax, -1.0, None, ALU.mult)
            act(eg, lgt[:, 0:4], AF.Exp, bias=ngmax, accum=sumg)
            recip(pgs, sumg)
            gm_b = bass.AP(gm.tensor, gm.offset, [list(gm.ap[0]), [1, 4], [0, 4]])
            pen3 = pen.rearrange("p (g j) -> p g j", j=4)
            ts("dve", pen3, gm_b, -1.0, 1e30, ALU.add, ALU.mult)
            tt("dve", elm, lgt[:, 4:20], pen, ALU.add)
            A("dve", lambda e, o=top8, i_=elm: e.max(o, i_), [elm], [top8])
            ts("dve", selm, elm, top8[:, 1:2], None, ALU.is_ge)
            cp("dve", M2b[:, gi * 16:(gi + 1) * 16], selm)
            ts("dve", M1b[:, gi * 16:(gi + 1) * 16], elm, top8[:, 0:1], None, ALU.is_equal)
            ts("dve", nm1, top8[:, 0:1], -1.0, None, ALU.mult)
            act(ex, elm, AF.Exp, bias=nm1)
            stt(ex, ex, 1.0, selm, ALU.mult, ALU.mult, accum=den2)
            recip(den2, den2)
            tt("dve", w1s[:, gi:gi + 1], den2, pgs, ALU.mult)
            tt("dve", w2s[:, gi:gi + 1], pgs, w1s[:, gi:gi + 1], ALU.subtract)

    if stop_after is None:
        NTT16 = NTT * 16
        def f32t(k):
            return sb(XNT + 4096 * k, 4096, F32)[:, 0:NTT16]
        TotS, RS, CI, EX, PP, TMP = [f32t(k) for k in range(6)]
        small2 = sb(XNT + 4096 * 6, 4096, F32)
        ne = small2[:, 0:16]
        ke = small2[:, 16:32]
        cki = small2[:, 32:48]
        base = small2[:, 48:64]
        cmp_ = small2[:, 64:320]
        ejf = small2[:, 320:384]
        cmp2 = sb(XNT + 4096 * 7, 4096, F32)
        pos1f = small2[:, 384:448]
        pos2f = small2[:, 448:512]
        idf0 = small2[:, 512:576]
        idf1 = small2[:, 576:640]
        for half in range(0, NTT16, 512):
            n_ = min(512, NTT16 - half)
            mm(banks[0][:, 0:n_], lstrict, M2b[:, half:half + n_], True, True)
            cp("dve", RS[:, half:half + n_], banks[0][:, 0:n_])
            mm(banks[1][:, 0:n_], ones[:, 0:128], M2b[:, half:half + n_], True, True)
            cp("dve", TotS[:, half:half + n_], banks[1][:, 0:n_])
        TotS3 = TotS.rearrange("p (t e) -> p t e", e=16)
        CI3 = CI.rearrange("p (t e) -> p t e", e=16)
        for e_ in range(16):
            A("dve", lambda e, o=CI3[:, :, e_], a=ones[:, 0:NTT], b_=TotS3[:, :, e_]:
              e.tensor_tensor_scan(o, a, b_, 0.0, ALU.mult, ALU.add), [ones[:, 0:NTT], TotS3[:, :, e_]], [CI3[:, :, e_]])
        tt("dve", EX, CI, TotS, ALU.subtract)
        cp("dve", ne, CI3[:, NTT - 1, :])
        ne_b = bass.AP(ne.tensor, ne.offset, [list(ne.ap[0]), [1, 16], [0, 16]])
        thr_b = bass.AP(thr.tensor, thr.offset, [list(thr.ap[0]), [0, 16], [1, 16]])
        tt("dve", cmp_.rearrange("p (a b) -> p a b", b=16), ne_b, thr_b, ALU.is_gt)
        A("dve", lambda e, o=ke, i_=cmp_.rearrange("p (a b) -> p a b", b=16): e.reduce_sum(o, i_, AX.X),
          [cmp_], [ke])
        A("dve", lambda e, o=cki, a=ones[:, 0:16], b_=ke: e.tensor_tensor_scan(o, a, b_, 0.0, ALU.mult, ALU.add),
          [ones[:, 0:16], ke], [cki])
        tt("dve", base, cki, ke, ALU.subtract)
        ts("dve", base, base, 512.0, None, ALU.mult)
        base_b = bass.AP(base.tensor, base.offset, [list(base.ap[0]), [0, NTT], [1, 16]])
        tt("dve", PP, RS, EX, ALU.add)
        PP3 = PP.rearrange("p (t e) -> p t e", e=16)
        tt("dve", PP3, PP3, base_b, ALU.add)
        tt("dve", TMP, PP, M1b[:, 0:NTT16], ALU.mult)
        A("dve", lambda e, o=pos1f[:, 0:NTT], i_=TMP.rearrange("p (t e) -> p t e", e=16): e.reduce_sum(o, i_, AX.X),
          [TMP], [pos1f[:, 0:NTT]])
        tt("dve", TMP, PP, M2b[:, 0:NTT16], ALU.mult)
        A("dve", lambda e, o=pos2f[:, 0:NTT], i_=TMP.rearrange("p (t e) -> p t e", e=16): e.reduce_sum(o, i_, AX.X),
          [TMP], [pos2f[:, 0:NTT]])
        tt("dve", pos2f[:, 0:NTT], pos2f[:, 0:NTT], pos1f[:, 0:NTT], ALU.subtract)
        cp("dve", pos1i[:, 0:NTT], pos1f[:, 0:NTT])
        cp("dve", pos2i[:, 0:NTT], pos2f[:, 0:NTT])
        cki_b = bass.AP(cki.tensor, cki.offset, [list(cki.ap[0]), [0, NSLOT], [1, 16]])
        jt_b = bass.AP(jt.tensor, jt.offset, [list(jt.ap[0]), [1, NSLOT], [0, 16]])
        c23 = cmp2[:, 0:NSLOT * 16].rearrange("p (j e) -> p j e", e=16)
        tt("dve", c23, cki_b, jt_b, ALU.is_le)
        A("dve", lambda e, o=ejf[:, 0:NSLOT], i_=c23: e.reduce_sum(o, i_, AX.X), [cmp2[:, 0:NSLOT * 16]], [ejf[:, 0:NSLOT]])
        ts("dve", ejf[:, 0:NSLOT], ejf[:, 0:NSLOT], 15.0, None, ALU.min)
        ts("dve", idf0[:, 0:NSLOT], ejf[:, 0:NSLOT], 256.0, pcol2, ALU.mult, ALU.add)
        ts("dve", idf1[:, 0:NSLOT], idf0[:, 0:NSLOT], 1.0, None, ALU.add)
        cp("dve", idxW0[:, 0:NSLOT], idf0[:, 0:NSLOT])
        cp("dve", idxW1[:, 0:NSLOT], idf1[:, 0:NSLOT])

        def idma(out, in_, out_off=None, in_off=None, reads=(), writes=()):
            def fn(e, o=out, i=in_, oo=out_off, io=in_off):
                return e.indirect_dma_start(
                    out=o, out_offset=(bass.IndirectOffsetOnAxis(ap=oo, axis=0) if oo is not None else None),
                    in_=i, in_offset=(bass.IndirectOffsetOnAxis(ap=io, axis=0) if io is not None else None))
            return A("pool", fn, reads, writes, dma=True)

        hnc_s = [sb(XNT + 110592 + 2048 * i, 2048, BF16) for i in range(16)]
        scat_ops = []
        for gi in range(NTT):
            hnc = hnc_s[gi % 16]
            rows = slice(gi * 128, (gi + 1) * 128)
            dma("sp", hnc, hn_scr[rows, :], reads=[hn_scr[rows, :]], writes=[hnc])
            scat_ops.append(idma(so_scr[:, :], hnc, out_off=pos1i[:, gi:gi + 1], reads=[hnc, pos1i[:, gi:gi + 1]]))
            scat_ops.append(idma(so_scr[:, :], hnc, out_off=pos2i[:, gi:gi + 1], reads=[hnc, pos2i[:, gi:gi + 1]]))

        D0 = XNT

        def d_views(j):
            wo = D0 + 24576 * (j % 2)
            wg = sb(wo, 8192, BF16)
            wu = sb(wo + 8192, 8192, BF16)
            wd = sb(wo + 16384, 8192, BF16)
            xtok = sb(D0 + 49152 + 8192 * (j % 2), 8192, BF16).rearrange("p (a c) -> p a c", a=4)
            return wg, wu, wd, xtok

        def d_load(j):
            wg, wu, wd, xtok = d_views(j)
            srows = so_scr[j * 512:(j + 1) * 512, :]
            dma("sp", xtok, srows.rearrange("(a p) c -> p a c", p=128), reads=[srows], writes=[xtok], extra_deps=scat_ops)
            for (dst, src) in ((wg, wg_d), (wu, wu_d), (wd, wd_d)):
                idma(dst[:, 0:2048], src[:, :], in_off=idxW0[:, j:j + 1], reads=[idxW0[:, j:j + 1]], writes=[dst[:, 0:2048]])
                idma(dst[:, 2048:4096], src[:, :], in_off=idxW1[:, j:j + 1], reads=[idxW1[:, j:j + 1]], writes=[dst[:, 2048:4096]])

        d_load(0)
        for j in range(NSLOT):
            if j + 1 < NSLOT:
                d_load(j + 1)
            wg, wu, wd, xtok = d_views(j)
            wg3 = wg.rearrange("p (k c) -> p k c", k=8)
            wu3 = wu.rearrange("p (k c) -> p k c", k=8)
            wd3 = wd.rearrange("p (k c) -> p k c", k=4)
            hsT = sb(D0 + 65536 + 8192 * (j % 2), 8192, BF16).rearrange("p (k t) -> p k t", k=8)
            hact = sb(D0 + 81920 + 4096 * (j % 2), 4096, BF16).rearrange("p (c t) -> p c t", c=4)
            ybuf = sb(D0 + 94208 + 16384 * (j % 2), 16384, F32).rearrange("p (a c) -> p a c", a=4)
            for a in range(4):
                b = 6 + a % 2
                pb = bankbf(b).rearrange("p (k t) -> p k t", k=8)
                for kc in range(8):
                    tr(pb[:, kc, :], xtok[:, a, kc * 128:(kc + 1) * 128], ident)
                cp("act" if a % 2 else "dve", hsT[:, :, a * 128:(a + 1) * 128], pb)
            for hc in range(4):
                gb = hc % 2
                ub = 2 + hc % 2
                for kc in range(8):
                    mm(banks[gb][:, :], wg3[:, kc, hc * 128:(hc + 1) * 128], hsT[:, kc, :], kc == 0, kc == 7)
                for kc in range(8):
                    mm(banks[ub][:, :], wu3[:, kc, hc * 128:(hc + 1) * 128], hsT[:, kc, :], kc == 0, kc == 7)
                sgt = sb(D0 + 90112 + 2048 * (hc % 2), 2048, F32)
                act(sgt, banks[gb][:, :], AF.Silu)
                tt("dve", hact[:, hc, :], sgt, banks[ub][:, :], ALU.mult)
            for a in range(4):
                for half in range(2):
                    yb = 4 + (a * 2 + half) % 2
                    for hc in range(4):
                        mm(banks[yb][:, :], hact[:, hc, a * 128:(a + 1) * 128], wd3[:, hc, half * 512:(half + 1) * 512], hc == 0, hc == 3)
                    cp("act" if half else "dve", ybuf[:, a, half * 512:(half + 1) * 512], banks[yb][:, :])
            yrows = y_scr[j * 512:(j + 1) * 512, :]
            dma("sp", yrows.rearrange("(a p) c -> p a c", p=128), ybuf, reads=[ybuf], writes=[yrows])

        gfin = sb(XNT + 151552, 4096, F32)
        dma("sp", gfin, gfin_d.partition_broadcast(128), writes=[gfin])
        junk3 = sb(XNT + 155648, 2048, BF16)
        def e_views(gi):
            eo = XNT + 12288 * (gi % 8)
            return sb(eo, 4096, F32), sb(eo + 4096, 4096, F32), sb(eo + 8192, 4096, F32)

        def e_load(gi):
            y1, y2, hb = e_views(gi)
            rows = slice(gi * 128, (gi + 1) * 128)
            idma(y1, y_scr[:, :], in_off=pos1i[:, gi:gi + 1], reads=[y_scr[:, :], pos1i[:, gi:gi + 1]], writes=[y1])
            idma(y2, y_scr[:, :], in_off=pos2i[:, gi:gi + 1], reads=[y_scr[:, :], pos2i[:, gi:gi + 1]], writes=[y2])
            dma("sp", hb, h_scr[rows, :], reads=[h_scr[rows, :]], writes=[hb])

        for g0 in range(min(6, NTT)):
            e_load(g0)
        for gi in range(NTT):
            sq_, i = gi // NT, gi % NT
            if gi + 6 < NTT:
                e_load(gi + 6)
            y1, y2, hb = e_views(gi)
            stt(hb, y1, w1s[:, gi:gi + 1], hb, ALU.mult, ALU.add)
            stt(hb, y2, w2s[:, gi:gi + 1], hb, ALU.mult, ALU.add)
            rstd = rms_rstd(hb, junk3, 64 + 4 * (gi % 8))
            stt(hb, hb, rstd, gfin, ALU.mult, ALU.mult)
            dma("sp", out_d[sq_, i * 128:(i + 1) * 128, :], hb, reads=[hb])

    sch.prepare()
    sems = {k: es.enter_context(nc.semaphore(f"s_{k}")) for k in ("pe", "act", "dve", "pool")}
    dsems = {}
    for q in ("sp", "pool", "act"):
        for k in range(sch.n_dma_sems):
            dsems[(q, k)] = es.enter_context(nc.semaphore(f"d_{q}{k}"))
    with nc.Block() as block:
        @block.tensor
        def _(eng):
            sch.emit_engine("pe", eng, sems, dsems)

        @block.scalar
        def _(eng):
            sch.emit_engine("act", eng, sems, dsems)

        @block.vector
        def _(eng):
            sch.emit_engine("dve", eng, sems, dsems)

        @block.gpsimd
        def _(eng):
            sch.emit_engine("pool", eng, sems, dsems)

        @block.sync
        def _(eng):
            sch.emit_engine("sp", eng, sems, dsems)
    es.close()
    return nc, sch


_CACHE = {}


def _get_nc(nseq):
    if nseq not in _CACHE:
        _CACHE[nseq] = build(nseq, Sched, F32, BF16, U8)[0]
    return _CACHE[nseq]


def kernel(x, attn_norm, w_in, hg_lb_logits, hg_norm, fox_f_bias, fox_norm, w_out, ffn_norm,
           w_group, b_group, w_expert, b_expert, w_gate, w_up, w_down, final_norm):
    f = lambda a: np.ascontiguousarray(np.asarray(a, dtype=np.float32))
    x = f(x)
    n_cores = 8
    B = x.shape[0]
    nseq = B // n_cores
    shared = {
        "w_in": f(w_in)[0], "w_out": f(w_out)[0],
        "w_gate": np.ascontiguousarray(f(w_gate)[0].reshape(16, 8, 128, 512).transpose(0, 2, 1, 3)).reshape(16 * 256, 2048),
        "w_up": np.ascontiguousarray(f(w_up)[0].reshape(16, 8, 128, 512).transpose(0, 2, 1, 3)).reshape(16 * 256, 2048),
        "w_down": np.ascontiguousarray(f(w_down)[0].reshape(16, 4, 128, 1024).transpose(0, 2, 1, 3)).reshape(16 * 256, 2048),
        "w_router": np.ascontiguousarray(np.concatenate([f(w_group)[0], f(w_expert)[0]], axis=1)),
        "b_router": np.ascontiguousarray(np.concatenate([f(b_group)[0], f(b_expert)[0]])[None, :]),
        "attn_norm": f(attn_norm), "ffn_norm": f(ffn_norm), "final_norm": f(final_norm).reshape(1, -1),
        "hg_lb_logits": f(hg_lb_logits), "hg_norm": f(hg_norm), "fox_f_bias": f(fox_f_bias),
        "fox_norm": f(fox_norm),
    }
    shared.update(make_consts())
    in_maps = []
    for c in range(n_cores):
        m = dict(shared)
        m["x"] = np.ascontiguousarray(x[c * nseq:(c + 1) * nseq])
        in_maps.append(m)
    nc = _get_nc(nseq)
    res = run_bass_kernel_spmd(nc, in_maps, core_ids=list(range(n_cores)))
    return np.concatenate([r["out"] for r in res.results], axis=0).astype(np.float32)
```

```python
import contextlib
import numpy as np
import ml_dtypes
import concourse.bass as bass
import concourse.mybir as mybir

F32 = mybir.dt.float32
BF16 = mybir.dt.bfloat16
U8 = mybir.dt.uint8
_ES = {F32: 4, BF16: 2, U8: 1, mybir.dt.int32: 4, mybir.dt.uint32: 4, mybir.dt.uint16: 2}


class Op:
    __slots__ = ("eng", "fn", "deps", "cdeps", "seq", "ms", "needed", "dma", "dsem", "dval", "dprev", "tag")

    def __init__(self, eng, fn, dma, tag):
        self.eng = eng
        self.fn = fn
        self.deps = set()
        self.cdeps = {}
        self.seq = 0
        self.ms = None
        self.needed = False
        self.dma = dma
        self.dsem = None
        self.dval = None
        self.dprev = 0
        self.tag = tag


class Sched:
    def __init__(self, n_dma_sems=8, same_engine_sync=True):
        self.ops = []
        self.recs = {}
        self.n_dma_sems = n_dma_sems
        self.same_engine_sync = same_engine_sync
        self.dram_track = set()
        self.embed_wait = True
        self.whole = {}

    def _regions(self, ap):
        t = ap.tensor
        cls = type(t).__name__
        if cls.startswith("DRam"):
            name = ap.name
            if name not in self.dram_track:
                return None
            es = _ES[ap.dtype]
            pat = ap.ap
            off = int(ap.offset)
            hi = off + sum((n - 1) * abs(st) for st, n in pat) + 1
            return name, False, 0, 1, [(off * es, hi * es)]
        psum = cls.startswith("PSum") or cls.startswith("Psum")
        name = ap.name
        if psum:
            return name, True, 0, 128, [(0, 1 << 20)]
        es = _ES[ap.dtype]
        pat = ap.ap
        off = int(ap.offset)
        pstride, pcnt = pat[0]
        p0 = off // pstride
        c0 = off % pstride
        free = list(pat[1:])
        if not free:
            return name, False, p0, p0 + pcnt, [(c0 * es, (c0 + 1) * es)]
        ls, ln = free[-1]
        run = (ln - 1) * abs(ls) + 1
        outer = free[:-1]
        nout = 1
        for s, n in outer:
            nout *= n
        ivs = []
        if nout <= 64:
            idx = [0] * len(outer)
            while True:
                st = c0 + sum(i * s for i, (s, n) in zip(idx, outer))
                ivs.append((st * es, (st + run) * es))
                k = len(outer) - 1
                while k >= 0:
                    idx[k] += 1
                    if idx[k] < outer[k][1]:
                        break
                    idx[k] = 0
                    k -= 1
                if k < 0:
                    break
        else:
            hi = c0 + sum((n - 1) * abs(s) for s, n in outer) + run
            ivs.append((c0 * es, hi * es))
        ivs.sort()
        out = [ivs[0]]
        for a, b in ivs[1:]:
            if a <= out[-1][1]:
                out[-1] = (out[-1][0], max(out[-1][1], b))
            else:
                out.append((a, b))
        return name, False, p0, p0 + pcnt, out

    def _access(self, op, ap, is_write):
        r = self._regions(ap)
        if r is None:
            return
        name, psum, p0, p1, ivs = r
        tab = self.recs.setdefault(name, {})
        w = is_write or psum
        SH = 20 if name in self.dram_track else 11
        for (b0, b1) in ivs:
            newrec = (p0, p1, b0, b1, op, w)
            for bk in range(b0 >> SH, ((b1 - 1) >> SH) + 1):
                lst = tab.get(bk)
                if lst is None:
                    tab[bk] = [newrec]
                    continue
                keep = []
                for rec in lst:
                    rp0, rp1, rb0, rb1, rop, rw = rec
                    ov = rp0 < p1 and p0 < rp1 and rb0 < b1 and b0 < rb1
                    if ov and (rw or w) and rop is not op:
                        if rop.dma:
                            op.deps.add(rop)
                        else:
                            c = op.cdeps.get(rop.eng)
                            if c is None or c.seq < rop.seq:
                                op.cdeps[rop.eng] = rop
                    if ov and w and rp0 >= p0 and rp1 <= p1 and rb0 >= b0 and rb1 <= b1:
                        continue
                    if (not w) and (not rw) and rop.eng == op.eng and not rop.dma and not op.dma \
                            and rp0 == p0 and rp1 == p1 and rb0 == b0 and rb1 == b1:
                        continue
                    keep.append(rec)
                keep.append(newrec)
                tab[bk] = keep

    def add(self, eng, fn, reads=(), writes=(), dma=False, tag=None, extra_deps=()):
        op = Op(eng, fn, dma, tag)
        op.seq = len(self.ops)
        for ap in reads:
            self._access(op, ap, False)
        for ap in writes:
            self._access(op, ap, True)
        op.deps.update(op.cdeps.values())
        op.deps.update(extra_deps)
        op.cdeps = None
        self.ops.append(op)
        return op

    def prepare(self):
        for op in self.ops:
            for d in op.deps:
                if d.dma:
                    continue
                if d.eng == op.eng and (d.eng == "pe" or not self.same_engine_sync) and not op.dma:
                    continue
                d.needed = True
        cnt = {}
        dcnt = {}
        dslot = {}
        for op in self.ops:
            if op.dma:
                k = dslot.get(op.eng, 0)
                dslot[op.eng] = k + 1
                slot = (op.eng, k % self.n_dma_sems)
                prev = dcnt.get(slot, 0)
                op.dsem = slot
                op.dprev = prev
                op.dval = prev + 16
                dcnt[slot] = op.dval
            elif op.needed:
                cnt[op.eng] = cnt.get(op.eng, 0) + 1
                op.ms = cnt[op.eng]
        self.final_dma = dcnt
        self.ms_total = cnt

    def emit_engine(self, eng_name, eng, sems, dsems):
        waited = {}
        pend = []

        def need(sem_key, sem, val):
            if val <= 0:
                return
            if waited.get(sem_key, 0) >= val:
                return
            waited[sem_key] = val
            pend.append((sem, val))

        n = 0
        for op in self.ops:
            if op.eng != eng_name:
                continue
            for d in op.deps:
                if d.dma:
                    need(d.dsem, dsems[d.dsem], d.dval)
                else:
                    if d.eng == eng_name and not op.dma and (eng_name == "pe" or not self.same_engine_sync):
                        continue
                    need(d.eng, sems[d.eng], d.ms)
            if op.dma:
                need(op.dsem, dsems[op.dsem], op.dprev)
            embed = None
            if self.embed_wait and pend and not op.dma:
                embed = pend.pop()
            for (sm, vl) in pend:
                eng.wait_ge(sm, vl)
            del pend[:]
            ins = op.fn(eng)
            if embed is not None:
                ins._wait_ge(embed[0], embed[1])
            if op.dma:
                ins.then_inc(dsems[op.dsem], 16)
            elif op.needed:
                ins.then_inc(sems[eng_name], 1)
            n += 1
        for slot, val in self.final_dma.items():
            if slot[0] == eng_name:
                need(slot, dsems[slot], val)
        for (sm, vl) in pend:
            eng.wait_ge(sm, vl)
        return n


from concourse.bass_utils import run_bass_kernel_spmd

AF = mybir.ActivationFunctionType
ALU = mybir.AluOpType
AX = mybir.AxisListType
I32 = mybir.dt.int32

D = 1024
S = 2048
NT = S // 128
HGW = 512
FOXW = 512
INC = 3592
NE = 16
EH = 512
EPS = 1e-6

ARENA = 212000
PERS = 208000
C_IDENT = 0
C_MASKFOX = 256
C_MASKHG = 512
C_CMATHG = 768
C_CVECA = 1024
C_CVECB = 1152
C_SEL = 1408
C_ONES = 3648
C_RESET = 7744
C_GATTN = 11840
C_GFFN = 15936
C_GFIN = 20032
C_SMALL = 24128
C_WOUT = 24640
C_WR = 41024
C_STAT = 41344
C_GATES = 42368
C_RT = 43392
XNT = 44544
OCAT = XNT + 32768
W = OCAT + 32768
WSIZE = PERS - W
TT = W + 65536


def make_consts():
    bf = ml_dtypes.bfloat16
    ident = np.eye(128, dtype=np.float32).astype(bf)
    s_ = np.arange(128)[:, None]
    t_ = np.arange(128)[None, :]
    maskfox = np.where(t_ >= s_, 0.0, -30000.0).astype(np.float32).astype(bf)
    maskhg = ((t_ >= s_) & ((t_ // 64) == (s_ // 64))).astype(np.float32).astype(bf)
    cvecA = np.zeros((65, 64), np.float32)
    cvecA[0:64, :] = 1.0 / 64
    cvecA[64, :] = EPS
    cvecB = np.zeros((128, 128), np.float32)
    cvecB[0, 64:128] = EPS
    cvecB[64:128, 64:128] = 1.0 / 64
    sel = np.zeros((97, 16, 70), np.float32)
    for h in range(8):
        sel[h, h, 64] = 8.0
        sel[32 + h, h, 65] = 8.0
        sel[64 + h, h, 66] = 8.0
        sel[96, h, 67:70] = 8.0
        sel[96, 8 + h, 64:67] = 1.0
        sel[h, 8 + h, 67] = -1.0
        sel[32 + h, 8 + h, 68] = -1.0
        sel[64 + h, 8 + h, 69] = -1.0
    lstrict = (s_ < t_).astype(np.float32).astype(bf)
    thr = np.tile((512.0 * np.arange(16, dtype=np.float32))[None, :], (128, 1))
    jt = np.tile(np.arange(64, dtype=np.float32)[None, :], (128, 1))
    pcol2 = (2.0 * np.arange(128, dtype=np.float32)).reshape(128, 1)
    return {
        "c_lstrict": lstrict, "c_thr": thr, "c_jt": jt, "c_pcol2": pcol2,
        "c_ident": ident,
        "c_maskfox": maskfox,
        "c_maskhg": maskhg,
        "c_cveca": cvecA.astype(bf),
        "c_cvecb": cvecB.astype(bf),
        "c_sel": sel.reshape(97, 16 * 70).astype(bf),
    }


def build(nseq, Sched, F32, BF16, U8, stop_after=None, dbg=False):
    nc = bass.Bass("TRN2", target_bir_lowering=False)
    dt_ = nc.dram_tensor
    x_d = dt_("x", [nseq, S, D], F32, kind="ExternalInput").ap()
    win_d = dt_("w_in", [D, INC], F32, kind="ExternalInput").ap()
    wout_d = dt_("w_out", [D, D], F32, kind="ExternalInput").ap()
    wg_d = dt_("w_gate", [NE * 256, 2048], F32, kind="ExternalInput").ap()
    wu_d = dt_("w_up", [NE * 256, 2048], F32, kind="ExternalInput").ap()
    wd_d = dt_("w_down", [NE * 256, 2048], F32, kind="ExternalInput").ap()
    cls_d = dt_("c_lstrict", [128, 128], BF16, kind="ExternalInput").ap()
    cthr_d = dt_("c_thr", [128, 16], F32, kind="ExternalInput").ap()
    cjt_d = dt_("c_jt", [128, 64], F32, kind="ExternalInput").ap()
    cp2_d = dt_("c_pcol2", [128, 1], F32, kind="ExternalInput").ap()
    NTT = nseq * NT
    NSLOT = nseq * 8 + 16
    NROW = NSLOT * 512
    h_scr = dt_("h_scr", [NTT * 128, D], F32, kind="Internal").ap()
    hn_scr = dt_("hn_scr", [NTT * 128, D], BF16, kind="Internal").ap()
    so_scr = dt_("so_scr", [NROW, D], BF16, kind="Internal").ap()
    y_scr = dt_("y_scr", [NROW, D], F32, kind="Internal").ap()
    wr_d = dt_("w_router", [D, 20], F32, kind="ExternalInput").ap()
    br_d = dt_("b_router", [1, 20], F32, kind="ExternalInput").ap()
    gattn_d = dt_("attn_norm", [1, D], F32, kind="ExternalInput").ap()
    gffn_d = dt_("ffn_norm", [1, D], F32, kind="ExternalInput").ap()
    gfin_d = dt_("final_norm", [1, D], F32, kind="ExternalInput").ap()
    lbl_d = dt_("hg_lb_logits", [2, HGW], F32, kind="ExternalInput").ap()
    hgn_d = dt_("hg_norm", [1, 128], F32, kind="ExternalInput").ap()
    fb_d = dt_("fox_f_bias", [1, 8], F32, kind="ExternalInput").ap()
    fxn_d = dt_("fox_norm", [1, 64], F32, kind="ExternalInput").ap()
    ci_d = dt_("c_ident", [128, 128], BF16, kind="ExternalInput").ap()
    cmf_d = dt_("c_maskfox", [128, 128], BF16, kind="ExternalInput").ap()
    cmh_d = dt_("c_maskhg", [128, 128], BF16, kind="ExternalInput").ap()
    cva_d = dt_("c_cveca", [65, 64], BF16, kind="ExternalInput").ap()
    cvb_d = dt_("c_cvecb", [128, 128], BF16, kind="ExternalInput").ap()
    csel_d = dt_("c_sel", [97, 16 * 70], BF16, kind="ExternalInput").ap()
    out_d = dt_("out", [nseq, S, D], F32, kind="ExternalOutput").ap()
    dbg_d = None
    if dbg:
        dbg_d = dt_("dbg", [S, D], F32, kind="ExternalOutput").ap()

    sch = Sched(n_dma_sems=24)
    sch.dram_track.update(["h_scr", "hn_scr", "so_scr", "y_scr"])
    es = contextlib.ExitStack()
    arena = es.enter_context(nc.sbuf_tensor("arena", [128, ARENA], U8))
    banks = [es.enter_context(nc.psum_tensor(f"bank{i}", [128, 512], F32)) for i in range(8)]

    def sb(off, nbytes, dt):
        return arena[:, off:off + nbytes].bitcast(dt)

    def bankbf(i):
        return banks[i][:, :].bitcast(BF16)

    ident = sb(C_IDENT, 256, BF16)
    maskfox = sb(C_MASKFOX, 256, BF16)
    maskhg = sb(C_MASKHG, 256, BF16)
    cmathg = sb(C_CMATHG, 256, BF16)
    cvecA = sb(C_CVECA, 128, BF16)
    cvecB = sb(C_CVECB, 256, BF16)
    sel = sb(C_SEL, 2240, BF16).rearrange("p (a b) -> p a b", a=16)
    ones = sb(C_ONES, 4096, BF16)
    reset = sb(C_RESET, 4096, BF16)
    gattn = sb(C_GATTN, 4096, F32)
    gffn = sb(C_GFFN, 4096, F32)
    M2b = sb(C_GFIN, 2048, BF16)
    M1b = sb(C_GFIN + 2048, 2048, BF16)
    small = sb(C_SMALL, 512, F32)
    lbc = small[:, 0:4]
    oml = small[:, 4:8]
    hgg = small[:, 8:9]
    foxg = small[:, 9:10]
    negb = small[:, 10:11]
    l0 = small[:, 11:15]
    l1 = small[:, 15:19]
    ltmp = small[:, 19:23]
    brt = small[:, 24:44]
    wout = sb(C_WOUT, 16384, BF16).rearrange("p (k c) -> p k c", k=8)
    wr = sb(C_WR, 320, BF16).rearrange("p (k c) -> p k c", k=8)
    stat = sb(C_STAT, 1024, F32)
    w1s = sb(C_GATES, 256, F32)
    w2s = sb(C_GATES + 256, 256, F32)
    pos1i = sb(C_GATES + 512, 256, I32)
    pos2i = sb(C_GATES + 768, 256, I32)
    lstrict = sb(PERS, 256, BF16)
    thr = sb(PERS + 256, 64, F32)
    jt = sb(PERS + 320, 256, F32)
    pcol2 = sb(PERS + 576, 4, F32)
    idxW0 = sb(PERS + 640, 256, I32)
    idxW1 = sb(PERS + 896, 256, I32)
    rt = sb(C_RT, 1024, F32)
    xnT = sb(XNT, 32768, BF16).rearrange("p (k t) -> p k t", k=8)
    ocat = sb(OCAT, 32768, BF16).rearrange("p (k t) -> p k t", k=8)

    def A(eng, fn, reads=(), writes=(), dma=False, tag=None, extra_deps=()):
        return sch.add(eng, fn, reads, writes, dma, tag, extra_deps)

    def dma(q, out, in_, reads=(), writes=(), extra_deps=(), **kw):
        return A(q, lambda e, o=out, i=in_, kw=kw: e.dma_start(out=o, in_=i, **kw), reads, writes, dma=True, extra_deps=extra_deps)

    def mm(out, lhsT, rhs, start, stop, skip=False):
        A("pe", lambda e, o=out, l=lhsT, r=rhs, s0=start, s1=stop, sk=skip: e.matmul(o, l, r, start=s0, stop=s1, skip_group_check=sk),
          [lhsT, rhs], [out])

    def tr(out, in_, idn):
        A("pe", lambda e, o=out, i=in_, d=idn: e.transpose(o, i, d), [in_, idn], [out])

    def act(out, in_, func, bias=None, scale=None, accum=None):
        rd = [in_]
        kw = {}
        if bias is not None:
            kw["bias"] = bias
            if not isinstance(bias, float):
                rd.append(bias)
        if scale is not None:
            kw["scale"] = scale
            if not isinstance(scale, float):
                rd.append(scale)
        wr_ = [out]
        if accum is not None:
            kw["accum_out"] = accum
            wr_.append(accum)
        A("act", lambda e, o=out, i=in_, f=func, kw=kw: e.activation(o, i, f, **kw), rd, wr_)

    def ts(eng, out, in0, s1, s2, op0, op1=None, accum=None):
        rd = [in0]
        for s_ in (s1, s2):
            if s_ is not None and not isinstance(s_, (float, int)):
                rd.append(s_)
        wr_ = [out]
        kw = {}
        if accum is not None:
            kw["accum_out"] = accum
            wr_.append(accum)
        if op1 is None:
            A(eng, lambda e, o=out, i=in0, a=s1, p=op0, kw=kw: e.tensor_scalar(o, i, a, None, p, **kw), rd, wr_)
        else:
            A(eng, lambda e, o=out, i=in0, a=s1, b=s2, p=op0, q=op1, kw=kw: e.tensor_scalar(o, i, a, b, p, q, **kw), rd, wr_)

    def tt(eng, out, in0, in1, op):
        A(eng, lambda e, o=out, a=in0, b=in1, p=op: e.tensor_tensor(o, a, b, p), [in0, in1], [out])

    def stt(out, in0, scalar, in1, op0, op1, accum=None):
        rd = [in0, in1]
        if not isinstance(scalar, (float, int)):
            rd.append(scalar)
        wr_ = [out]
        kw = {}
        if accum is not None:
            kw["accum_out"] = accum
            wr_.append(accum)
        A("dve", lambda e, o=out, a=in0, s_=scalar, b=in1, p=op0, q=op1, kw=kw:
          e.scalar_tensor_tensor(o, a, s_, b, p, q, **kw), rd, wr_)

    def cp(eng, out, in_):
        if eng == "act":
            A(eng, lambda e, o=out, i=in_: e.copy(o, i), [in_], [out])
        else:
            A(eng, lambda e, o=out, i=in_: e.tensor_copy(o, i), [in_], [out])

    def memset(eng, out, val):
        A(eng, lambda e, o=out, v=val: e.memset(o, v), [], [out])

    def recip(out, in_):
        A("dve", lambda e, o=out, i=in_: e.reciprocal(o, i), [in_], [out])

    dma("sp", ident, ci_d, writes=[ident])
    dma("sp", maskfox, cmf_d, writes=[maskfox])
    dma("sp", maskhg, cmh_d, writes=[maskhg])
    dma("sp", cvecA[0:65, :], cva_d, writes=[cvecA[0:65, :]])
    dma("sp", cvecB, cvb_d, writes=[cvecB])
    dma("sp", sel[0:97, :, :], csel_d.rearrange("p (a b) -> p a b", a=16), writes=[sel[0:97, :, :]])
    dma("sp", gattn, gattn_d.partition_broadcast(128), writes=[gattn])
    dma("sp", gffn, gffn_d.partition_broadcast(128), writes=[gffn])
    dma("sp", lstrict, cls_d, writes=[lstrict])
    dma("sp", thr, cthr_d, writes=[thr])
    dma("sp", jt, cjt_d, writes=[jt])
    dma("sp", pcol2, cp2_d, writes=[pcol2])
    dma("sp", brt, br_d.partition_broadcast(128), writes=[brt])
    nonc = dict(allow_slow_non_contiguous=True)
    dma("sp", l0, lbl_d[0:1, :].rearrange("o (h p) -> p (o h)", p=128), writes=[l0], **nonc)
    dma("sp", l1, lbl_d[1:2, :].rearrange("o (h p) -> p (o h)", p=128), writes=[l1], **nonc)
    dma("sp", hgg, hgn_d.rearrange("o p -> p o"), writes=[hgg], **nonc)
    dma("sp", foxg[0:64, :], fxn_d.rearrange("o p -> p o"), writes=[foxg[0:64, :]], **nonc)
    dma("sp", foxg[64:128, :], fxn_d.rearrange("o p -> p o"), writes=[foxg[64:128, :]], **nonc)
    memset("dve", negb, 0.0)
    for g in range(3):
        dma("sp", negb[32 * g:32 * g + 8, :], fb_d.rearrange("o p -> p o"), writes=[negb[32 * g:32 * g + 8, :]], **nonc)
    dma("pool", wout, wout_d.rearrange("(k p) c -> p k c", p=128), writes=[wout])
    dma("pool", wr, wr_d.rearrange("(k p) c -> p k c", p=128), writes=[wr])
    memset("dve", ones, 1.0)
    memset("dve", reset, 1.0)
    memset("dve", reset.rearrange("p (c j) -> p c j", j=64)[:, :, 0:1], 0.0)
    memset("dve", cmathg, 1.0 / 128)
    tt("dve", ltmp, l1, l0, ALU.subtract)
    act(ltmp, ltmp, AF.Exp)
    ts("dve", ltmp, ltmp, 1.0, None, ALU.add)
    recip(lbc, ltmp)
    ts("dve", oml, lbc, -1.0, 1.0, ALU.mult, ALU.add)

    def rms_rstd(src, junk, col):
        ssq = stat[:, col:col + 1]
        var = stat[:, col + 1:col + 2]
        lnv = stat[:, col + 2:col + 3]
        rstd = stat[:, col + 3:col + 4]
        act(junk, src, AF.Square, accum=ssq)
        ts("dve", var, ssq, 1.0 / D, EPS, ALU.mult, ALU.add)
        act(lnv, var, AF.Ln)
        act(rstd, lnv, AF.Exp, scale=-0.5)
        return rstd

    pbank = [0]

    def next_bank(lo, hi):
        b = lo + (pbank[0] % (hi - lo))
        pbank[0] += 1
        return b

    for sq_ in range(nseq):
        xt_s = [sb(W + 77056 + 4096 * i, 4096, F32) for i in range(2)]
        junk = sb(W + 85248, 2048, BF16)
        xs_s = [sb(W + 87296 + 2048 * i, 2048, BF16) for i in range(2)]
        wfox = sb(W, 24704, BF16).rearrange("p (k c) -> p k c", k=8)
        dma("pool", wfox, win_d.rearrange("(k p) c -> p k c", p=128)[:, :, 2048:INC], writes=[wfox])
        for i in range(NT):
            xt = xt_s[i % 2]
            xs = xs_s[i % 2]
            dma("sp", xt, x_d[sq_, i * 128:(i + 1) * 128, :], writes=[xt])
            rstd = rms_rstd(xt, junk, 4 * (i % 8))
            stt(xs, xt, rstd, gattn, ALU.mult, ALU.mult)
            b = next_bank(0, 2)
            pb = bankbf(b).rearrange("p (k t) -> p k t", k=8)
            for kc in range(8):
                tr(pb[:, kc, :], xs[:, kc * 128:(kc + 1) * 128], ident)
            cp("act" if i % 2 else "dve", xnT[:, :, i * 128:(i + 1) * 128], pb)
        if stop_after == "prep":
            break

        vaug = sb(W + 24704, 24704, BF16).rearrange("p (t c) -> p t c", t=NT)
        qa_s = [sb(W + 49408 + 4096 * i, 4096, BF16) for i in range(2)]
        ka_s = [sb(W + 57600 + 4096 * i, 4096, BF16) for i in range(2)]
        parts = sb(W + 65792, 4096, BF16)
        pT_s = [sb(W + 69888 + 1024 * i, 1024, BF16) for i in range(2)] + [sb(W + 94592, 1024, BF16)]
        sqf = sb(W + 71936, 1024, BF16)
        lnr = sb(W + 72960, 2048, F32)
        rr = sb(W + 75008, 2048, F32)
        fr0 = sb(W + 77056, 8192, F32)
        fr1 = sb(W + 85248, 8192, F32)

        def vcol(h):
            return (h // 2) * 193 + (0 if h % 2 == 0 else 65)

        wf3 = sb(W + 93440, 1152, BF16).rearrange("p (k c) -> p k c", k=8)
        hiT = sb(W + 69888, 4096, BF16)
        memset("dve", parts, 1.0)
        memset("pool", wf3, 0.0)
        memset("pool", vaug, 0.0)
        for h in range(8):
            c1 = vcol(h) + (64 if h % 2 == 0 else 0)
            memset("dve", vaug[:, :, c1:c1 + 1], 1.0)
        for g in range(3):
            cp("dve", wf3[:, :, 32 * g:32 * g + 8], wfox[:, :, 1536:1544])
        for tb in range(4):
            b = next_bank(0, 2)
            pf = banks[b][0:72, :]
            for kc in range(8):
                mm(pf, wf3[:, kc, :], xnT[:, kc, tb * 512:(tb + 1) * 512], kc == 0, kc == 7)
            act(fr0[0:72, tb * 512:(tb + 1) * 512], pf, AF.Identity, bias=negb[0:72, :])
        f0 = fr0[0:72, :]
        f1 = fr1[0:72, :]
        h72 = hiT[0:72, :]
        chain = [
            lambda: ts("dve", f1, f0, -80.0, None, ALU.max),
            lambda: act(f1, f1, AF.Exp, scale=-1.0),
            lambda: act(f1, f1, AF.Ln, bias=1.0),
            lambda: ts("dve", f0, f1, -1.0, None, ALU.mult),
            lambda: A("dve", lambda e, o=f1, a=ones[0:72, :], b_=f0: e.tensor_tensor_scan(o, a, b_, 0.0, ALU.mult, ALU.add),
                      [ones[0:72, :], f0], [f1]),
            lambda: cp("dve", h72, f1),
            lambda: cp("dve", parts[0:8, :], hiT[0:8, :]),
            lambda: tt("dve", f0, f1, h72, ALU.subtract),
            lambda: cp("dve", h72, f0),
            lambda: cp("dve", parts[32:40, :], hiT[32:40, :]),
            lambda: tt("dve", f1, f0, h72, ALU.subtract),
            lambda: cp("dve", parts[64:72, :], fr1[64:72, :]),
        ]
        for i in range(NT):
            b = next_bank(0, 2)
            pv = banks[b]
            for kc in range(8):
                mm(pv[:, :], xnT[:, kc, i * 128:(i + 1) * 128], wfox[:, kc, 1024:1536], kc == 0, kc == 7)
            pv4 = pv[:, :].rearrange("p (h two d) -> p h two d", two=2, d=64)
            vg = vaug[:, i, :].rearrange("p (h c) -> p h c", c=193)
            cp("dve", vg[:, :, 0:64], pv4[:, :, 0, :])
            cp("act", vg[:, :, 129:193], pv4[:, :, 1, :])
            if chain:
                chain.pop(0)()
        while chain:
            chain.pop(0)()

        fox_pending = []
        lgcnt = [0]

        def proj_groups(h):
            qa_ = qa_s[h % 2]
            ka_ = ka_s[h % 2]
            out = []
            for tb in range(4):
                for which in (0, 1):
                    st = {}

                    def c0_(tb=tb, which=which, h=h, st=st):
                        cs = slice(tb * 512, (tb + 1) * 512)
                        st["pq"] = banks[next_bank(0, 2)]
                        pq = st["pq"]
                        mm(pq[0:70, :], sel[0:97, which * 8 + h, :], parts[0:97, cs], True, True)
                        wc = which * 512 + h * 64
                        for kc in range(0, 2):
                            mm(pq[0:64, :], wfox[:, kc, wc:wc + 64], xnT[:, kc, cs], False, False, skip=True)

                    def c1_(tb=tb, which=which, h=h, st=st):
                        cs = slice(tb * 512, (tb + 1) * 512)
                        pq = st["pq"]
                        wc = which * 512 + h * 64
                        for kc in range(2, 5):
                            mm(pq[0:64, :], wfox[:, kc, wc:wc + 64], xnT[:, kc, cs], False, False, skip=True)

                    def c2_(tb=tb, which=which, h=h, st=st, qa_=qa_, ka_=ka_):
                        cs = slice(tb * 512, (tb + 1) * 512)
                        pq = st["pq"]
                        wc = which * 512 + h * 64
                        for kc in range(5, 8):
                            mm(pq[0:64, :], wfox[:, kc, wc:wc + 64], xnT[:, kc, cs], False, False, skip=True)
                        if which == 0:
                            ts("dve", qa_[0:70, cs], pq[0:70, :], 0.125, None, ALU.mult)
                        else:
                            cp("dve", ka_[0:70, cs], pq[0:70, :])
                    out += [c0_, c1_, c2_]
            return out

        for g_ in proj_groups(0):
            g_()
        for h in range(8):
            qa = qa_s[h % 2]
            ka = ka_s[h % 2]
            odd = h % 2
            nxt = proj_groups(h + 1) if h + 1 < 8 else []
            it_cnt = [0]
            vc = vcol(h)
            vw = 65 if not odd else 128
            for tb in range(4):
                t0 = tb * 512
                ob = 5 + (h * 4 + tb) % 2
                n_s = 4 * (tb + 1)
                orow = slice(0, 65) if not odd else slice(0, 128)
                lgb = {}

                def emit_qk(j, t0=t0, ka=ka, qa=qa, lgb=lgb):
                    s0 = j * 128
                    c0 = max(0, s0 - t0)
                    diag = s0 >= t0
                    lb_ = 2 + lgcnt[0] % 3
                    lgcnt[0] += 1
                    lgb[j] = lb_
                    lg = banks[lb_]
                    mm(lg[:, c0:512], ka[0:70, s0:s0 + 128], qa[0:70, t0 + c0:t0 + 512], True, not diag)
                    if diag:
                        mm(lg[:, c0:c0 + 128], ident, maskfox, False, True)

                emit_qk(0)
                if n_s > 1:
                    emit_qk(1)
                for j in range(n_s):
                    if j + 2 < n_s:
                        emit_qk(j + 2)
                    s0 = j * 128
                    c0 = max(0, s0 - t0)
                    lg = banks[lgb[j]]
                    pT = pT_s[j % 3]
                    act(pT[:, c0:512], lg[:, c0:512], AF.Exp)
                    mm(banks[ob][orow, c0:512], vaug[:, j, vc:vc + vw], pT[:, c0:512], j == 0, j == n_s - 1)
                    if j == min(1, n_s - 1) and fox_pending:
                        fox_pending.pop()()
                    it_cnt[0] += 1
                    if (it_cnt[0] % 2 == 0) and nxt:
                        nxt.pop(0)()

                def norm_ops(ob=ob, odd=odd, h=h, t0=t0):
                    po = banks[ob]
                    if not odd:
                        act(sqf[0:65, :], po[0:65, :], AF.Square)
                        mm(banks[7][0:64, :], cvecA[0:65, :], sqf[0:65, :], True, True)
                        prow = slice(0, 64)
                    else:
                        act(sqf[:, :], po[:, :], AF.Square)
                        mm(banks[7][:, :], cvecB[:, :], sqf[:, :], True, True)
                        prow = slice(64, 128)
                    act(lnr[prow, :], banks[7][prow, :], AF.Ln)
                    act(rr[prow, :], lnr[prow, :], AF.Exp, scale=-0.5)
                    stt(ocat[prow, 4 + h // 2, t0:t0 + 512], po[prow, :], foxg[prow, :], rr[prow, :], ALU.mult, ALU.mult)

                fox_pending.append(norm_ops)
            while nxt:
                nxt.pop(0)()
        while fox_pending:
            fox_pending.pop()()
        if stop_after == "fox":
            break

        whg = sb(W, 32768, BF16).rearrange("p (k c) -> p k c", k=8)
        dma("pool", whg, win_d.rearrange("(k p) c -> p k c", p=128)[:, :, 0:2048], writes=[whg])
        T0 = sb(W + 32768, 8192, F32)
        T1 = sb(W + 40960, 8192, F32)
        T2 = sb(W + 49152, 8192, F32)
        T3 = sb(W + 57344, 8192, F32)
        qtl = sb(W + 65536, 4096, BF16)
        ktl = sb(W + 69632, 4096, BF16)
        ktok = sb(W + 73728, 4096, BF16).rearrange("p (t k) -> p t k", t=NT)
        vtok = sb(W + 77824, 4096, BF16).rearrange("p (t k) -> p t k", t=NT)
        sgT = sb(W + 81920, 4096, BF16)
        Sbf = sb(W + 86016, 8192, BF16).rearrange("p (c v) -> p c v", c=32)
        U_s = [sb(W + 95744 + 512 * i, 512, F32) for i in range(4)]
        scm_s = [sb(W + 95232 + 256 * i, 256, BF16) for i in range(2)]
        lnr2 = sb(W + 40960, 2048, F32)
        rr2 = sb(W + 40960 + 2048, 2048, F32)
        t1b = sb(W + 40960 + 4096, 2048, F32)
        sqh = sb(W + 40960 + 6144, 1024, BF16)
        for h in range(4):
            hs = slice(h * 128, (h + 1) * 128)
            def hproj(coff, dst, fn_, h=h):
                for tb in range(4):
                    cs = slice(tb * 512, (tb + 1) * 512)
                    b = next_bank(0, 2)
                    pp = banks[b]
                    for kc in range(8):
                        mm(pp[:, :], whg[:, kc, coff + h * 128:coff + (h + 1) * 128], xnT[:, kc, cs], kc == 0, kc == 7)
                    act(dst[:, cs], pp[:, :], fn_)

            hproj(512, T1, AF.Sigmoid)
            ts("dve", T1, T1, oml[:, h:h + 1], lbc[:, h:h + 1], ALU.mult, ALU.add)
            hproj(0, T0, AF.Silu)
            act(T2, T1, AF.Ln)
            ts("dve", T1, T1, -1.0, 1.0, ALU.mult, ALU.add)
            A("dve", lambda e, o=T3, a=reset, b_=T2: e.tensor_tensor_scan(o, a, b_, 0.0, ALU.mult, ALU.add),
              [reset, T2], [T3])
            hproj(1536, sgT, AF.Silu)
            act(T2, T3, AF.Exp)
            act(T3, T3, AF.Exp, scale=-1.0)
            tt("dve", ktl, T1, T3, ALU.mult)
            for i4 in range(4):
                b = next_bank(0, 2)
                pp = banks[b]
                for ii in range(4):
                    i = i4 * 4 + ii
                    for kc in range(8):
                        mm(pp[:, ii * 128:(ii + 1) * 128], xnT[:, kc, i * 128:(i + 1) * 128],
                           whg[:, kc, 1024 + h * 128:1024 + (h + 1) * 128], kc == 0, kc == 7)
                cp("dve", vtok[:, i4 * 4:(i4 + 1) * 4, :], pp[:, :].rearrange("p (t k) -> p t k", t=4))
            tt("dve", qtl, T0, T2, ALU.mult)
            for i8 in range(2):
                b = next_bank(0, 2)
                pb = bankbf(b).rearrange("p (t k) -> p t k", t=8)
                for ii in range(8):
                    i = i8 * 8 + ii
                    tr(pb[:, ii, :], ktl[:, i * 128:(i + 1) * 128], ident)
                cp("act", ktok[:, i8 * 8:(i8 + 1) * 8, :], pb)
            for c in range(32):
                i, par = c // 2, c % 2
                if c % 4 == 0:
                    sb_ = next_bank(2, 5)
                dS = banks[sb_][:, (c % 4) * 128:(c % 4 + 1) * 128]
                ps_ = slice(par * 64, par * 64 + 64)
                mm(dS, ktok[ps_, i, :], vtok[ps_, i, :], True, True)
                Uc = U_s[c % 4]
                Up = U_s[(c + 3) % 4]
                if c == 0:
                    memset("pool", Sbf[:, 0, :], 0.0)
                    cp("dve", Uc, dS)
                else:
                    Dp = T2[:, (c - 1) * 64 + 63:(c - 1) * 64 + 64]
                    act(Sbf[:, c, :], Up, AF.Copy, scale=Dp)
                    stt(Uc, Up, Dp, dS, ALU.mult, ALU.add)
            for tb in range(4):
                ob = 5 + (h * 4 + tb) % 2
                po = banks[ob]
                scb = {}

                def emit_sc(ii, tb=tb, scb=scb):
                    i = tb * 4 + ii
                    cs = slice(i * 128, (i + 1) * 128)
                    lb_ = next_bank(2, 5)
                    sc = banks[lb_][:, 0:128]
                    mm(sc, ktl[:, cs], qtl[:, cs], True, True)
                    scm = scm_s[i % 2]
                    tt("dve", scm, sc, maskhg, ALU.mult)
                    scb[ii] = scm

                emit_sc(0)
                for ii in range(4):
                    i = tb * 4 + ii
                    if ii + 1 < 4:
                        emit_sc(ii + 1)
                    scm = scb[ii]
                    oo = po[:, ii * 128:(ii + 1) * 128]
                    mm(oo, vtok[:, i, :], scm, True, False)
                    mm(oo[:, 0:64], Sbf[:, 2 * i, :], qtl[:, i * 128:i * 128 + 64], False, False)
                    mm(oo[:, 64:128], Sbf[:, 2 * i + 1, :], qtl[:, i * 128 + 64:i * 128 + 128], False, True)
                act(sqh, po[:, :], AF.Square)
                mm(banks[7][:, :], cmathg, sqh, True, True)
                act(lnr2, banks[7][:, :], AF.Ln, bias=EPS)
                act(rr2, lnr2, AF.Exp, scale=-0.5)
                stt(t1b, po[:, :], hgg, rr2, ALU.mult, ALU.mult)
                tt("pool", ocat[:, h, tb * 512:(tb + 1) * 512], t1b, sgT[:, tb * 512:(tb + 1) * 512], ALU.mult)
        if stop_after == "hg":
            break

        hb_s = [sb(W + 4096 * i, 4096, F32) for i in range(2)]
        xt2_s = [sb(W + 8192 + 4096 * i, 4096, F32) for i in range(2)]
        junk2 = sb(W + 16384, 2048, BF16)
        hn_s = [sb(W + 18432 + 2048 * i, 2048, BF16) for i in range(2)]
        hnt_s = [sb(W + 22528 + 2048 * i, 2048, BF16).rearrange("p (k t) -> p k t", k=8) for i in range(2)]
        for i in range(NT):
            gi = sq_ * NT + i
            rows = slice(gi * 128, (gi + 1) * 128)
            xt = xt2_s[i % 2]
            hb = hb_s[i % 2]
            if i == 0:
                dma("sp", xt, x_d[sq_, 0:128, :], writes=[xt])
            if i + 1 < NT:
                dma("sp", xt2_s[(i + 1) % 2], x_d[sq_, (i + 1) * 128:(i + 2) * 128, :], writes=[xt2_s[(i + 1) % 2]])
            for half in range(2):
                b = next_bank(0, 2)
                ph = banks[b]
                for fc in range(8):
                    mm(ph[:, :], ocat[:, fc, i * 128:(i + 1) * 128], wout[:, fc, half * 512:(half + 1) * 512], fc == 0, fc == 7)
                tt("dve", hb[:, half * 512:(half + 1) * 512], ph[:, :], xt[:, half * 512:(half + 1) * 512], ALU.add)
            dma("sp", h_scr[rows, :], hb, reads=[hb], writes=[h_scr[rows, :]])
            rstd = rms_rstd(hb, junk2, 32 + 4 * (i % 8))
            hn = hn_s[i % 2]
            stt(hn, hb, rstd, gffn, ALU.mult, ALU.mult)
            dma("sp", hn_scr[rows, :], hn, reads=[hn], writes=[hn_scr[rows, :]])
            b = next_bank(2, 4)
            pb = bankbf(b).rearrange("p (k t) -> p k t", k=8)
            for kc in range(8):
                tr(pb[:, kc, :], hn[:, kc * 128:(kc + 1) * 128], ident)
            hnt = hnt_s[i % 2]
            cp("act", hnt, pb)
            b = next_bank(4, 6)
            pr = banks[b][:, 0:20]
            for kc in range(8):
                mm(pr, hnt[:, kc, :], wr[:, kc, :], kc == 0, kc == 7)
            ro = (i % 2) * 128
            lgt = rt[:, ro:ro + 20]
            gmax = rt[:, ro + 20:ro + 21]
            ngmax = rt[:, ro + 21:ro + 22]
            gm = rt[:, ro + 22:ro + 26]
            eg = rt[:, ro + 26:ro + 30]
            sumg = rt[:, ro + 30:ro + 31]
            pgs = rt[:, ro + 31:ro + 32]
            pen = rt[:, ro + 32:ro + 48]
            elm = rt[:, ro + 48:ro + 64]
            top8 = rt[:, ro + 64:ro + 72]
            nm1 = rt[:, ro + 72:ro + 73]
            selm = rt[:, ro + 73:ro + 89]
            den2 = rt[:, ro + 89:ro + 90]
            ex = rt[:, ro + 91:ro + 107]
            tt("dve", lgt, pr, brt, ALU.add)
            A("dve", lambda e, o=gmax, i_=lgt[:, 0:4]: e.reduce_max(o, i_, AX.X), [lgt[:, 0:4]], [gmax])
            ts("dve", gm, lgt[:, 0:4], gmax, None, ALU.is_equal)
            ts("dve", ngmax, gmax, -1.0, None, ALU.mult)
            act(eg, lgt[:, 0:4], AF.Exp, bias=ngmax, accum=sumg)
            recip(pgs, sumg)
            gm_b = bass.AP(gm.tensor, gm.offset, [list(gm.ap[0]), [1, 4], [0, 4]])
            pen3 = pen.rearrange("p (g j) -> p g j", j=4)
            ts("dve", pen3, gm_b, -1.0, 1e30, ALU.add, ALU.mult)
            tt("dve", elm, lgt[:, 4:20], pen, ALU.add)
            A("dve", lambda e, o=top8, i_=elm: e.max(o, i_), [elm], [top8])
            ts("dve", selm, elm, top8[:, 1:2], None, ALU.is_ge)
            cp("dve", M2b[:, gi * 16:(gi + 1) * 16], selm)
            ts("dve", M1b[:, gi * 16:(gi + 1) * 16], elm, top8[:, 0:1], None, ALU.is_equal)
            ts("dve", nm1, top8[:, 0:1], -1.0, None, ALU.mult)
            act(ex, elm, AF.Exp, bias=nm1)
            stt(ex, ex, 1.0, selm, ALU.mult, ALU.mult, accum=den2)
            recip(den2, den2)
            tt("dve", w1s[:, gi:gi + 1], den2, pgs, ALU.mult)
            tt("dve", w2s[:, gi:gi + 1], pgs, w1s[:, gi:gi + 1], ALU.subtract)

    if stop_after is None:
        NTT16 = NTT * 16
        def f32t(k):
            return sb(XNT + 4096 * k, 4096, F32)[:, 0:NTT16]
        TotS, RS, CI, EX, PP, TMP = [f32t(k) for k in range(6)]
        small2 = sb(XNT + 4096 * 6, 4096, F32)
        ne = small2[:, 0:16]
        ke = small2[:, 16:32]
        cki = small2[:, 32:48]
        base = small2[:, 48:64]
        cmp_ = small2[:, 64:320]
        ejf = small2[:, 320:384]
        cmp2 = sb(XNT + 4096 * 7, 4096, F32)
        pos1f = small2[:, 384:448]
        pos2f = small2[:, 448:512]
        idf0 = small2[:, 512:576]
        idf1 = small2[:, 576:640]
        for half in range(0, NTT16, 512):
            n_ = min(512, NTT16 - half)
            mm(banks[0][:, 0:n_], lstrict, M2b[:, half:half + n_], True, True)
            cp("dve", RS[:, half:half + n_], banks[0][:, 0:n_])
            mm(banks[1][:, 0:n_], ones[:, 0:128], M2b[:, half:half + n_], True, True)
            cp("dve", TotS[:, half:half + n_], banks[1][:, 0:n_])
        TotS3 = TotS.rearrange("p (t e) -> p t e", e=16)
        CI3 = CI.rearrange("p (t e) -> p t e", e=16)
        for e_ in range(16):
            A("dve", lambda e, o=CI3[:, :, e_], a=ones[:, 0:NTT], b_=TotS3[:, :, e_]:
              e.tensor_tensor_scan(o, a, b_, 0.0, ALU.mult, ALU.add), [ones[:, 0:NTT], TotS3[:, :, e_]], [CI3[:, :, e_]])
        tt("dve", EX, CI, TotS, ALU.subtract)
        cp("dve", ne, CI3[:, NTT - 1, :])
        ne_b = bass.AP(ne.tensor, ne.offset, [list(ne.ap[0]), [1, 16], [0, 16]])
        thr_b = bass.AP(thr.tensor, thr.offset, [list(thr.ap[0]), [0, 16], [1, 16]])
        tt("dve", cmp_.rearrange("p (a b) -> p a b", b=16), ne_b, thr_b, ALU.is_gt)
        A("dve", lambda e, o=ke, i_=cmp_.rearrange("p (a b) -> p a b", b=16): e.reduce_sum(o, i_, AX.X),
          [cmp_], [ke])
        A("dve", lambda e, o=cki, a=ones[:, 0:16], b_=ke: e.tensor_tensor_scan(o, a, b_, 0.0, ALU.mult, ALU.add),
          [ones[:, 0:16], ke], [cki])
        tt("dve", base, cki, ke, ALU.subtract)
        ts("dve", base, base, 512.0, None, ALU.mult)
        base_b = bass.AP(base.tensor, base.offset, [list(base.ap[0]), [0, NTT], [1, 16]])
        tt("dve", PP, RS, EX, ALU.add)
        PP3 = PP.rearrange("p (t e) -> p t e", e=16)
        tt("dve", PP3, PP3, base_b, ALU.add)
        tt("dve", TMP, PP, M1b[:, 0:NTT16], ALU.mult)
        A("dve", lambda e, o=pos1f[:, 0:NTT], i_=TMP.rearrange("p (t e) -> p t e", e=16): e.reduce_sum(o, i_, AX.X),
          [TMP], [pos1f[:, 0:NTT]])
        tt("dve", TMP, PP, M2b[:, 0:NTT16], ALU.mult)
        A("dve", lambda e, o=pos2f[:, 0:NTT], i_=TMP.rearrange("p (t e) -> p t e", e=16): e.reduce_sum(o, i_, AX.X),
          [TMP], [pos2f[:, 0:NTT]])
        tt("dve", pos2f[:, 0:NTT], pos2f[:, 0:NTT], pos1f[:, 0:NTT], ALU.subtract)
        cp("dve", pos1i[:, 0:NTT], pos1f[:, 0:NTT])
        cp("dve", pos2i[:, 0:NTT], pos2f[:, 0:NTT])
        cki_b = bass.AP(cki.tensor, cki.offset, [list(cki.ap[0]), [0, NSLOT], [1, 16]])
        jt_b = bass.AP(jt.tensor, jt.offset, [list(jt.ap[0]), [1, NSLOT], [0, 16]])
        c23 = cmp2[:, 0:NSLOT * 16].rearrange("p (j e) -> p j e", e=16)
        tt("dve", c23, cki_b, jt_b, ALU.is_le)
        A("dve", lambda e, o=ejf[:, 0:NSLOT], i_=c23: e.reduce_sum(o, i_, AX.X), [cmp2[:, 0:NSLOT * 16]], [ejf[:, 0:NSLOT]])
        ts("dve", ejf[:, 0:NSLOT], ejf[:, 0:NSLOT], 15.0, None, ALU.min)
        ts("dve", idf0[:, 0:NSLOT], ejf[:, 0:NSLOT], 256.0, pcol2, ALU.mult, ALU.add)
        ts("dve", idf1[:, 0:NSLOT], idf0[:, 0:NSLOT], 1.0, None, ALU.add)
        cp("dve", idxW0[:, 0:NSLOT], idf0[:, 0:NSLOT])
        cp("dve", idxW1[:, 0:NSLOT], idf1[:, 0:NSLOT])

        def idma(out, in_, out_off=None, in_off=None, reads=(), writes=()):
            def fn(e, o=out, i=in_, oo=out_off, io=in_off):
                return e.indirect_dma_start(
                    out=o, out_offset=(bass.IndirectOffsetOnAxis(ap=oo, axis=0) if oo is not None else None),
                    in_=i, in_offset=(bass.IndirectOffsetOnAxis(ap=io, axis=0) if io is not None else None))
            return A("pool", fn, reads, writes, dma=True)

        hnc_s = [sb(XNT + 110592 + 2048 * i, 2048, BF16) for i in range(16)]
        scat_ops = []
        for gi in range(NTT):
            hnc = hnc_s[gi % 16]
            rows = slice(gi * 128, (gi + 1) * 128)
            dma("sp", hnc, hn_scr[rows, :], reads=[hn_scr[rows, :]], writes=[hnc])
            scat_ops.append(idma(so_scr[:, :], hnc, out_off=pos1i[:, gi:gi + 1], reads=[hnc, pos1i[:, gi:gi + 1]]))
            scat_ops.append(idma(so_scr[:, :], hnc, out_off=pos2i[:, gi:gi + 1], reads=[hnc, pos2i[:, gi:gi + 1]]))

        D0 = XNT

        def d_views(j):
            wo = D0 + 24576 * (j % 2)
            wg = sb(wo, 8192, BF16)
            wu = sb(wo + 8192, 8192, BF16)
            wd = sb(wo + 16384, 8192, BF16)
            xtok = sb(D0 + 49152 + 8192 * (j % 2), 8192, BF16).rearrange("p (a c) -> p a c", a=4)
            return wg, wu, wd, xtok

        def d_load(j):
            wg, wu, wd, xtok = d_views(j)
            srows = so_scr[j * 512:(j + 1) * 512, :]
            dma("sp", xtok, srows.rearrange("(a p) c -> p a c", p=128), reads=[srows], writes=[xtok], extra_deps=scat_ops)
            for (dst, src) in ((wg, wg_d), (wu, wu_d), (wd, wd_d)):
                idma(dst[:, 0:2048], src[:, :], in_off=idxW0[:, j:j + 1], reads=[idxW0[:, j:j + 1]], writes=[dst[:, 0:2048]])
                idma(dst[:, 2048:4096], src[:, :], in_off=idxW1[:, j:j + 1], reads=[idxW1[:, j:j + 1]], writes=[dst[:, 2048:4096]])

        d_load(0)
        for j in range(NSLOT):
            if j + 1 < NSLOT:
                d_load(j + 1)
            wg, wu, wd, xtok = d_views(j)
            wg3 = wg.rearrange("p (k c) -> p k c", k=8)
            wu3 = wu.rearrange("p (k c) -> p k c", k=8)
            wd3 = wd.rearrange("p (k c) -> p k c", k=4)
            hsT = sb(D0 + 65536 + 8192 * (j % 2), 8192, BF16).rearrange("p (k t) -> p k t", k=8)
            hact = sb(D0 + 81920 + 4096 * (j % 2), 4096, BF16).rearrange("p (c t) -> p c t", c=4)
            ybuf = sb(D0 + 94208 + 16384 * (j % 2), 16384, F32).rearrange("p (a c) -> p a c", a=4)
            for a in range(4):
                b = 6 + a % 2
                pb = bankbf(b).rearrange("p (k t) -> p k t", k=8)
                for kc in range(8):
                    tr(pb[:, kc, :], xtok[:, a, kc * 128:(kc + 1) * 128], ident)
                cp("act" if a % 2 else "dve", hsT[:, :, a * 128:(a + 1) * 128], pb)
            for hc in range(4):
                gb = hc % 2
                ub = 2 + hc % 2
                for kc in range(8):
                    mm(banks[gb][:, :], wg3[:, kc, hc * 128:(hc + 1) * 128], hsT[:, kc, :], kc == 0, kc == 7)
                for kc in range(8):
                    mm(banks[ub][:, :], wu3[:, kc, hc * 128:(hc + 1) * 128], hsT[:, kc, :], kc == 0, kc == 7)
                sgt = sb(D0 + 90112 + 2048 * (hc % 2), 2048, F32)
                act(sgt, banks[gb][:, :], AF.Silu)
                tt("dve", hact[:, hc, :], sgt, banks[ub][:, :], ALU.mult)
            for a in range(4):
                for half in range(2):
                    yb = 4 + (a * 2 + half) % 2
                    for hc in range(4):
                        mm(banks[yb][:, :], hact[:, hc, a * 128:(a + 1) * 128], wd3[:, hc, half * 512:(half + 1) * 512], hc == 0, hc == 3)
                    cp("act" if half else "dve", ybuf[:, a, half * 512:(half + 1) * 512], banks[yb][:, :])
            yrows = y_scr[j * 512:(j + 1) * 512, :]
            dma("sp", yrows.rearrange("(a p) c -> p a c", p=128), ybuf, reads=[ybuf], writes=[yrows])

        gfin = sb(XNT + 151552, 4096, F32)
        dma("sp", gfin, gfin_d.partition_broadcast(128), writes=[gfin])
        junk3 = sb(XNT + 155648, 2048, BF16)
        def e_views(gi):
            eo = XNT + 12288 * (gi % 8)
            return sb(eo, 4096, F32), sb(eo + 4096, 4096, F32), sb(eo + 8192, 4096, F32)

        def e_load(gi):
            y1, y2, hb = e_views(gi)
            rows = slice(gi * 128, (gi + 1) * 128)
            idma(y1, y_scr[:, :], in_off=pos1i[:, gi:gi + 1], reads=[y_scr[:, :], pos1i[:, gi:gi + 1]], writes=[y1])
            idma(y2, y_scr[:, :], in_off=pos2i[:, gi:gi + 1], reads=[y_scr[:, :], pos2i[:, gi:gi + 1]], writes=[y2])
            dma("sp", hb, h_scr[rows, :], reads=[h_scr[rows, :]], writes=[hb])

        for g0 in range(min(6, NTT)):
            e_load(g0)
        for gi in range(NTT):
            sq_, i = gi // NT, gi % NT
            if gi + 6 < NTT:
                e_load(gi + 6)
            y1, y2, hb = e_views(gi)
            stt(hb, y1, w1s[:, gi:gi + 1], hb, ALU.mult, ALU.add)
            stt(hb, y2, w2s[:, gi:gi + 1], hb, ALU.mult, ALU.add)
            rstd = rms_rstd(hb, junk3, 64 + 4 * (gi % 8))
            stt(hb, hb, rstd, gfin, ALU.mult, ALU.mult)
            dma("sp", out_d[sq_, i * 128:(i + 1) * 128, :], hb, reads=[hb])

    sch.prepare()
    sems = {k: es.enter_context(nc.semaphore(f"s_{k}")) for k in ("pe", "act", "dve", "pool")}
    dsems = {}
    for q in ("sp", "pool", "act"):
        for k in range(sch.n_dma_sems):
            dsems[(q, k)] = es.enter_context(nc.semaphore(f"d_{q}{k}"))
    with nc.Block() as block:
        @block.tensor
        def _(eng):
            sch.emit_engine("pe", eng, sems, dsems)

        @block.scalar
        def _(eng):
            sch.emit_engine("act", eng, sems, dsems)

        @block.vector
        def _(eng):
            sch.emit_engine("dve", eng, sems, dsems)

        @block.gpsimd
        def _(eng):
            sch.emit_engine("pool", eng, sems, dsems)

        @block.sync
        def _(eng):
            sch.emit_engine("sp", eng, sems, dsems)
    es.close()
    return nc, sch


_CACHE = {}


def _get_nc(nseq):
    if nseq not in _CACHE:
        _CACHE[nseq] = build(nseq, Sched, F32, BF16, U8)[0]
    return _CACHE[nseq]


def kernel(x, attn_norm, w_in, hg_lb_logits, hg_norm, fox_f_bias, fox_norm, w_out, ffn_norm,
           w_group, b_group, w_expert, b_expert, w_gate, w_up, w_down, final_norm):
    f = lambda a: np.ascontiguousarray(np.asarray(a, dtype=np.float32))
    x = f(x)
    n_cores = 8
    B = x.shape[0]
    nseq = B // n_cores
    shared = {
        "w_in": f(w_in)[0], "w_out": f(w_out)[0],
        "w_gate": np.ascontiguousarray(f(w_gate)[0].reshape(16, 8, 128, 512).transpose(0, 2, 1, 3)).reshape(16 * 256, 2048),
        "w_up": np.ascontiguousarray(f(w_up)[0].reshape(16, 8, 128, 512).transpose(0, 2, 1, 3)).reshape(16 * 256, 2048),
        "w_down": np.ascontiguousarray(f(w_down)[0].reshape(16, 4, 128, 1024).transpose(0, 2, 1, 3)).reshape(16 * 256, 2048),
        "w_router": np.ascontiguousarray(np.concatenate([f(w_group)[0], f(w_expert)[0]], axis=1)),
        "b_router": np.ascontiguousarray(np.concatenate([f(b_group)[0], f(b_expert)[0]])[None, :]),
        "attn_norm": f(attn_norm), "ffn_norm": f(ffn_norm), "final_norm": f(final_norm).reshape(1, -1),
        "hg_lb_logits": f(hg_lb_logits), "hg_norm": f(hg_norm), "fox_f_bias": f(fox_f_bias),
        "fox_norm": f(fox_norm),
    }
    shared.update(make_consts())
    in_maps = []
    for c in range(n_cores):
        m = dict(shared)
        m["x"] = np.ascontiguousarray(x[c * nseq:(c + 1) * nseq])
        in_maps.append(m)
    nc = _get_nc(nseq)
    res = run_bass_kernel_spmd(nc, in_maps, core_ids=list(range(n_cores)))
    return np.concatenate([r["out"] for r in res.results], axis=0).astype(np.float32)
```
